# Optimizing a Trainium2 kernel written in Bass

```python
import math
import jax, jax.numpy as jnp
from jax import lax
import numpy as np

D_MODEL = 4096
BATCH = 4
SEQ = 2048
DEPTH = 4

CTX_LEN = 256
GRID_W = 64
N_MIXERS = 3
HEAD_DIM = 128
ROPE_THETA = 10000.0
NORM_EPS = 1e-6
MASK_VALUE = -1e30
ADA_RANK = D_MODEL // 8
A_HEADS = D_MODEL // HEAD_DIM
A_KV_HEADS = A_HEADS // 4
WINDOW = 128
BLOCK = 128
B_HEADS = D_MODEL // (2 * HEAD_DIM)
CONV_W = 3
N_EXPERTS = 64
TOP_K = 8
N_GROUPS = 8
TOPK_GROUPS = 4
EXPERT_FF = D_MODEL * 3 // 64
SHARED_FF = EXPERT_FF
ROUTED_SCALE = 2.5

kernel_name = "hybrid_interleaved_dit_moe_prefix"


def rms_norm(x, g):
    xf = x.astype(jnp.float32)
    y = xf * lax.rsqrt(jnp.mean(xf * xf, axis=-1, keepdims=True) + NORM_EPS)
    return (y * g.astype(jnp.float32)).astype(x.dtype)


def modulate(h, shift, scale):
    return h * (1 + scale) + shift


def axial_rope_tables(n_tok):
    rows = n_tok // GRID_W
    row = jnp.broadcast_to(jnp.arange(rows)[:, None], (rows, GRID_W)).reshape(-1).astype(jnp.float32)
    col = jnp.broadcast_to(jnp.arange(GRID_W)[None, :], (rows, GRID_W)).reshape(-1).astype(jnp.float32)
    n_freq = HEAD_DIM // 4
    inv = ROPE_THETA ** (-jnp.arange(n_freq, dtype=jnp.float32) / n_freq)
    ang = jnp.concatenate([row[:, None] * inv, col[:, None] * inv], axis=-1)
    return jnp.cos(ang), jnp.sin(ang)


def apply_rope(x, cos, sin):
    half = HEAD_DIM // 2
    shape = (1, x.shape[1]) + (1,) * (x.ndim - 3) + (half,)
    cs = cos.reshape(shape).astype(x.dtype)
    sn = sin.reshape(shape).astype(x.dtype)
    x1, x2 = x[..., :half], x[..., half:]
    return jnp.concatenate([x1 * cs - x2 * sn, x2 * cs + x1 * sn], axis=-1)


def _bands(t, nb):
    bsz = t.shape[0]
    tp = jnp.pad(t, ((0, 0), (BLOCK, BLOCK), (0, 0), (0, 0))).reshape(bsz, nb + 2, BLOCK, t.shape[2], t.shape[3])
    return jnp.concatenate([tp[:, :-2], tp[:, 1:-1], tp[:, 2:]], axis=2)


def windowed_gqa_sink(h, hc, w_qkv, w_o, sink, cos, sin, need_ctx):
    bsz, n_tok, _ = h.shape
    n_ctx = hc.shape[1]
    kv, grp, dh = A_KV_HEADS, A_HEADS // A_KV_HEADS, HEAD_DIM
    nq, nk = A_HEADS * dh, kv * dh
    scale = dh ** -0.5
    qkv = h @ w_qkv
    q = apply_rope(qkv[..., :nq].reshape(bsz, n_tok, kv, grp, dh), cos, sin)
    k = apply_rope(qkv[..., nq:nq + nk].reshape(bsz, n_tok, kv, dh), cos, sin)
    v = qkv[..., nq + nk:].reshape(bsz, n_tok, kv, dh)
    qkv_c = hc @ (w_qkv if need_ctx else w_qkv[:, nq:])
    kc = qkv_c[..., -2 * nk:-nk].reshape(bsz, n_ctx, kv, dh)
    vc = qkv_c[..., -nk:].reshape(bsz, n_ctx, kv, dh)
    nb = n_tok // BLOCK
    qb = q.reshape(bsz, nb, BLOCK, kv, grp, dh)
    kb, vb = _bands(k, nb), _bands(v, nb)
    s_lat = jnp.einsum('bnqkgd,bnskd->bnkgqs', qb, kb).astype(jnp.float32) * scale
    s_ctx = jnp.einsum('bnqkgd,bckd->bnkgqc', qb, kc).astype(jnp.float32) * scale
    qpos = jnp.arange(nb)[:, None] * BLOCK + jnp.arange(BLOCK)[None, :]
    kpos = jnp.arange(nb)[:, None] * BLOCK - BLOCK + jnp.arange(3 * BLOCK)[None, :]
    rel = kpos[:, None, :] - qpos[:, :, None]
    valid = (jnp.abs(rel) <= WINDOW) & (kpos[:, None, :] >= 0) & (kpos[:, None, :] < n_tok)
    s_lat = jnp.where(valid[None, :, None, None], s_lat, MASK_VALUE)
    sink_col = jnp.broadcast_to(sink.astype(jnp.float32).reshape(1, 1, kv, grp, 1, 1), s_lat.shape[:-1] + (1,))
    p = jax.nn.softmax(jnp.concatenate([s_lat, s_ctx, sink_col], axis=-1), axis=-1).astype(v.dtype)
    nw = 3 * BLOCK
    o = (jnp.einsum('bnkgqs,bnskd->bnqkgd', p[..., :nw], vb)
         + jnp.einsum('bnkgqc,bckd->bnqkgd', p[..., nw:nw + n_ctx], vc))
    y = o.reshape(bsz, n_tok, nq) @ w_o
    if not need_ctx:
        return y, None
    qc = qkv_c[..., :nq].reshape(bsz, n_ctx, kv, grp, dh)
    sc = jnp.einsum('bqkgd,bckd->bkgqc', qc, kc).astype(jnp.float32) * scale
    sink_c = jnp.broadcast_to(sink.astype(jnp.float32).reshape(1, kv, grp, 1, 1), sc.shape[:-1] + (1,))
    pc = jax.nn.softmax(jnp.concatenate([sc, sink_c], axis=-1), axis=-1).astype(vc.dtype)
    oc = jnp.einsum('bkgqc,bckd->bqkgd', pc[..., :n_ctx], vc)
    yc = oc.reshape(bsz, n_ctx, nq) @ w_o
    return y, yc


def _diff_core(q, k, v, lam, scale):
    s = jnp.einsum('bqhcd,bkhcd->bhcqk', q, k).astype(jnp.float32) * scale
    p = jax.nn.softmax(s, axis=-1)
    a = (p[:, :, 0] - lam * p[:, :, 1]).astype(v.dtype)
    return jnp.einsum('bhqk,bkhe->bqhe', a, v)


def differential_attention(h, hc, w_qkv, w_o, lam_vecs, subln_g, lam_init, cos, sin, need_ctx):
    bsz, n_tok, _ = h.shape
    n_ctx = hc.shape[1]
    nh, dh = B_HEADS, HEAD_DIM
    nq = nh * 2 * dh
    scale = dh ** -0.5
    qkv = h @ w_qkv
    q = apply_rope(qkv[..., :nq].reshape(bsz, n_tok, nh, 2, dh), cos, sin)
    k = apply_rope(qkv[..., nq:2 * nq].reshape(bsz, n_tok, nh, 2, dh), cos, sin)
    v = qkv[..., 2 * nq:].reshape(bsz, n_tok, nh, 2 * dh)
    qkv_c = hc @ (w_qkv if need_ctx else w_qkv[:, nq:])
    kc = qkv_c[..., -2 * nq:-nq].reshape(bsz, n_ctx, nh, 2, dh)
    vc = qkv_c[..., -nq:].reshape(bsz, n_ctx, nh, 2 * dh)
    lf = lam_vecs.astype(jnp.float32)
    lam = jnp.exp(jnp.sum(lf[0] * lf[1])) - jnp.exp(jnp.sum(lf[2] * lf[3])) + lam_init
    k_all = jnp.concatenate([k, kc], axis=1)
    v_all = jnp.concatenate([v, vc], axis=1)
    nb = n_tok // BLOCK
    qb = jnp.moveaxis(q.reshape(bsz, nb, BLOCK, nh, 2, dh), 1, 0)
    o = lax.map(lambda q_blk: _diff_core(q_blk, k_all, v_all, lam, scale), qb)
    o = jnp.moveaxis(o, 0, 1).reshape(bsz, n_tok, nh, 2 * dh)
    out_scale = 1.0 - lam_init
    y = (rms_norm(o, subln_g) * out_scale).reshape(bsz, n_tok, nq) @ w_o
    if not need_ctx:
        return y, None
    qc = qkv_c[..., :nq].reshape(bsz, n_ctx, nh, 2, dh)
    oc = _diff_core(qc, kc, vc, lam, scale)
    yc = (rms_norm(oc, subln_g) * out_scale).reshape(bsz, n_ctx, nq) @ w_o
    return y, yc


def depthwise_conv_centred(u, w):
    return lax.conv_general_dilated(
        u, w[:, None, :].astype(u.dtype), window_strides=(1,),
        padding=[(CONV_W // 2, CONV_W // 2)],
        dimension_numbers=('NWC', 'WIO', 'NWC'), feature_group_count=u.shape[-1])


def short_conv_mixer(h, w_in, conv_w, w_out):
    gate_b, gate_c, u = jnp.split(h @ w_in, 3, axis=-1)
    z = depthwise_conv_centred(gate_c * u, conv_w)
    return (gate_b * z) @ w_out


def moe_ffn(h, router_w, router_b, w_gate, w_up, w_down, s_gate, s_up, s_down):
    shp = h.shape
    t = h.reshape(-1, shp[-1])
    scores = jax.nn.sigmoid((t @ router_w).astype(jnp.float32))
    biased = scores + router_b.astype(jnp.float32)
    per_group = N_EXPERTS // N_GROUPS
    grp_score = lax.top_k(biased.reshape(-1, N_GROUPS, per_group), 2)[0].sum(-1)
    _, gidx = lax.top_k(grp_score, TOPK_GROUPS)
    gmask = jnp.any(gidx[..., None] == jnp.arange(N_GROUPS), axis=-2)
    emask = jnp.repeat(gmask, per_group, axis=-1)
    _, eidx = lax.top_k(jnp.where(emask, biased, MASK_VALUE), TOP_K)
    w = jnp.take_along_axis(scores, eidx, axis=-1)
    w = w / jnp.sum(w, axis=-1, keepdims=True) * ROUTED_SCALE
    gates = jnp.sum(jnp.where(eidx[..., None] == jnp.arange(N_EXPERTS), w[..., None], 0.0), axis=-2).astype(t.dtype)
    hg = jnp.einsum('td,edf->tef', t, w_gate)
    hu = jnp.einsum('td,edf->tef', t, w_up)
    act = jax.nn.silu(hg) * hu * gates[..., None]
    routed = jnp.einsum('tef,efd->td', act, w_down)
    shared = (jax.nn.silu(t @ s_gate) * (t @ s_up)) @ s_down
    return (routed + shared).reshape(shp)


def setup_inputs(seed: int = 0) -> dict:
    key = jax.random.key(seed)
    ks = jax.random.split(key, 28)
    d = D_MODEL
    n_a = (DEPTH + N_MIXERS - 1) // N_MIXERS
    n_b = (DEPTH + N_MIXERS - 2) // N_MIXERS
    n_c = DEPTH // N_MIXERS
    f32 = jnp.float32

    def nrm(k, shape, s):
        return jax.random.normal(k, shape, f32) * s

    qkv_a = (A_HEADS + 2 * A_KV_HEADS) * HEAD_DIM
    wb = B_HEADS * 2 * HEAD_DIM
    return {
        'x': nrm(ks[0], (BATCH, SEQ, d), 1.0),
        'c': nrm(ks[1], (BATCH, d), 1.0),
        'ctx': nrm(ks[2], (BATCH, CTX_LEN, d), 1.0),
        'c_ctx': nrm(ks[3], (d,), 1.0),
        'ada_down': nrm(ks[4], (DEPTH, d, ADA_RANK), d ** -0.5),
        'ada_up': nrm(ks[5], (DEPTH, ADA_RANK, 6 * d), 0.3 * ADA_RANK ** -0.5),
        'ada_b': nrm(ks[6], (DEPTH, 6 * d), 0.02),
        'norm1_g': 1.0 + nrm(ks[7], (DEPTH, d), 0.02),
        'norm2_g': 1.0 + nrm(ks[8], (DEPTH, d), 0.02),
        'a_w_qkv': nrm(ks[9], (n_a, d, qkv_a), d ** -0.5),
        'a_w_o': nrm(ks[10], (n_a, A_HEADS * HEAD_DIM, d), (A_HEADS * HEAD_DIM) ** -0.5),
        'a_sink': nrm(ks[11], (n_a, A_HEADS), 0.5),
        'b_w_qkv': nrm(ks[12], (n_b, d, 3 * wb), d ** -0.5),
        'b_w_o': nrm(ks[13], (n_b, wb, d), wb ** -0.5),
        'b_lam': nrm(ks[14], (n_b, 4, HEAD_DIM), 0.1),
        'b_subln_g': 1.0 + nrm(ks[15], (n_b, 2 * HEAD_DIM), 0.02),
        'c_w_in': nrm(ks[16], (n_c, d, 3 * d), d ** -0.5),
        'c_conv': nrm(ks[17], (n_c, CONV_W, d), CONV_W ** -0.5),
        'c_w_out': nrm(ks[18], (n_c, d, d), d ** -0.5),
        'router_w': nrm(ks[19], (DEPTH, d, N_EXPERTS), d ** -0.5),
        'router_b': nrm(ks[20], (DEPTH, N_EXPERTS), 0.01),
        'exp_gate': nrm(ks[21], (DEPTH, N_EXPERTS, d, EXPERT_FF), d ** -0.5),
        'exp_up': nrm(ks[22], (DEPTH, N_EXPERTS, d, EXPERT_FF), d ** -0.5),
        'exp_down': nrm(ks[23], (DEPTH, N_EXPERTS, EXPERT_FF, d), EXPERT_FF ** -0.5),
        'sh_gate': nrm(ks[24], (DEPTH, d, SHARED_FF), d ** -0.5),
        'sh_up': nrm(ks[25], (DEPTH, d, SHARED_FF), d ** -0.5),
        'sh_down': nrm(ks[26], (DEPTH, SHARED_FF, d), SHARED_FF ** -0.5),
        'final_g': 1.0 + nrm(ks[27], (d,), 0.02),
    }


def reference(x, c, ctx, c_ctx, ada_down, ada_up, ada_b, norm1_g, norm2_g,
              a_w_qkv, a_w_o, a_sink, b_w_qkv, b_w_o, b_lam, b_subln_g,
              c_w_in, c_conv, c_w_out, router_w, router_b, exp_gate, exp_up, exp_down,
              sh_gate, sh_up, sh_down, final_g):
    cos, sin = axial_rope_tables(x.shape[1])
    mod_src = jax.nn.silu(c)
    mod_src_ctx = jax.nn.silu(c_ctx)
    for i in range(DEPTH):
        kind, j = i % N_MIXERS, i // N_MIXERS
        need_ctx = i < DEPTH - 1
        mod = (mod_src @ ada_down[i]) @ ada_up[i] + ada_b[i]
        sh1, sc1, g1, sh2, sc2, g2 = jnp.split(mod[:, None, :], 6, axis=-1)
        modc = (mod_src_ctx @ ada_down[i]) @ ada_up[i] + ada_b[i]
        csh1, csc1, cg1, csh2, csc2, cg2 = jnp.split(modc, 6, axis=-1)
        a = modulate(rms_norm(x, norm1_g[i]), sh1, sc1)
        if kind == 0:
            ac = modulate(rms_norm(ctx, norm1_g[i]), csh1, csc1)
            y, yc = windowed_gqa_sink(a, ac, a_w_qkv[j], a_w_o[j], a_sink[j], cos, sin, need_ctx)
        elif kind == 1:
            ac = modulate(rms_norm(ctx, norm1_g[i]), csh1, csc1)
            lam_init = 0.8 - 0.6 * math.exp(-0.3 * i)
            y, yc = differential_attention(a, ac, b_w_qkv[j], b_w_o[j], b_lam[j], b_subln_g[j],
                                           lam_init, cos, sin, need_ctx)
        else:
            y = short_conv_mixer(a, c_w_in[j], c_conv[j], c_w_out[j])
            yc = None
            if need_ctx:
                ac = modulate(rms_norm(ctx, norm1_g[i]), csh1, csc1)
                yc = short_conv_mixer(ac, c_w_in[j], c_conv[j], c_w_out[j])
        x = x + g1 * y
        f = modulate(rms_norm(x, norm2_g[i]), sh2, sc2)
        x = x + g2 * moe_ffn(f, router_w[i], router_b[i], exp_gate[i], exp_up[i], exp_down[i],
                             sh_gate[i], sh_up[i], sh_down[i])
        if need_ctx:
            ctx = ctx + cg1 * yc
            fc = modulate(rms_norm(ctx, norm2_g[i]), csh2, csc2)
            ctx = ctx + cg2 * moe_ffn(fc, router_w[i], router_b[i], exp_gate[i], exp_up[i], exp_down[i],
                                      sh_gate[i], sh_up[i], sh_down[i])
    return rms_norm(x, final_g)
```

```python
import math
import numpy as np
import concourse.bass as bass
import concourse.mybir as mybir
from concourse.bass_utils import run_bass_kernel_spmd

F32 = mybir.dt.float32
BF16 = mybir.dt.bfloat16
ALU = mybir.AluOpType
AF = mybir.ActivationFunctionType
AX = mybir.AxisListType

D = 4096
KC = 32
CTX = 256
HD = 128
NE = 64
FF = 192
G = 384
EPS = 1e-6
ENGS = ('pe', 'act', 'dve', 'pool', 'sp')
BNAME = {'pe': 'tensor', 'act': 'scalar', 'dve': 'vector', 'pool': 'gpsimd', 'sp': 'sync'}
DMAK = 6


class Op:
    __slots__ = ('eng', 'fn', 'deps', 'sig', 'val', 'sem', 'dma', 'prev')


class Glob:
    def __init__(self, nc, stack):
        self.nc = nc
        self.sem = {e: stack.enter_context(nc.semaphore("s_" + e)) for e in ENGS}
        self.cnt = {e: 0 for e in ENGS}
        self.dsem = {e: [stack.enter_context(nc.semaphore("d_%s%d" % (e, i))) for i in range(DMAK)]
                     for e in ('sp', 'pool')}
        self.dcnt = {e: [0] * DMAK for e in ('sp', 'pool')}
        self.di = {e: 0 for e in ('sp', 'pool')}
        self.dlast = {e: [None] * DMAK for e in ('sp', 'pool')}
        self.n_ins = 0


class Prog:
    def __init__(self, gl):
        self.gl = gl
        self.ops = {e: [] for e in ENGS}
        self.lastw = {}
        self.readers = {}
        self.nd = 0

    def add(self, eng, fn, reads=(), writes=(), dma=False):
        op = Op()
        op.eng = eng; op.fn = fn; op.sig = False; op.dma = dma; op.prev = None; op.sem = None; op.val = 0
        deps = {}
        for b in reads:
            w = self.lastw.get(b)
            if w is not None:
                deps[id(w)] = w
        for b in writes:
            w = self.lastw.get(b)
            if w is not None:
                deps[id(w)] = w
            rd = self.readers.get(b)
            if rd:
                for r in rd.values():
                    deps[id(r)] = r
        if dma:
            self.nd += 1
            rk = (eng, self.nd)
        else:
            rk = eng
        for b in reads:
            self.readers.setdefault(b, {})[rk] = op
        for b in writes:
            self.lastw[b] = op
            self.readers[b] = {}
        op.deps = [d for d in deps.values() if d is not op and not (d.eng == 'pe' and eng == 'pe')]
        self.ops[eng].append(op)
        return op

    def mm(self, out, lhsT, rhs, start, stop, reads, writes):
        return self.add('pe', lambda e: e.matmul(out, lhsT=lhsT, rhs=rhs, start=start, stop=stop), reads, writes)

    def load(self, out, in_, reads=(), writes=()):
        return self.add('sp', lambda e: e.dma_start(out=out, in_=in_), reads, writes, dma=True)

    def store(self, out, in_, reads=(), writes=()):
        return self.add('pool', lambda e: e.dma_start(out=out, in_=in_), reads, writes, dma=True)

    def emit(self):
        gl = self.gl
        nc = gl.nc
        bar = [(gl.sem[e], gl.cnt[e]) for e in ENGS if gl.cnt[e] > 0]
        for e in ('sp', 'pool'):
            for i in range(DMAK):
                if gl.dcnt[e][i] > 0:
                    bar.append((gl.dsem[e][i], gl.dcnt[e][i]))
        for e in ENGS:
            for op in self.ops[e]:
                for d in op.deps:
                    d.sig = True
            if self.ops[e]:
                last = [o for o in self.ops[e] if not o.dma]
                if last:
                    last[-1].sig = True
        for e in ENGS:
            for op in self.ops[e]:
                if op.dma:
                    i = gl.di[e] % DMAK
                    gl.di[e] += 1
                    gl.dcnt[e][i] += 16
                    op.sem = gl.dsem[e][i]; op.val = gl.dcnt[e][i]
                    op.prev = gl.dlast[e][i]
                    gl.dlast[e][i] = op
                elif op.sig:
                    gl.cnt[e] += 1
                    op.sem = gl.sem[e]; op.val = gl.cnt[e]
        with nc.Block() as block:
            for e in ENGS:
                ops = self.ops[e]

                def body(eng, e=e, ops=ops):
                    known = {}
                    for (s, v) in bar:
                        eng.wait_ge(s, v)
                        known[id(s)] = v
                    for op in ops:
                        waits = {}
                        for d in op.deps:
                            k = id(d.sem)
                            if waits.get(k, (None, 0))[1] < d.val:
                                waits[k] = (d.sem, d.val)
                        if op.dma and op.prev is not None:
                            k = id(op.prev.sem)
                            if waits.get(k, (None, 0))[1] < op.prev.val:
                                waits[k] = (op.prev.sem, op.prev.val)
                        for k, (s, v) in waits.items():
                            if known.get(k, 0) < v:
                                eng.wait_ge(s, v)
                                known[k] = v
                        ins = op.fn(eng)
                        gl.n_ins += 1
                        if op.dma:
                            ins.then_inc(op.sem, 16)
                        elif op.sig:
                            ins.then_inc(op.sem, 1)
                    if e in ('sp', 'pool'):
                        for i in range(DMAK):
                            if gl.dcnt[e][i] > 0:
                                eng.wait_ge(gl.dsem[e][i], gl.dcnt[e][i])
                getattr(block, BNAME[e])(body)


class Ctx:
    pass


_UC = [0]


def U(name):
    _UC[0] += 1
    return "%s_%d" % (name, _UC[0])


def row_of(cx, t128):
    tb = cx.TB // 128
    b, r = divmod(t128, tb)
    return b if r < cx.S // 128 else cx.B


def segs_of_group(cx, g):
    out = []
    for j in range(G // 128):
        r = row_of(cx, g * (G // 128) + j)
        if out and out[-1][2] == r:
            out[-1] = (out[-1][0], out[-1][1] + 128, r)
        else:
            out.append((j * 128, 128, r))
    return out


def phase_convert(cx, jobs):
    nc = cx.nc
    p = Prog(cx.gl)
    NS = 3
    with nc.sbuf_tensor(U("cv_s"), [128, NS, 8192], F32) as st, nc.sbuf_tensor(U("cv_b"), [128, NS, 8192], BF16) as bt:
        i = 0
        for (src, dst, P, A, Bc) in jobs:
            astep = max(1, 8192 // Bc)
            for a0 in range(0, A, astep):
                a1 = min(A, a0 + astep)
                n = (a1 - a0) * Bc
                s = i % NS
                sv = st[:P, s, 0:n].rearrange("p (a b) -> p a b", b=Bc)
                p.load(sv, src[:, a0:a1, :], writes=[('cs', s)])
                if i % 2 == 0:
                    p.add('dve', lambda e, o=bt[:P, s, 0:n], x=st[:P, s, 0:n]: e.tensor_copy(out=o, in_=x),
                          reads=[('cs', s)], writes=[('cb', s)])
                else:
                    p.add('act', lambda e, o=bt[:P, s, 0:n], x=st[:P, s, 0:n]: e.activation(out=o, in_=x, func=AF.Copy),
                          reads=[('cs', s)], writes=[('cb', s)])
                p.store(dst[:, a0 * Bc:a1 * Bc], bt[:P, s, 0:n], reads=[('cb', s)])
                i += 1
        p.emit()


def phase_mod(cx, l):
    nc = cx.nc
    p = Prog(cx.gl)
    R = cx.B + 1
    with (nc.sbuf_tensor(U("md_src"), [128, KC, R], F32) as src, nc.sbuf_tensor(U("md_srcb"), [128, KC, R], BF16) as srcb,
          nc.sbuf_tensor(U("md_sg"), [128, KC, R], F32) as sg,
          nc.sbuf_tensor(U("md_wd"), [128, KC * 512], BF16) as wd, nc.sbuf_tensor(U("md_h"), [128, 4, R], BF16) as h1,
          nc.sbuf_tensor(U("md_wu"), [128, 2, 4 * 2048], BF16) as wu, nc.sbuf_tensor(U("md_ab"), [128, 192], F32) as ab,
          nc.sbuf_tensor(U("md_raw"), [128, 192, R], F32) as raw, nc.sbuf_tensor(U("md_g"), [128, 2, KC], F32) as ng,
          nc.psum_tensor(U("md_ps"), [128, 4, 512], F32) as ps):
        p.load(src[:], cx.csT.rearrange("(c p) r -> p c r", p=128), writes=['src'])
        p.load(wd[:], cx.img['ada_down'][l, 0], writes=['wd'])
        p.load(ab[:], cx.ada_bT[l], writes=['ab'])
        p.load(ng[:, 0, :], cx.n1gT[l], writes=['ng'])
        p.load(ng[:, 1, :], cx.n2gT[l], writes=['ng'])
        p.add('act', lambda e: e.activation(out=sg[:], in_=src[:], func=AF.Sigmoid), reads=['src'], writes=['sg'])
        p.add('dve', lambda e: e.tensor_tensor(out=srcb[:], in0=src[:], in1=sg[:], op=ALU.mult), reads=['src', 'sg'], writes=['srcb'])
        for m in range(4):
            for k in range(KC):
                p.mm(ps[:, m, 0:R], wd[:, k * 512 + m * 128: k * 512 + (m + 1) * 128], srcb[:, k, :], k == 0, k == KC - 1,
                     reads=['wd', 'srcb'], writes=[('ps', m)])
            p.add('dve', lambda e, m=m: e.tensor_copy(out=h1[:, m, :], in_=ps[:, m, 0:R]), reads=[('ps', m)], writes=['h1'])
        for s in range(12):
            sl = s % 2
            p.load(wu[:, sl, :], cx.img['ada_up'][l, s], writes=[('wu', sl)])
            for mm_ in range(16):
                m = s * 16 + mm_
                pb = mm_ % 4
                for k in range(4):
                    p.mm(ps[:, pb, 0:R], wu[:, sl, k * 2048 + mm_ * 128: k * 2048 + (mm_ + 1) * 128], h1[:, k, :], k == 0, k == 3,
                         reads=[('wu', sl), 'h1'], writes=[('ps', pb)])
                p.add('dve', lambda e, m=m, pb=pb: e.tensor_scalar(out=raw[:, m, :], in0=ps[:, pb, 0:R], scalar1=ab[:, m:m + 1],
                                                                   scalar2=None, op0=ALU.add),
                      reads=[('ps', pb), 'ab'], writes=['raw'])
        mv = cx.modv
        for which, (ish, isc, ig) in enumerate(((0, 1, 2), (3, 4, 5))):
            o = which * 3
            p.add('dve', lambda e, o=o, isc=isc: e.tensor_scalar(out=mv[:, o, :, :], in0=raw[:, isc * 32:(isc + 1) * 32, :], scalar1=1.0,
                                                                   scalar2=None, op0=ALU.add), reads=['raw'], writes=['mv'])
            p.add('dve', lambda e, o=o, which=which: e.tensor_tensor(out=mv[:, o, :, :], in0=mv[:, o, :, :],
                                                                     in1=ng[:, which, :].unsqueeze(2).to_broadcast([128, KC, R]), op=ALU.mult),
                  reads=['mv', 'ng'], writes=['mv'])
            p.add('dve', lambda e, o=o, ish=ish: e.tensor_copy(out=mv[:, o + 1, :, :], in_=raw[:, ish * 32:(ish + 1) * 32, :]), reads=['raw'], writes=['mv'])
            p.add('dve', lambda e, o=o, ig=ig: e.tensor_copy(out=mv[:, o + 2, :, :], in_=raw[:, ig * 32:(ig + 1) * 32, :]), reads=['raw'], writes=['mv'])
        p.emit()


def phase_norm(cx, l, which, final=False):
    nc = cx.nc
    p = Prog(cx.gl)
    mv = cx.modv
    NGR = cx.NT // G
    router = (which == 2 and not final)
    with (nc.sbuf_tensor(U("nm_x"), [128, 2, KC, G], F32) as xg, nc.sbuf_tensor(U("nm_a"), [128, 2, KC, G], BF16) as ag,
          nc.sbuf_tensor(U("nm_sq"), [128, 2, G], F32) as sq, nc.sbuf_tensor(U("nm_r"), [128, G], F32) as rs,
          nc.sbuf_tensor(U("nm_fg"), [128, KC], F32) as fg,
          nc.sbuf_tensor(U("nm_rw"), [128, KC, NE], F32) as rw, nc.sbuf_tensor(U("nm_rb"), [128, NE], F32) as rb,
          nc.sbuf_tensor(U("nm_t"), [128, 12, NE], F32) as tt, nc.sbuf_tensor(U("nm_s"), [128, 8, 8], F32) as sm,
          nc.sbuf_tensor(U("nm_cm"), [128, 8, 8], F32) as cmp8,
          nc.sbuf_tensor(U("nm_gt"), [64, 2, G], BF16) as gts,
          nc.psum_tensor(U("nm_ps"), [128, 512], F32) as ps, nc.psum_tensor(U("nm_pl"), [128, 2, 512], F32) as pl,
          nc.psum_tensor(U("nm_pt"), [128, 2, 512], F32) as pt):
        if final:
            p.load(fg[:], cx.fgT, writes=['fg'])
        if router:
            p.load(rw[:], cx.router_w[l].rearrange("(c p) e -> p c e", p=128), writes=['rw'])
            p.load(rb[:], cx.router_b[l:l + 1, :].to_broadcast((128, NE)), writes=['rb'])
        for g in range(NGR):
            s = g % 2
            X = ('x', s)
            p.load(xg[:, s], cx.xT[:, g * G:(g + 1) * G].rearrange("(c p) t -> p c t", p=128), writes=[X])
            for c in range(KC):
                q = c % 2
                p.add('act', lambda e, s=s, c=c, q=q: e.activation(out=sq[:, q, :], in_=xg[:, s, c, :], func=AF.Square),
                      reads=[X], writes=[('sq', q)])
                p.mm(ps[:, 0:G], cx.onesF[:], sq[:, q, :], c == 0, c == KC - 1, reads=[('sq', q)], writes=['ps'])
            p.add('dve', lambda e: e.tensor_scalar(out=rs[:], in0=ps[:, 0:G], scalar1=1.0 / D, scalar2=EPS, op0=ALU.mult, op1=ALU.add),
                  reads=['ps'], writes=['rs'])
            p.add('act', lambda e: e.activation(out=rs[:], in_=rs[:], func=AF.Sqrt), reads=['rs'], writes=['rs'])
            p.add('dve', lambda e: e.reciprocal(out=rs[:], in_=rs[:]), reads=['rs'], writes=['rs'])
            h = KC // 2
            p.add('dve', lambda e, s=s: e.tensor_tensor(out=xg[:, s, 0:h], in0=xg[:, s, 0:h], in1=rs[:].unsqueeze(1).to_broadcast([128, h, G]), op=ALU.mult),
                  reads=[X, 'rs'], writes=[X])
            p.add('pool', lambda e, s=s: e.tensor_tensor(out=xg[:, s, h:KC], in0=xg[:, s, h:KC], in1=rs[:].unsqueeze(1).to_broadcast([128, h, G]), op=ALU.mult),
                  reads=[X, 'rs'], writes=[X])
            A = ('a', s)
            if final:
                for c in range(KC):
                    eng = 'act' if c % 2 == 0 else 'dve'
                    if eng == 'act':
                        p.add('act', lambda e, s=s, c=c: e.activation(out=xg[:, s, c, :], in_=xg[:, s, c, :], func=AF.Copy, scale=fg[:, c:c + 1]),
                              reads=[X, 'fg'], writes=[X])
                    else:
                        p.add('dve', lambda e, s=s, c=c: e.tensor_scalar(out=xg[:, s, c, :], in0=xg[:, s, c, :], scalar1=fg[:, c:c + 1], scalar2=None, op0=ALU.mult),
                              reads=[X, 'fg'], writes=[X])
                p.store(cx.outT[:, g * G:(g + 1) * G].rearrange("(c p) t -> p c t", p=128), xg[:, s], reads=[X])
                continue
            o = (which - 1) * 3
            for (c0, ncol, r) in segs_of_group(cx, g):
                for c in range(KC):
                    dst = xg[:, s, c, c0:c0 + ncol] if router else ag[:, s, c, c0:c0 + ncol]
                    wr = [X] if router else [A]
                    if c % 2 == 0:
                        p.add('act', lambda e, dst=dst, s=s, c=c, c0=c0, ncol=ncol, r=r: e.activation(
                            out=dst, in_=xg[:, s, c, c0:c0 + ncol], func=AF.Identity, scale=mv[:, o, c, r:r + 1], bias=mv[:, o + 1, c, r:r + 1]),
                            reads=[X, 'mv'], writes=wr)
                    else:
                        p.add('dve', lambda e, dst=dst, s=s, c=c, c0=c0, ncol=ncol, r=r: e.tensor_scalar(
                            out=dst, in0=xg[:, s, c, c0:c0 + ncol], scalar1=mv[:, o, c, r:r + 1], scalar2=mv[:, o + 1, c, r:r + 1],
                            op0=ALU.mult, op1=ALU.add), reads=[X, 'mv'], writes=wr)
            if router:
                p.add('pool', lambda e, s=s: e.tensor_copy(out=ag[:, s], in_=xg[:, s]), reads=[X], writes=[A])
                GT = ('gt', s)
                for j in range(G // 128):
                    pj = j % 2
                    for c in range(KC):
                        p.mm(pl[:, pj, 0:NE], xg[:, s, c, j * 128:(j + 1) * 128], rw[:, c, :], c == 0, c == KC - 1,
                             reads=[X, 'rw'], writes=[('pl', pj)])
                    sc, bi, t2, msk, w, sel = (tt[:, i, :] for i in range(6))
                    v3 = lambda a: a.rearrange("p (g k) -> p g k", k=8)
                    m1, m2, gs, gm, pen, cnt = (sm[:, i, :] for i in range(6))
                    top8 = sm[:, 6, :]
                    thr = sm[:, 7, 0:1]
                    wsum = sm[:, 7, 1:2]
                    T = 'tt'
                    p.add('act', lambda e, pj=pj, sc=sc: e.activation(out=sc, in_=pl[:, pj, 0:NE], func=AF.Sigmoid), reads=[('pl', pj)], writes=[T])
                    dv = lambda fn: p.add('dve', fn, reads=[T, 'rb'], writes=[T])
                    dv(lambda e, bi=bi, sc=sc: e.tensor_tensor(out=bi, in0=sc, in1=rb[:], op=ALU.add))
                    dv(lambda e, m1=m1, bi=bi: e.tensor_reduce(out=m1, in_=v3(bi), axis=AX.X, op=ALU.max))
                    dv(lambda e, t2=t2, bi=bi, m1=m1: e.tensor_tensor(out=v3(t2), in0=v3(bi), in1=m1.unsqueeze(2).to_broadcast([128, 8, 8]), op=ALU.is_equal))
                    dv(lambda e, t2=t2, bi=bi: e.scalar_tensor_tensor(out=t2, in0=t2, scalar=-1e30, in1=bi, op0=ALU.mult, op1=ALU.add))
                    dv(lambda e, m2=m2, t2=t2: e.tensor_reduce(out=m2, in_=v3(t2), axis=AX.X, op=ALU.max))
                    dv(lambda e, gs=gs, m1=m1, m2=m2: e.tensor_tensor(out=gs, in0=m1, in1=m2, op=ALU.add))
                    dv(lambda e, gs=gs: e.tensor_tensor(out=cmp8[:], in0=gs.unsqueeze(1).to_broadcast([128, 8, 8]),
                                                        in1=gs.unsqueeze(2).to_broadcast([128, 8, 8]), op=ALU.is_gt))
                    dv(lambda e, cnt=cnt: e.tensor_reduce(out=cnt, in_=cmp8[:], axis=AX.X, op=ALU.add))
                    dv(lambda e, pen=pen, cnt=cnt: e.tensor_scalar(out=pen, in0=cnt, scalar1=3.5, scalar2=-1e30, op0=ALU.is_gt, op1=ALU.mult))
                    dv(lambda e, msk=msk, bi=bi, pen=pen: e.tensor_tensor(out=v3(msk), in0=v3(bi), in1=pen.unsqueeze(2).to_broadcast([128, 8, 8]), op=ALU.add))
                    dv(lambda e, top8=top8, msk=msk: e.max(out=top8, in_=msk))
                    dv(lambda e, thr=thr, top8=top8: e.tensor_reduce(out=thr, in_=top8, axis=AX.X, op=ALU.min))
                    dv(lambda e, sel=sel, msk=msk, thr=thr: e.tensor_scalar(out=sel, in0=msk, scalar1=thr, scalar2=None, op0=ALU.is_ge))
                    dv(lambda e, w=w, sc=sc, sel=sel: e.tensor_tensor(out=w, in0=sc, in1=sel, op=ALU.mult))
                    dv(lambda e, wsum=wsum, w=w: e.tensor_reduce(out=wsum, in_=w, axis=AX.X, op=ALU.add))
                    dv(lambda e, wsum=wsum: e.reciprocal(out=wsum, in_=wsum))
                    dv(lambda e, w=w, wsum=wsum: e.tensor_scalar(out=w, in0=w, scalar1=wsum, scalar2=2.5, op0=ALU.mult, op1=ALU.mult))
                    p.add('pe', lambda e, pj=pj, w=w: e.transpose(pt[0:NE, pj, 0:128], w, cx.ident[:]), reads=[T], writes=[('pt', pj)])
                    p.add('act', lambda e, pj=pj, s=s, j=j: e.activation(out=gts[:, s, j * 128:(j + 1) * 128], in_=pt[0:NE, pj, 0:128], func=AF.Copy),
                          reads=[('pt', pj)], writes=[GT])
                p.store(cx.gT[:, g * G:(g + 1) * G], gts[:, s, :], reads=[GT])
            p.store(cx.aT[:, g * G:(g + 1) * G].rearrange("(c p) t -> p c t", p=128), ag[:, s], reads=[A])
        p.emit()


def phase_proj(cx, inT, wimg, nslab, evac_T=None, evac_N=None, modes=None, kc=KC):
    nc = cx.nc
    p = Prog(cx.gl)
    GB = 3
    TBK = GB * G
    nblk = cx.NT // TBK
    assert cx.NT % TBK == 0
    with (nc.sbuf_tensor(U("pj_a"), [128, kc, TBK], BF16) as at, nc.sbuf_tensor(U("pj_w"), [128, 2, kc * 512], BF16) as wt,
          nc.psum_tensor(U("pj_ps"), [128, 4, 512], F32) as ps):
        cx.pj_extra(p)
        it = 0
        pi = 0
        for blk in range(nblk):
            p.load(at[:], inT[:, blk * TBK:(blk + 1) * TBK].rearrange("(c p) t -> p c t", p=128), writes=['at'])
            for s in range(nslab):
                sl = it % 2
                it += 1
                p.load(wt[:, sl, :], wimg[s], writes=[('w', sl)])
                mode = modes[s] if modes else 'T'
                if mode == 'T':
                    for mi in range(4):
                        for gi in range(GB):
                            pb = pi % 4
                            pi += 1
                            for k in range(kc):
                                p.mm(ps[:, pb, 0:G], wt[:, sl, k * 512 + mi * 128:k * 512 + (mi + 1) * 128], at[:, k, gi * G:(gi + 1) * G],
                                     k == 0, k == kc - 1, reads=[('w', sl), 'at'], writes=[('ps', pb)])
                            evac_T(p, ps[:, pb, 0:G], ('ps', pb), s * 4 + mi, blk * GB + gi)
                else:
                    for ti in range(TBK // 128):
                        pb = pi % 4
                        pi += 1
                        for k in range(kc):
                            p.mm(ps[:, pb, :], at[:, k, ti * 128:(ti + 1) * 128], wt[:, sl, k * 512:(k + 1) * 512],
                                 k == 0, k == kc - 1, reads=[('w', sl), 'at'], writes=[('ps', pb)])
                        evac_N(p, ps[:, pb, :], ('ps', pb), s, blk * (TBK // 128) + ti)
        p.emit()


def make_store_T(cx, stage, outT, nslot=4):
    st = {'i': 0}

    def ev(p, ps_ap, pskey, m, g):
        s = st['i'] % nslot
        st['i'] += 1
        key = ('stg', s)
        if st['i'] % 2 == 0:
            p.add('act', lambda e: e.activation(out=stage[:, s, 0:G], in_=ps_ap, func=AF.Copy), reads=[pskey], writes=[key])
        else:
            p.add('dve', lambda e: e.tensor_copy(out=stage[:, s, 0:G], in_=ps_ap), reads=[pskey], writes=[key])
        p.store(outT[m * 128:(m + 1) * 128, g * G:(g + 1) * G], stage[:, s, 0:G], reads=[key])
    return ev


def make_store_N(cx, stage, outN, col_of_slab, nslot=4):
    st = {'i': 0}

    def ev(p, ps_ap, pskey, s_, t):
        s = st['i'] % nslot
        st['i'] += 1
        key = ('stg', s)
        if st['i'] % 2 == 0:
            p.add('act', lambda e: e.activation(out=stage[:, s, :], in_=ps_ap, func=AF.Copy), reads=[pskey], writes=[key])
        else:
            p.add('dve', lambda e: e.tensor_copy(out=stage[:, s, :], in_=ps_ap), reads=[pskey], writes=[key])
        c0 = col_of_slab(s_)
        p.store(outN[t * 128:(t + 1) * 128, c0:c0 + 512], stage[:, s, :], reads=[key])
    return ev


def make_resid_T(cx, xst, gate_idx):
    st = {'i': 0}
    mv = cx.modv

    def ev(p, ps_ap, pskey, m, g):
        s = st['i'] % 3
        st['i'] += 1
        key = ('xs', s)
        xa = cx.xT[m * 128:(m + 1) * 128, g * G:(g + 1) * G]
        p.load(xst[:, s, :], xa, writes=[key])
        for (c0, ncol, r) in segs_of_group(cx, g):
            p.add('dve', lambda e, c0=c0, ncol=ncol, r=r: e.scalar_tensor_tensor(
                out=xst[:, s, c0:c0 + ncol], in0=ps_ap[:, c0:c0 + ncol], scalar=mv[:, gate_idx, m, r:r + 1], in1=xst[:, s, c0:c0 + ncol],
                op0=ALU.mult, op1=ALU.add), reads=[pskey, key, 'mv'], writes=[key])
        p.store(xa, xst[:, s, :], reads=[key])
    return ev


def run_proj_store(cx, inT, wimg, nslab, outT=None, outN=None, modes=None, col_of_slab=None, resid_gate=None):
    nc = cx.nc
    with (nc.sbuf_tensor(U("pj_st"), [128, 4, 512], BF16) as stage, nc.sbuf_tensor(U("pj_xs"), [128, 3, G], F32) as xst):
        cx.pj_extra = lambda p: None
        evT = None
        if resid_gate is not None:
            evT = make_resid_T(cx, xst, resid_gate)
        elif outT is not None:
            evT = make_store_T(cx, stage, outT)
        evN = make_store_N(cx, stage, outN, col_of_slab) if outN is not None else None
        phase_proj(cx, inT, wimg, nslab, evac_T=evT, evac_N=evN, modes=modes)


def rope_load(cx, p, dst, swb, swp, tmp, key, src_rows, t0, n, pos0):
    p.load(dst, src_rows[:, t0:t0 + n], writes=[key])
    p.load(swb[0:64, 0:n], src_rows[64:128, t0:t0 + n], writes=[(key, 'swb')])
    p.load(swb[64:128, 0:n], src_rows[0:64, t0:t0 + n], writes=[(key, 'swb')])
    p.add('pool', lambda e: e.tensor_tensor(out=tmp[:, 0:n], in0=swb[:, 0:n], in1=cx.sin2[:, pos0:pos0 + n], op=ALU.mult),
          reads=[(key, 'swb')], writes=[(key, 'tmp')])
    p.add('dve', lambda e: e.tensor_tensor(out=swp[:, 0:n], in0=dst, in1=cx.cos2[:, pos0:pos0 + n], op=ALU.mult),
          reads=[key], writes=[(key, 'sw')])
    p.add('dve', lambda e: e.tensor_tensor(out=dst, in0=swp[:, 0:n], in1=tmp[:, 0:n], op=ALU.add),
          reads=[(key, 'sw'), (key, 'tmp')], writes=[key])


def phase_attn_a(cx, j):
    nc = cx.nc
    p = Prog(cx.gl)
    S, C, TB = cx.S, cx.C, cx.TB
    NQB = S // 128
    SC = HD ** -0.5
    with (nc.sbuf_tensor(U("aa_k"), [128, 2, TB], BF16) as kt, nc.sbuf_tensor(U("aa_q"), [128, 2, TB], BF16) as qt,
          nc.sbuf_tensor(U("aa_sw"), [128, 2, S], F32) as swp, nc.sbuf_tensor(U("aa_tm"), [128, 2, S], F32) as tmp, nc.sbuf_tensor(U("aa_swb"), [128, 2, S], BF16) as swb,
          nc.sbuf_tensor(U("aa_v"), [128, 2, TB // 128, 128], BF16) as vt,
          nc.sbuf_tensor(U("aa_p"), [128, 2, NQB + 2, 512], BF16) as pt_, nc.sbuf_tensor(U("aa_pc"), [128, 2, 2, S + C], BF16) as pc,
          nc.sbuf_tensor(U("aa_o"), [128, 2, S + C], BF16) as ot, nc.sbuf_tensor(U("aa_r"), [128, 2, 512], F32) as rc,
          nc.sbuf_tensor(U("aa_es"), [128, 32], F32) as es,
          nc.psum_tensor(U("aa_s"), [128, 3, 512], F32) as pss, nc.psum_tensor(U("aa_po"), [128, 2, 512], F32) as pso,
          nc.psum_tensor(U("aa_pz"), [128, 2, 512], F32) as psz):
        p.load(es[:], cx.a_sink[j:j + 1, :].to_broadcast((128, 32)), writes=['es'])
        p.add('act', lambda e: e.activation(out=es[:], in_=es[:], func=AF.Exp), reads=['es'], writes=['es'])
        si = 0
        oi = 0
        hi = 0
        for b in range(cx.B):
            t0 = b * TB
            for kv in range(8):
                ks = (b * 8 + kv) % 2
                K = ('k', ks)
                krows = cx.qkvT[4096 + kv * 128:4096 + (kv + 1) * 128, :]
                rope_load(cx, p, kt[:, ks, 0:S], swb[:, 0, :], swp[:, 0, :], tmp[:, 0, :], K, krows, t0, S, 0)
                p.load(kt[:, ks, S:TB], krows[:, t0 + S:t0 + TB], writes=[K])
                V = ('v', ks)
                p.load(vt[:, ks], cx.vN[t0:t0 + TB, kv * 128:(kv + 1) * 128].rearrange("(n p) d -> p n d", p=128), writes=[V])
                for gq in range(4):
                    h = kv * 4 + gq
                    qs = hi % 2
                    hi += 1
                    Q = ('q', qs)
                    qrows = cx.qkvT[h * 128:(h + 1) * 128, :]
                    rope_load(cx, p, qt[:, qs, 0:S], swb[:, 1, :], swp[:, 1, :], tmp[:, 1, :], Q, qrows, t0, S, 0)
                    p.load(qt[:, qs, S:TB], qrows[:, t0 + S:t0 + TB], writes=[Q])
                    P = ('p', qs)
                    PC = ('pc', qs)
                    for kb in range(NQB):
                        q0 = max(0, kb - 1)
                        q1 = min(NQB, kb + 2)
                        n = (q1 - q0) * 128
                        sb = si % 3
                        si += 1
                        p.mm(pss[:, sb, 0:n], kt[:, ks, kb * 128:(kb + 1) * 128], qt[:, qs, q0 * 128:q1 * 128], True, True,
                             reads=[K, Q], writes=[('pss', sb)])
                        p.add('act', lambda e, sb=sb, n=n, qs=qs, kb=kb: e.activation(out=pt_[:, qs, kb, 0:n], in_=pss[:, sb, 0:n], func=AF.Exp, scale=SC),
                              reads=[('pss', sb)], writes=[P])
                        m0 = (q0 - (kb - 1)) * 128
                        p.add('pool', lambda e, n=n, qs=qs, kb=kb, m0=m0: e.tensor_tensor(out=pt_[:, qs, kb, 0:n], in0=pt_[:, qs, kb, 0:n],
                                                                                         in1=cx.wmask[:, m0:m0 + n], op=ALU.mult),
                              reads=[P], writes=[P])
                    for cb in range(C // 128):
                        for q0 in range(0, S + C, 512):
                            n = min(512, S + C - q0)
                            sb = si % 3
                            si += 1
                            p.mm(pss[:, sb, 0:n], kt[:, ks, S + cb * 128:S + (cb + 1) * 128], qt[:, qs, q0:q0 + n], True, True,
                                 reads=[K, Q], writes=[('pss', sb)])
                            p.add('act', lambda e, sb=sb, n=n, qs=qs, cb=cb, q0=q0: e.activation(out=pc[:, qs, cb, q0:q0 + n], in_=pss[:, sb, 0:n], func=AF.Exp, scale=SC),
                                  reads=[('pss', sb)], writes=[PC])
                    O = ('o', qs)
                    nqb_all = (S + C) // 128
                    for qg in range(0, nqb_all, 4):
                        ob = oi % 2
                        oi += 1
                        nq = min(4, nqb_all - qg)
                        for qi in range(nq):
                            qb = qg + qi
                            terms = []
                            if qb < NQB:
                                for kb in (qb - 1, qb, qb + 1):
                                    if 0 <= kb < NQB:
                                        q0 = max(0, kb - 1)
                                        off = (qb - q0) * 128
                                        terms.append((vt[:, ks, kb, :], pt_[:, qs, kb, off:off + 128]))
                            for cb in range(C // 128):
                                terms.append((vt[:, ks, NQB + cb, :], pc[:, qs, cb, qb * 128:(qb + 1) * 128]))
                            for ti, (vv, pp) in enumerate(terms):
                                p.mm(pso[:, ob, qi * 128:(qi + 1) * 128], vv, pp, ti == 0, ti == len(terms) - 1,
                                     reads=[V, P, PC], writes=[('pso', ob)])
                            for ti, (vv, pp) in enumerate(terms):
                                p.mm(psz[:, ob, qi * 128:(qi + 1) * 128], cx.onesB[:], pp, ti == 0, ti == len(terms) - 1,
                                     reads=[P, PC], writes=[('psz', ob)])
                        n = nq * 128
                        R = ('rc', ob)
                        p.add('dve', lambda e, ob=ob, n=n, h=h: e.tensor_scalar(out=rc[:, ob, 0:n], in0=psz[:, ob, 0:n], scalar1=es[:, h:h + 1], scalar2=None, op0=ALU.add),
                              reads=[('psz', ob), 'es'], writes=[R])
                        p.add('dve', lambda e, ob=ob, n=n: e.reciprocal(out=rc[:, ob, 0:n], in_=rc[:, ob, 0:n]), reads=[R], writes=[R])
                        p.add('dve', lambda e, ob=ob, n=n, qs=qs, qg=qg: e.tensor_tensor(out=ot[:, qs, qg * 128:qg * 128 + n], in0=pso[:, ob, 0:n], in1=rc[:, ob, 0:n], op=ALU.mult),
                              reads=[('pso', ob), R], writes=[O])
                    p.store(cx.oT[h * 128:(h + 1) * 128, t0:t0 + TB], ot[:, qs, :], reads=[O])
        p.emit()


def phase_attn_b(cx, l):
    nc = cx.nc
    p = Prog(cx.gl)
    S, C, TB = cx.S, cx.C, cx.TB
    NKB = TB // 128
    SC = HD ** -0.5
    lam_init = 0.8 - 0.6 * math.exp(-0.3 * l)
    from contextlib import ExitStack
    with ExitStack() as es_:
        kt = es_.enter_context(nc.sbuf_tensor(U("ab_k"), [128, 2, 2, TB], BF16))
        qt = es_.enter_context(nc.sbuf_tensor(U("ab_q"), [128, 2, 2, TB], BF16))
        swp = es_.enter_context(nc.sbuf_tensor(U("ab_sw"), [128, 2, S], F32))
        tmp = es_.enter_context(nc.sbuf_tensor(U("ab_tm"), [128, 2, S], F32))
        swb = es_.enter_context(nc.sbuf_tensor(U("ab_swb"), [128, 2, S], BF16))
        vt = es_.enter_context(nc.sbuf_tensor(U("ab_v"), [128, 2, NKB, 256], BF16))
        pt_ = es_.enter_context(nc.sbuf_tensor(U("ab_p"), [128, 4, 512], BF16))
        o32 = es_.enter_context(nc.sbuf_tensor(U("ab_o"), [128, 2, 2, 512], F32))
        ob16 = es_.enter_context(nc.sbuf_tensor(U("ab_ob"), [128, 2, 2, 512], BF16))
        rc = es_.enter_context(nc.sbuf_tensor(U("ab_r"), [128, 2, 512], F32))
        sq = es_.enter_context(nc.sbuf_tensor(U("ab_sq"), [128, 2, 512], F32))
        lm = es_.enter_context(nc.sbuf_tensor(U("ab_l"), [128, 8], F32))
        sg = es_.enter_context(nc.sbuf_tensor(U("ab_sg"), [128, 2], F32))
        pss = es_.enter_context(nc.psum_tensor(U("ab_s"), [128, 2, 512], F32))
        pso = es_.enter_context(nc.psum_tensor(U("ab_po"), [128, 4, 512], F32))
        psz = es_.enter_context(nc.psum_tensor(U("ab_pz"), [128, 2, 512], F32))
        p.load(lm[:, 0:4], cx.b_lamT, writes=['lm'])
        p.load(sg[:], cx.b_sgT, writes=['sg'])
        p.add('dve', lambda e: e.tensor_tensor(out=lm[:, 4:5], in0=lm[:, 0:1], in1=lm[:, 1:2], op=ALU.mult), reads=['lm'], writes=['lm'])
        p.add('dve', lambda e: e.tensor_tensor(out=lm[:, 5:6], in0=lm[:, 2:3], in1=lm[:, 3:4], op=ALU.mult), reads=['lm'], writes=['lm'])
        p.mm(psz[:, 0, 0:2], cx.onesF[:], lm[:, 4:6], True, True, reads=['lm'], writes=[('psz', 0)])
        p.add('act', lambda e: e.activation(out=lm[:, 6:8], in_=psz[:, 0, 0:2], func=AF.Exp), reads=[('psz', 0)], writes=['lm'])
        p.add('dve', lambda e: e.tensor_tensor(out=lm[:, 6:7], in0=lm[:, 7:8], in1=lm[:, 6:7], op=ALU.subtract), reads=['lm'], writes=['lm'])
        p.add('dve', lambda e: e.tensor_scalar(out=lm[:, 6:7], in0=lm[:, 6:7], scalar1=-lam_init, scalar2=None, op0=ALU.add), reads=['lm'], writes=['lm'])
        p.add('dve', lambda e: e.tensor_scalar(out=sg[:], in0=sg[:], scalar1=1.0 - lam_init, scalar2=None, op0=ALU.mult), reads=['sg'], writes=['sg'])
        si = 0
        pi = 0
        for b in range(cx.B):
            t0 = b * TB
            for h in range(16):
                ks = (b * 16 + h) % 2
                K = ('k', ks)
                Q = ('q', ks)
                V = ('v', ks)
                for c in range(2):
                    krows = cx.qkvT[4096 + (h * 2 + c) * 128:4096 + (h * 2 + c + 1) * 128, :]
                    rope_load(cx, p, kt[:, ks, c, 0:S], swb[:, 0, :], swp[:, 0, :], tmp[:, 0, :], (K, c), krows, t0, S, 0)
                    p.load(kt[:, ks, c, S:TB], krows[:, t0 + S:t0 + TB], writes=[(K, c)])
                    qrows = cx.qkvT[(h * 2 + c) * 128:(h * 2 + c + 1) * 128, :]
                    rope_load(cx, p, qt[:, ks, c, 0:S], swb[:, 1, :], swp[:, 1, :], tmp[:, 1, :], (Q, c), qrows, t0, S, 0)
                    p.load(qt[:, ks, c, S:TB], qrows[:, t0 + S:t0 + TB], writes=[(Q, c)])
                p.load(vt[:, ks], cx.vN[t0:t0 + TB, h * 256:(h + 1) * 256].rearrange("(n p) d -> p n d", p=128), writes=[V])
                chunks = [(q0, 512, 0, NKB) for q0 in range(0, S, 512)] + [(S, C, S // 128, NKB)]
                for (q0, n, kb0, kb1) in chunks:
                    nk = kb1 - kb0
                    for ki, kb in enumerate(range(kb0, kb1)):
                        for c in range(2):
                            sb = si % 2
                            si += 1
                            ps_ = pi % 4
                            pi += 1
                            p.mm(pss[:, sb, 0:n], kt[:, ks, c, kb * 128:(kb + 1) * 128], qt[:, ks, c, q0:q0 + n], True, True,
                                 reads=[(K, c), (Q, c)], writes=[('pss', sb)])
                            p.add('act', lambda e, sb=sb, n=n, ps_=ps_: e.activation(out=pt_[:, ps_, 0:n], in_=pss[:, sb, 0:n], func=AF.Exp, scale=SC),
                                  reads=[('pss', sb)], writes=[('p', ps_)])
                            for et in range(2):
                                p.mm(pso[:, c * 2 + et, 0:n], vt[:, ks, kb, et * 128:(et + 1) * 128], pt_[:, ps_, 0:n], ki == 0, ki == nk - 1,
                                     reads=[V, ('p', ps_)], writes=[('pso', c * 2 + et)])
                            p.mm(psz[:, c, 0:n], cx.onesB[:], pt_[:, ps_, 0:n], ki == 0, ki == nk - 1,
                                 reads=[('p', ps_)], writes=[('psz', c)])
                    for c in range(2):
                        p.add('dve', lambda e, c=c, n=n: e.reciprocal(out=rc[:, c, 0:n], in_=psz[:, c, 0:n]), reads=[('psz', c)], writes=[('rc', c)])
                    p.add('dve', lambda e, n=n: e.tensor_scalar(out=rc[:, 1, 0:n], in0=rc[:, 1, 0:n], scalar1=lm[:, 6:7], scalar2=None, op0=ALU.mult),
                          reads=[('rc', 1), 'lm'], writes=[('rc', 1)])
                    for et in range(2):
                        p.add('dve', lambda e, et=et, n=n: e.tensor_tensor(out=o32[:, 0, et, 0:n], in0=pso[:, et, 0:n], in1=rc[:, 0, 0:n], op=ALU.mult),
                              reads=[('pso', et), ('rc', 0)], writes=[('o32', 0, et)])
                        p.add('dve', lambda e, et=et, n=n: e.tensor_tensor(out=o32[:, 1, et, 0:n], in0=pso[:, 2 + et, 0:n], in1=rc[:, 1, 0:n], op=ALU.mult),
                              reads=[('pso', 2 + et), ('rc', 1)], writes=[('o32', 1, et)])
                        p.add('pool', lambda e, et=et, n=n: e.tensor_tensor(out=o32[:, 0, et, 0:n], in0=o32[:, 0, et, 0:n], in1=o32[:, 1, et, 0:n], op=ALU.add),
                              reads=[('o32', 0, et), ('o32', 1, et)], writes=[('o32', 0, et)])
                        p.add('act', lambda e, et=et, n=n: e.activation(out=sq[:, et, 0:n], in_=o32[:, 0, et, 0:n], func=AF.Square),
                              reads=[('o32', 0, et)], writes=[('sq', et)])
                    sb = si % 2
                    si += 1
                    for et in range(2):
                        p.mm(pss[:, sb, 0:n], cx.onesF[:], sq[:, et, 0:n], et == 0, et == 1, reads=[('sq', et)], writes=[('pss', sb)])
                    p.add('dve', lambda e, sb=sb, n=n: e.tensor_scalar(out=rc[:, 0, 0:n], in0=pss[:, sb, 0:n], scalar1=1.0 / 256, scalar2=EPS, op0=ALU.mult, op1=ALU.add),
                          reads=[('pss', sb)], writes=[('rc', 0)])
                    p.add('act', lambda e, n=n: e.activation(out=rc[:, 0, 0:n], in_=rc[:, 0, 0:n], func=AF.Sqrt), reads=[('rc', 0)], writes=[('rc', 0)])
                    p.add('dve', lambda e, n=n: e.reciprocal(out=rc[:, 0, 0:n], in_=rc[:, 0, 0:n]), reads=[('rc', 0)], writes=[('rc', 0)])
                    osl = (q0 // 512) % 2
                    for et in range(2):
                        p.add('dve', lambda e, et=et, n=n, osl=osl: e.scalar_tensor_tensor(out=ob16[:, osl, et, 0:n], in0=o32[:, 0, et, 0:n], scalar=sg[:, et:et + 1],
                                                                                         in1=rc[:, 0, 0:n], op0=ALU.mult, op1=ALU.mult),
                              reads=[('o32', 0, et), ('rc', 0), 'sg'], writes=[('ob', osl, et)])
                        p.store(cx.oT[h * 256 + et * 128:h * 256 + (et + 1) * 128, t0 + q0:t0 + q0 + n], ob16[:, osl, et, 0:n], reads=[('ob', osl, et)])
        p.emit()


def phase_conv(cx):
    nc = cx.nc
    p = Prog(cx.gl)
    S, C, TB = cx.S, cx.C, cx.TB
    LM = S
    with (nc.sbuf_tensor(U("cv_in"), [128, 2, 3, LM], BF16) as xin, nc.sbuf_tensor(U("cv_v"), [128, 2, LM + 2], F32) as vv,
          nc.sbuf_tensor(U("cv_z"), [128, 2, LM], F32) as zz, nc.sbuf_tensor(U("cv_m"), [128, 2, LM], BF16) as mm_,
          nc.sbuf_tensor(U("cv_w"), [128, KC, 3], F32) as cw):
        p.load(cw[:], cx.c_convT, writes=['cw'])
        it = 0
        for c in range(KC):
            for b in range(cx.B):
                for (o0, L) in ((0, S), (S, C)):
                    s = it % 2
                    it += 1
                    t0 = b * TB + o0
                    I = ('in', s)
                    for w3 in range(3):
                        p.load(xin[:, s, w3, 0:L], cx.qkvT[w3 * 4096 + c * 128:w3 * 4096 + (c + 1) * 128, t0:t0 + L], writes=[I])
                    Vk = ('v', s)
                    p.add('pool', lambda e, s=s, L=L: e.memset(vv[:, s, 0:1], 0.0), writes=[Vk])
                    p.add('pool', lambda e, s=s, L=L: e.memset(vv[:, s, L + 1:L + 2], 0.0), writes=[Vk])
                    p.add('pool', lambda e, s=s, L=L: e.tensor_tensor(out=vv[:, s, 1:L + 1], in0=xin[:, s, 1, 0:L], in1=xin[:, s, 2, 0:L], op=ALU.mult),
                          reads=[I], writes=[Vk])
                    Z = ('z', s)
                    p.add('dve', lambda e, s=s, L=L, c=c: e.tensor_scalar(out=zz[:, s, 0:L], in0=vv[:, s, 0:L], scalar1=cw[:, c, 0:1], scalar2=None, op0=ALU.mult),
                          reads=[Vk, 'cw'], writes=[Z])
                    p.add('dve', lambda e, s=s, L=L, c=c: e.scalar_tensor_tensor(out=zz[:, s, 0:L], in0=vv[:, s, 1:L + 1], scalar=cw[:, c, 1:2], in1=zz[:, s, 0:L],
                                                                                 op0=ALU.mult, op1=ALU.add), reads=[Vk, 'cw', Z], writes=[Z])
                    p.add('dve', lambda e, s=s, L=L, c=c: e.scalar_tensor_tensor(out=zz[:, s, 0:L], in0=vv[:, s, 2:L + 2], scalar=cw[:, c, 2:3], in1=zz[:, s, 0:L],
                                                                                 op0=ALU.mult, op1=ALU.add), reads=[Vk, 'cw', Z], writes=[Z])
                    M = ('m', s)
                    p.add('pool', lambda e, s=s, L=L: e.tensor_tensor(out=mm_[:, s, 0:L], in0=zz[:, s, 0:L], in1=xin[:, s, 0, 0:L], op=ALU.mult),
                          reads=[Z, I], writes=[M])
                    p.store(cx.oT[c * 128:(c + 1) * 128, t0:t0 + L], mm_[:, s, 0:L], reads=[M])
        p.emit()


def phase_moe(cx, l):
    nc = cx.nc
    p = Prog(cx.gl)
    mv = cx.modv
    NGR = cx.NT // G
    with (nc.sbuf_tensor(U("mo_f"), [128, KC, G], BF16) as ft, nc.sbuf_tensor(U("mo_acc"), [128, KC, G], F32) as acc,
          nc.sbuf_tensor(U("mo_wg"), [128, 2, KC * FF], BF16) as wg, nc.sbuf_tensor(U("mo_wu"), [128, 2, KC * FF], BF16) as wu,
          nc.sbuf_tensor(U("mo_wd"), [128, 2, 2 * D], BF16) as wd, nc.sbuf_tensor(U("mo_gt"), [64, G], BF16) as gt,
          nc.sbuf_tensor(U("mo_sel"), [64, NE, 128], BF16) as sel, nc.sbuf_tensor(U("mo_sil"), [128, 2, G], F32) as sil,
          nc.sbuf_tensor(U("mo_t1"), [128, 2, G], F32) as t1, nc.sbuf_tensor(U("mo_act"), [128, 2, 2, G], BF16) as act,
          nc.sbuf_tensor(U("mo_xs"), [128, 2, G], F32) as xs,
          nc.psum_tensor(U("mo_hg"), [128, 2, 512], F32) as hg, nc.psum_tensor(U("mo_hu"), [128, 2, 512], F32) as hu,
          nc.psum_tensor(U("mo_gb"), [128, 512], F32) as gb, nc.psum_tensor(U("mo_dn"), [128, 3, 512], F32) as dn):
        p.add('dve', lambda e: e.tensor_copy(out=sel[:], in_=cx.identB[0:64, 0:64].unsqueeze(2).to_broadcast([64, NE, 128])), writes=['sel'])
        it = 0
        di = 0
        xi = 0
        for g in range(NGR):
            p.load(ft[:], cx.aT[:, g * G:(g + 1) * G].rearrange("(c p) t -> p c t", p=128), writes=['ft'])
            p.load(gt[:], cx.gT[:, g * G:(g + 1) * G], writes=['gt'])
            for ex in range(NE + 1):
                s = it % 2
                it += 1
                W = ('w', s)
                WD = ('wd', s)
                p.load(wg[:, s, :], cx.img_eg[l][ex], writes=[W])
                p.load(wu[:, s, :], cx.img_eu[l][ex], writes=[W])
                p.load(wd[:, s, :], cx.img_ed[l][ex], writes=[WD])
                for (wt_, ps_, nm) in ((wg, hg, 'hg'), (wu, hu, 'hu')):
                    for mt, rows in ((0, 128), (1, 64)):
                        for k in range(KC):
                            p.mm(ps_[0:rows, mt, 0:G], wt_[:, s, k * FF + mt * 128:k * FF + mt * 128 + rows], ft[:, k, :], k == 0, k == KC - 1,
                                 reads=[W, 'ft'], writes=[(nm, mt)])
                if ex < NE:
                    p.mm(gb[:, 0:G], sel[:, ex, :], gt[:, :], True, True, reads=['sel', 'gt'], writes=['gb'])
                A = ('act', s)
                for mt, rows in ((0, 128), (1, 64)):
                    p.add('act', lambda e, mt=mt, rows=rows: e.activation(out=sil[0:rows, mt, :], in_=hg[0:rows, mt, 0:G], func=AF.Silu),
                          reads=[('hg', mt)], writes=[('sil', mt)])
                    if ex < NE:
                        p.add('dve', lambda e, mt=mt, rows=rows: e.tensor_tensor(out=t1[0:rows, mt, :], in0=sil[0:rows, mt, :], in1=hu[0:rows, mt, 0:G], op=ALU.mult),
                              reads=[('sil', mt), ('hu', mt)], writes=[('t1', mt)])
                        p.add('dve', lambda e, mt=mt, rows=rows, s=s: e.tensor_tensor(out=act[0:rows, s, mt, :], in0=t1[0:rows, mt, :], in1=gb[0:rows, 0:G], op=ALU.mult),
                              reads=[('t1', mt), 'gb'], writes=[A])
                    else:
                        p.add('dve', lambda e, mt=mt, rows=rows, s=s: e.tensor_tensor(out=act[0:rows, s, mt, :], in0=sil[0:rows, mt, :], in1=hu[0:rows, mt, 0:G], op=ALU.mult),
                              reads=[('sil', mt), ('hu', mt)], writes=[A])
                for c in range(KC):
                    db = di % 3
                    di += 1
                    p.mm(dn[:, db, 0:G], wd[:, s, c * 128:(c + 1) * 128], act[:, s, 0, :], True, False, reads=[WD, A], writes=[('dn', db)])
                    p.mm(dn[:, db, 0:G], wd[0:64, s, D + c * 128:D + (c + 1) * 128], act[0:64, s, 1, :], False, True, reads=[WD, A], writes=[('dn', db)])
                    if ex == 0:
                        p.add('dve', lambda e, c=c, db=db: e.tensor_copy(out=acc[:, c, :], in_=dn[:, db, 0:G]), reads=[('dn', db)], writes=[('acc', c)])
                    else:
                        p.add('dve', lambda e, c=c, db=db: e.tensor_tensor(out=acc[:, c, :], in0=acc[:, c, :], in1=dn[:, db, 0:G], op=ALU.add),
                              reads=[('dn', db), ('acc', c)], writes=[('acc', c)])
            for c in range(KC):
                s = xi % 2
                xi += 1
                key = ('xs', s)
                xa = cx.xT[c * 128:(c + 1) * 128, g * G:(g + 1) * G]
                p.load(xs[:, s, :], xa, writes=[key])
                for (c0, ncol, r) in segs_of_group(cx, g):
                    p.add('dve', lambda e, s=s, c=c, c0=c0, ncol=ncol, r=r: e.scalar_tensor_tensor(
                        out=xs[:, s, c0:c0 + ncol], in0=acc[:, c, c0:c0 + ncol], scalar=mv[:, 5, c, r:r + 1], in1=xs[:, s, c0:c0 + ncol],
                        op0=ALU.mult, op1=ALU.add), reads=[('acc', c), key, 'mv'], writes=[key])
                p.store(xa, xs[:, s, :], reads=[key])
        p.emit()


def build(B, S, layers, n_a, n_b, n_c):
    L = len(layers)
    nc = bass.Bass("TRN2", target_bir_lowering=False)
    cx = Ctx()
    cx.nc = nc
    cx.B, cx.S, cx.C = B, S, CTX
    cx.TB = S + CTX
    cx.NT = B * cx.TB
    NT = cx.NT
    assert cx.TB % G == 0 and NT % (3 * G) == 0

    kinds = [k for (k, j) in layers]
    cx.in_names = []

    def inp(name, shape, need=True):
        if not need:
            return None
        cx.in_names.append(name)
        return nc.dram_tensor(name, list(shape), F32, kind="ExternalInput").ap()

    def scr(name, shape, dt):
        return nc.dram_tensor(name, list(shape), dt).ap()

    xT0 = inp("xT0", [D, NT])
    cx.csT = inp("csT", [D, B + 1])
    ada_down = inp("ada_down", [L, D, 512])
    ada_up = inp("ada_up", [L, 512, 6 * D])
    cx.ada_bT = inp("ada_bT", [L, 128, 192])
    cx.n1gT = inp("n1gT", [L, 128, KC])
    cx.n2gT = inp("n2gT", [L, 128, KC])
    cx.fgT = inp("fgT", [128, KC])
    a_w_qkv = inp("a_w_qkv", [max(n_a, 1), D, 6144], 0 in kinds)
    a_w_o = inp("a_w_o", [max(n_a, 1), D, D], 0 in kinds)
    cx.a_sink = inp("a_sink", [max(n_a, 1), 32], 0 in kinds)
    b_w_qkv = inp("b_w_qkv", [max(n_b, 1), D, 3 * D], 1 in kinds)
    b_w_o = inp("b_w_o", [max(n_b, 1), D, D], 1 in kinds)
    cx.b_lamT = inp("b_lamT", [128, 4], 1 in kinds)
    cx.b_sgT = inp("b_sgT", [128, 2], 1 in kinds)
    c_w_in = inp("c_w_in", [max(n_c, 1), D, 3 * D], 2 in kinds)
    cx.c_convT = inp("c_convT", [128, KC, 3], 2 in kinds)
    c_w_out = inp("c_w_out", [max(n_c, 1), D, D], 2 in kinds)
    cx.router_w = inp("router_w", [L, D, NE])
    cx.router_b = inp("router_b", [L, NE])
    exp_gate = inp("exp_gate", [L, NE, D, FF])
    exp_up = inp("exp_up", [L, NE, D, FF])
    exp_down = inp("exp_down", [L, NE, FF, D])
    sh_gate = inp("sh_gate", [L, D, FF])
    sh_up = inp("sh_up", [L, D, FF])
    sh_down = inp("sh_down", [L, FF, D])
    identD = inp("ident", [128, 128])
    cos2D = inp("cos2", [128, S])
    sin2D = inp("sin2", [128, S])
    wmaskD = inp("wmask", [128, 384])
    cx.outT = nc.dram_tensor("outT", [D, NT], F32, kind="ExternalOutput").ap()

    cx.xT = scr("xT", [D, NT], F32)
    cx.aT = scr("aT", [D, NT], BF16)
    cx.gT = scr("gT", [NE, NT], BF16)
    cx.qkvT = scr("qkvT", [3 * D, NT], BF16)
    cx.vN = scr("vN", [NT, D], BF16)
    cx.oT = scr("oT", [D, NT], BF16)
    cx.img = {}
    jobs = []

    def img_proj(name, src, nl, K, N, slabw):
        kc = K // 128
        nslab = N // slabw
        im = scr("im_" + name, [nl, nslab, 128, kc * slabw], BF16)
        cx.img[name] = im
        for l_ in range(nl):
            v = src[l_].rearrange("(c p) n -> p c n", p=128)
            for s in range(nslab):
                jobs.append((v[:, :, s * slabw:(s + 1) * slabw], im[l_, s], 128, kc, slabw))

    kinds = [k for (k, j) in layers]
    img_proj('ada_down', ada_down, L, D, 512, 512)
    img_proj('ada_up', ada_up, L, 512, 6 * D, 2048)
    if 0 in kinds:
        img_proj('a_w_qkv', a_w_qkv, n_a, D, 6144, 512)
        img_proj('a_w_o', a_w_o, n_a, D, D, 512)
    if 1 in kinds:
        img_proj('b_w_qkv', b_w_qkv, n_b, D, 3 * D, 512)
        img_proj('b_w_o', b_w_o, n_b, D, D, 512)
    if 2 in kinds:
        img_proj('c_w_in', c_w_in, n_c, D, 3 * D, 512)
        img_proj('c_w_out', c_w_out, n_c, D, D, 512)
    cx.img_eg = [scr("im_eg%d" % l_, [NE + 1, 128, KC * FF], BF16) for l_ in range(L)]
    cx.img_eu = [scr("im_eu%d" % l_, [NE + 1, 128, KC * FF], BF16) for l_ in range(L)]
    cx.img_ed = [scr("im_ed%d" % l_, [NE + 1, 128, 2 * D], BF16) for l_ in range(L)]
    for l_ in range(L):
        for ex in range(NE + 1):
            sg_ = exp_gate[l_, ex] if ex < NE else sh_gate[l_]
            su_ = exp_up[l_, ex] if ex < NE else sh_up[l_]
            sd_ = exp_down[l_, ex] if ex < NE else sh_down[l_]
            jobs.append((sg_.rearrange("(c p) f -> p c f", p=128), cx.img_eg[l_][ex], 128, KC, FF))
            jobs.append((su_.rearrange("(c p) f -> p c f", p=128), cx.img_eu[l_][ex], 128, KC, FF))
            jobs.append((sd_[0:128, :].rearrange("p (a n) -> p a n", a=1), cx.img_ed[l_][ex][:, 0:D], 128, 1, D))
            jobs.append((sd_[128:192, :].rearrange("p (a n) -> p a n", a=1), cx.img_ed[l_][ex][0:64, D:2 * D], 64, 1, D))

    from contextlib import ExitStack
    with ExitStack() as stack:
        cx.gl = Glob(nc, stack)
        E = stack.enter_context
        cx.modv = E(nc.sbuf_tensor(U("modv"), [128, 6, KC, B + 1], F32))
        cx.ident = E(nc.sbuf_tensor(U("identS"), [128, 128], F32))
        cx.identB = E(nc.sbuf_tensor(U("identB"), [128, 128], BF16))
        cx.onesF = E(nc.sbuf_tensor(U("onesF"), [128, 128], F32))
        cx.onesB = E(nc.sbuf_tensor(U("onesB"), [128, 128], BF16))
        cx.cos2 = E(nc.sbuf_tensor(U("cos2S"), [128, S], F32))
        cx.sin2 = E(nc.sbuf_tensor(U("sin2S"), [128, S], F32))
        cx.wmask = E(nc.sbuf_tensor(U("wmaskS"), [128, 384], BF16))
        wm32 = E(nc.sbuf_tensor(U("wm32"), [128, 384], F32))
        p = Prog(cx.gl)
        p.load(cx.ident[:], identD, writes=['id'])
        p.load(cx.cos2[:], cos2D, writes=['cs'])
        p.load(cx.sin2[:], sin2D, writes=['cs'])
        p.load(wm32[:], wmaskD, writes=['wm'])
        p.add('dve', lambda e: e.tensor_copy(out=cx.identB[:], in_=cx.ident[:]), reads=['id'], writes=['idb'])
        p.add('dve', lambda e: e.tensor_copy(out=cx.wmask[:], in_=wm32[:]), reads=['wm'], writes=['wmb'])
        p.add('dve', lambda e: e.memset(cx.onesF[:], 1.0), writes=['of'])
        p.add('dve', lambda e: e.memset(cx.onesB[:], 1.0), writes=['ob'])
        with nc.sbuf_tensor(U("in_x"), [128, 2, 8192], F32) as xb:
            i = 0
            for r0 in range(0, D, 128):
                for c0 in range(0, NT, 8192):
                    n = min(8192, NT - c0)
                    s = i % 2
                    i += 1
                    p.load(xb[:, s, 0:n], xT0[r0:r0 + 128, c0:c0 + n], writes=[('xb', s)])
                    p.store(cx.xT[r0:r0 + 128, c0:c0 + n], xb[:, s, 0:n], reads=[('xb', s)])
            p.emit()
        phase_convert(cx, jobs)
        for li, (kind, j) in enumerate(layers):
            phase_mod(cx, li)
            phase_norm(cx, li, 1)
            if kind == 0:
                modes = ['T'] * 10 + ['N'] * 2
                run_proj_store(cx, cx.aT, cx.img['a_w_qkv'][j], 12, outT=cx.qkvT, outN=cx.vN, modes=modes,
                               col_of_slab=lambda s: (s - 10) * 512)
                phase_attn_a(cx, j)
                run_proj_store(cx, cx.oT, cx.img['a_w_o'][j], 8, resid_gate=2)
            elif kind == 1:
                modes = ['T'] * 16 + ['N'] * 8
                run_proj_store(cx, cx.aT, cx.img['b_w_qkv'][j], 24, outT=cx.qkvT, outN=cx.vN, modes=modes,
                               col_of_slab=lambda s: (s - 16) * 512)
                phase_attn_b(cx, cx.layer_ids[li])
                run_proj_store(cx, cx.oT, cx.img['b_w_o'][j], 8, resid_gate=2)
            else:
                run_proj_store(cx, cx.aT, cx.img['c_w_in'][j], 24, outT=cx.qkvT)
                phase_conv(cx)
                run_proj_store(cx, cx.oT, cx.img['c_w_out'][j], 8, resid_gate=2)
            phase_norm(cx, li, 2)
            phase_moe(cx, li)
        phase_norm(cx, 0, 1, final=True)
    cx.n_ins = cx.gl.n_ins
    return nc, cx


def rope_tables(S):
    rows = S // 64
    row = np.repeat(np.arange(rows), 64).astype(np.float32)
    col = np.tile(np.arange(64), rows).astype(np.float32)
    n_freq = HD // 4
    inv = (10000.0 ** (-np.arange(n_freq, dtype=np.float32) / n_freq)).astype(np.float32)
    ang = np.concatenate([row[:, None] * inv, col[:, None] * inv], axis=-1)
    cos, sin = np.cos(ang).T.astype(np.float32), np.sin(ang).T.astype(np.float32)
    cos2 = np.concatenate([cos, cos], axis=0)
    sin2 = np.concatenate([-sin, sin], axis=0)
    return np.ascontiguousarray(cos2), np.ascontiguousarray(sin2)


def window_mask():
    i = np.arange(128)[:, None]
    jj = np.arange(128)[None, :]
    m = np.concatenate([(i <= jj), np.ones((128, 128), bool), (jj <= i)], axis=1)
    return m.astype(np.float32)


def prep_shared(inputs, layers, layer_ids, S):
    f = lambda a: np.ascontiguousarray(np.asarray(a, dtype=np.float32))
    li = list(layer_ids)

    def pick(a):
        a = f(a)
        return a if li == list(range(a.shape[0])) else np.ascontiguousarray(a[li])
    pm = lambda a: np.ascontiguousarray(a.reshape(a.shape[0], -1, 128).transpose(0, 2, 1))
    m = {
        'ada_down': pick(inputs['ada_down']), 'ada_up': pick(inputs['ada_up']),
        'ada_bT': pm(pick(inputs['ada_b'])), 'n1gT': pm(pick(inputs['norm1_g'])), 'n2gT': pm(pick(inputs['norm2_g'])),
        'fgT': pm(f(inputs['final_g'])[None])[0],
        'a_w_qkv': f(inputs['a_w_qkv']), 'a_w_o': f(inputs['a_w_o']), 'a_sink': f(inputs['a_sink']),
        'b_w_qkv': f(inputs['b_w_qkv']), 'b_w_o': f(inputs['b_w_o']),
        'b_lamT': np.ascontiguousarray(f(inputs['b_lam'])[0].T), 'b_sgT': np.ascontiguousarray(f(inputs['b_subln_g'])[0].reshape(2, 128).T),
        'c_w_in': f(inputs['c_w_in']), 'c_convT': np.ascontiguousarray(f(inputs['c_conv'])[0].reshape(3, KC, 128).transpose(2, 1, 0)),
        'c_w_out': f(inputs['c_w_out']),
        'router_w': pick(inputs['router_w']), 'router_b': pick(inputs['router_b']),
        'exp_gate': pick(inputs['exp_gate']), 'exp_up': pick(inputs['exp_up']), 'exp_down': pick(inputs['exp_down']),
        'sh_gate': pick(inputs['sh_gate']), 'sh_up': pick(inputs['sh_up']), 'sh_down': pick(inputs['sh_down']),
        'ident': np.eye(128, dtype=np.float32), 'wmask': window_mask(),
    }
    m['cos2'], m['sin2'] = rope_tables(S)
    return m


def run(inputs, layer_ids, layers=None):
    f = lambda a: np.ascontiguousarray(np.asarray(a, dtype=np.float32))
    x, ctx, c, c_ctx = f(inputs['x']), f(inputs['ctx']), f(inputs['c']), f(inputs['c_ctx'])
    B, S, _ = x.shape
    if layers is None:
        layers = [(i % 3, i // 3) for i in layer_ids]
    n_a, n_b, n_c = inputs['a_w_qkv'].shape[0], inputs['b_w_qkv'].shape[0], inputs['c_w_in'].shape[0]
    Ctx.layer_ids = list(layer_ids)
    nc, cx = build(1, S, layers, n_a, n_b, n_c)
    shared = prep_shared(inputs, layers, layer_ids, S)
    maps = []
    for b in range(B):
        m = dict(shared)
        m['xT0'] = np.ascontiguousarray(np.concatenate([x[b], ctx[b]], axis=0).T)
        m['csT'] = np.ascontiguousarray(np.stack([c[b], c_ctx], axis=0).T)
        maps.append({k: v for k, v in m.items() if k in cx.in_names})
    res = run_bass_kernel_spmd(nc, maps, core_ids=list(range(B)))
    out = np.stack([res.results[b]["outT"].T[:S, :] for b in range(B)], axis=0)
    return np.ascontiguousarray(out.astype(np.float32))


def kernel(**inputs):
    return run(inputs, list(range(4)))
```

```python
import math
import numpy as np
import concourse.bass as bass
import concourse.mybir as mybir
from concourse.bass_utils import run_bass_kernel_spmd

F32 = mybir.dt.float32
BF16 = mybir.dt.bfloat16
ALU = mybir.AluOpType
AF = mybir.ActivationFunctionType
AX = mybir.AxisListType

D = 4096
KC = 32
CTX = 256
HD = 128
NE = 64
FF = 192
G = 384
EPS = 1e-6
ENGS = ('pe', 'act', 'dve', 'pool', 'sp')
BNAME = {'pe': 'tensor', 'act': 'scalar', 'dve': 'vector', 'pool': 'gpsimd', 'sp': 'sync'}
DMAK = 6
DEBUG_MODE = ''


class Op:
    __slots__ = ('eng', 'fn', 'deps', 'sig', 'val', 'sem', 'dma', 'prev')


class Glob:
    def __init__(self, nc, stack):
        self.nc = nc
        self.sem = {e: stack.enter_context(nc.semaphore("s_" + e)) for e in ENGS}
        self.cnt = {e: 0 for e in ENGS}
        self.dsem = {e: [stack.enter_context(nc.semaphore("d_%s%d" % (e, i))) for i in range(DMAK)]
                     for e in ('sp', 'pool')}
        self.dcnt = {e: [0] * DMAK for e in ('sp', 'pool')}
        self.di = {e: 0 for e in ('sp', 'pool')}
        self.dlast = {e: [None] * DMAK for e in ('sp', 'pool')}
        self.n_ins = 0


class Prog:
    def __init__(self, gl):
        self.gl = gl
        self.ops = {e: [] for e in ENGS}
        self.lastw = {}
        self.readers = {}
        self.nd = 0

    def add(self, eng, fn, reads=(), writes=(), dma=False):
        op = Op()
        op.eng = eng; op.fn = fn; op.sig = False; op.dma = dma; op.prev = None; op.sem = None; op.val = 0
        deps = {}
        for b in reads:
            w = self.lastw.get(b)
            if w is not None:
                deps[id(w)] = w
        for b in writes:
            w = self.lastw.get(b)
            if w is not None:
                deps[id(w)] = w
            rd = self.readers.get(b)
            if rd:
                for r in rd.values():
                    deps[id(r)] = r
        if dma:
            self.nd += 1
            rk = (eng, self.nd)
        else:
            rk = eng
        for b in reads:
            self.readers.setdefault(b, {})[rk] = op
        for b in writes:
            self.lastw[b] = op
            self.readers[b] = {}
        op.deps = [d for d in deps.values() if d is not op and not (d.eng == 'pe' and eng == 'pe')]
        self.ops[eng].append(op)
        return op

    def mm(self, out, lhsT, rhs, start, stop, reads, writes):
        return self.add('pe', lambda e: e.matmul(out, lhsT=lhsT, rhs=rhs, start=start, stop=stop), reads, writes)

    def load(self, out, in_, reads=(), writes=()):
        return self.add('sp', lambda e: e.dma_start(out=out, in_=in_), reads, writes, dma=True)

    def store(self, out, in_, reads=(), writes=()):
        return self.add('pool', lambda e: e.dma_start(out=out, in_=in_), reads, writes, dma=True)

    def emit(self):
        gl = self.gl
        nc = gl.nc
        bar = [(gl.sem[e], gl.cnt[e]) for e in ENGS if gl.cnt[e] > 0]
        for e in ('sp', 'pool'):
            for i in range(DMAK):
                if gl.dcnt[e][i] > 0:
                    bar.append((gl.dsem[e][i], gl.dcnt[e][i]))
        for e in ENGS:
            for op in self.ops[e]:
                for d in op.deps:
                    d.sig = True
            if self.ops[e]:
                last = [o for o in self.ops[e] if not o.dma]
                if last:
                    last[-1].sig = True
        for e in ENGS:
            for op in self.ops[e]:
                if op.dma:
                    i = gl.di[e] % DMAK
                    gl.di[e] += 1
                    gl.dcnt[e][i] += 16
                    op.sem = gl.dsem[e][i]; op.val = gl.dcnt[e][i]
                    op.prev = gl.dlast[e][i]
                    gl.dlast[e][i] = op
                elif op.sig:
                    gl.cnt[e] += 1
                    op.sem = gl.sem[e]; op.val = gl.cnt[e]
        with nc.Block() as block:
            for e in ENGS:
                ops = self.ops[e]

                def body(eng, e=e, ops=ops):
                    known = {}
                    for (s, v) in bar:
                        eng.wait_ge(s, v)
                        known[id(s)] = v
                    for op in ops:
                        waits = {}
                        for d in op.deps:
                            k = id(d.sem)
                            if waits.get(k, (None, 0))[1] < d.val:
                                waits[k] = (d.sem, d.val)
                        if op.dma and op.prev is not None:
                            k = id(op.prev.sem)
                            if waits.get(k, (None, 0))[1] < op.prev.val:
                                waits[k] = (op.prev.sem, op.prev.val)
                        for k, (s, v) in waits.items():
                            if known.get(k, 0) < v:
                                eng.wait_ge(s, v)
                                known[k] = v
                        ins = op.fn(eng)
                        gl.n_ins += 1
                        if op.dma:
                            ins.then_inc(op.sem, 16)
                        elif op.sig:
                            ins.then_inc(op.sem, 1)
                    if e in ('sp', 'pool'):
                        for i in range(DMAK):
                            if gl.dcnt[e][i] > 0:
                                eng.wait_ge(gl.dsem[e][i], gl.dcnt[e][i])
                getattr(block, BNAME[e])(body)


class Ctx:
    pass


_UC = [0]


def U(name):
    _UC[0] += 1
    return "%s_%d" % (name, _UC[0])


def row_of(cx, t128):
    tb = cx.TB // 128
    b, r = divmod(t128, tb)
    return b if r < cx.S // 128 else cx.B


def segs_of_group(cx, g):
    out = []
    for j in range(G // 128):
        r = row_of(cx, g * (G // 128) + j)
        if out and out[-1][2] == r:
            out[-1] = (out[-1][0], out[-1][1] + 128, r)
        else:
            out.append((j * 128, 128, r))
    return out


def phase_convert(cx, jobs):
    nc = cx.nc
    p = Prog(cx.gl)
    NS = 3
    with nc.sbuf_tensor(U("cv_s"), [128, NS, 8192], F32) as st, nc.sbuf_tensor(U("cv_b"), [128, NS, 8192], BF16) as bt:
        i = 0
        for (src, dst, P, A, Bc) in jobs:
            astep = max(1, 8192 // Bc)
            for a0 in range(0, A, astep):
                a1 = min(A, a0 + astep)
                n = (a1 - a0) * Bc
                s = i % NS
                sv = st[:P, s, 0:n].rearrange("p (a b) -> p a b", b=Bc)
                p.load(sv, src[:, a0:a1, :], writes=[('cs', s)])
                if i % 2 == 0:
                    p.add('dve', lambda e, o=bt[:P, s, 0:n], x=st[:P, s, 0:n]: e.tensor_copy(out=o, in_=x),
                          reads=[('cs', s)], writes=[('cb', s)])
                else:
                    p.add('act', lambda e, o=bt[:P, s, 0:n], x=st[:P, s, 0:n]: e.activation(out=o, in_=x, func=AF.Copy),
                          reads=[('cs', s)], writes=[('cb', s)])
                p.store(dst[:, a0 * Bc:a1 * Bc], bt[:P, s, 0:n], reads=[('cb', s)])
                i += 1
        p.emit()


def phase_mod(cx, l):
    nc = cx.nc
    p = Prog(cx.gl)
    R = cx.B + 1
    with (nc.sbuf_tensor(U("md_src"), [128, KC, R], F32) as src, nc.sbuf_tensor(U("md_srcb"), [128, KC, R], BF16) as srcb,
          nc.sbuf_tensor(U("md_sg"), [128, KC, R], F32) as sg,
          nc.sbuf_tensor(U("md_wd"), [128, KC * 512], BF16) as wd, nc.sbuf_tensor(U("md_h"), [128, 4, R], BF16) as h1,
          nc.sbuf_tensor(U("md_wu"), [128, 2, 4 * 2048], BF16) as wu, nc.sbuf_tensor(U("md_ab"), [128, 192], F32) as ab,
          nc.sbuf_tensor(U("md_raw"), [128, 192, R], F32) as raw, nc.sbuf_tensor(U("md_g"), [128, 2, KC], F32) as ng,
          nc.psum_tensor(U("md_ps"), [128, 4, 512], F32) as ps):
        p.load(src[:], cx.csT.rearrange("(c p) r -> p c r", p=128), writes=['src'])
        p.load(wd[:], cx.img['ada_down'][l, 0], writes=['wd'])
        p.load(ab[:], cx.ada_bT[l], writes=['ab'])
        p.load(ng[:, 0, :], cx.n1gT[l], writes=['ng'])
        p.load(ng[:, 1, :], cx.n2gT[l], writes=['ng'])
        p.add('act', lambda e: e.activation(out=sg[:], in_=src[:], func=AF.Sigmoid), reads=['src'], writes=['sg'])
        p.add('dve', lambda e: e.tensor_tensor(out=srcb[:], in0=src[:], in1=sg[:], op=ALU.mult), reads=['src', 'sg'], writes=['srcb'])
        for m in range(4):
            for k in range(KC):
                p.mm(ps[:, m, 0:R], wd[:, k * 512 + m * 128: k * 512 + (m + 1) * 128], srcb[:, k, :], k == 0, k == KC - 1,
                     reads=['wd', 'srcb'], writes=[('ps', m)])
            p.add('dve', lambda e, m=m: e.tensor_copy(out=h1[:, m, :], in_=ps[:, m, 0:R]), reads=[('ps', m)], writes=['h1'])
        for s in range(12):
            sl = s % 2
            p.load(wu[:, sl, :], cx.img['ada_up'][l, s], writes=[('wu', sl)])
            for mm_ in range(16):
                m = s * 16 + mm_
                pb = mm_ % 4
                for k in range(4):
                    p.mm(ps[:, pb, 0:R], wu[:, sl, k * 2048 + mm_ * 128: k * 2048 + (mm_ + 1) * 128], h1[:, k, :], k == 0, k == 3,
                         reads=[('wu', sl), 'h1'], writes=[('ps', pb)])
                p.add('dve', lambda e, m=m, pb=pb: e.tensor_scalar(out=raw[:, m, :], in0=ps[:, pb, 0:R], scalar1=ab[:, m:m + 1],
                                                                   scalar2=None, op0=ALU.add),
                      reads=[('ps', pb), 'ab'], writes=['raw'])
        mv = cx.modv
        for which, (ish, isc, ig) in enumerate(((0, 1, 2), (3, 4, 5))):
            o = which * 3
            p.add('dve', lambda e, o=o, isc=isc: e.tensor_scalar(out=mv[:, o, :, :], in0=raw[:, isc * 32:(isc + 1) * 32, :], scalar1=1.0,
                                                                   scalar2=None, op0=ALU.add), reads=['raw'], writes=['mv'])
            p.add('dve', lambda e, o=o, which=which: e.tensor_tensor(out=mv[:, o, :, :], in0=mv[:, o, :, :],
                                                                     in1=ng[:, which, :].unsqueeze(2).to_broadcast([128, KC, R]), op=ALU.mult),
                  reads=['mv', 'ng'], writes=['mv'])
            p.add('dve', lambda e, o=o, ish=ish: e.tensor_copy(out=mv[:, o + 1, :, :], in_=raw[:, ish * 32:(ish + 1) * 32, :]), reads=['raw'], writes=['mv'])
            p.add('dve', lambda e, o=o, ig=ig: e.tensor_copy(out=mv[:, o + 2, :, :], in_=raw[:, ig * 32:(ig + 1) * 32, :]), reads=['raw'], writes=['mv'])
        p.emit()


def phase_norm(cx, l, which, final=False):
    nc = cx.nc
    p = Prog(cx.gl)
    mv = cx.modv
    NGR = cx.NT // G
    router = (which == 2 and not final)
    with (nc.sbuf_tensor(U("nm_x"), [128, 2, KC, G], F32) as xg, nc.sbuf_tensor(U("nm_a"), [128, 2, KC, G], BF16) as ag,
          nc.sbuf_tensor(U("nm_sq"), [128, 2, G], F32) as sq, nc.sbuf_tensor(U("nm_r"), [128, G], F32) as rs,
          nc.sbuf_tensor(U("nm_fg"), [128, KC], F32) as fg,
          nc.sbuf_tensor(U("nm_rw"), [128, KC, NE], F32) as rw, nc.sbuf_tensor(U("nm_rb"), [128, NE], F32) as rb,
          nc.sbuf_tensor(U("nm_t"), [128, 12, NE], F32) as tt, nc.sbuf_tensor(U("nm_s"), [128, 8, 8], F32) as sm,
          nc.sbuf_tensor(U("nm_cm"), [128, 8, 8], F32) as cmp8,
          nc.sbuf_tensor(U("nm_gt"), [64, 2, G], BF16) as gts,
          nc.psum_tensor(U("nm_ps"), [128, 512], F32) as ps, nc.psum_tensor(U("nm_pl"), [128, 2, 512], F32) as pl,
          nc.psum_tensor(U("nm_pt"), [128, 2, 512], F32) as pt):
        if final:
            p.load(fg[:], cx.fgT, writes=['fg'])
        if router:
            p.load(rw[:], cx.router_w[l].rearrange("(c p) e -> p c e", p=128), writes=['rw'])
            p.load(rb[:], cx.router_b[l:l + 1, :].to_broadcast((128, NE)), writes=['rb'])
        for g in range(NGR):
            s = g % 2
            X = ('x', s)
            p.load(xg[:, s], cx.xT[:, g * G:(g + 1) * G].rearrange("(c p) t -> p c t", p=128), writes=[X])
            for c in range(KC):
                q = c % 2
                p.add('act', lambda e, s=s, c=c, q=q: e.activation(out=sq[:, q, :], in_=xg[:, s, c, :], func=AF.Square),
                      reads=[X], writes=[('sq', q)])
                p.mm(ps[:, 0:G], cx.onesF[:], sq[:, q, :], c == 0, c == KC - 1, reads=[('sq', q)], writes=['ps'])
            p.add('dve', lambda e: e.tensor_scalar(out=rs[:], in0=ps[:, 0:G], scalar1=1.0 / D, scalar2=EPS, op0=ALU.mult, op1=ALU.add),
                  reads=['ps'], writes=['rs'])
            p.add('act', lambda e: e.activation(out=rs[:], in_=rs[:], func=AF.Sqrt), reads=['rs'], writes=['rs'])
            p.add('dve', lambda e: e.reciprocal(out=rs[:], in_=rs[:]), reads=['rs'], writes=['rs'])
            h = KC // 2
            p.add('dve', lambda e, s=s: e.tensor_tensor(out=xg[:, s, 0:h], in0=xg[:, s, 0:h], in1=rs[:].unsqueeze(1).to_broadcast([128, h, G]), op=ALU.mult),
                  reads=[X, 'rs'], writes=[X])
            p.add('pool', lambda e, s=s: e.tensor_tensor(out=xg[:, s, h:KC], in0=xg[:, s, h:KC], in1=rs[:].unsqueeze(1).to_broadcast([128, h, G]), op=ALU.mult),
                  reads=[X, 'rs'], writes=[X])
            A = ('a', s)
            if final:
                for c in range(KC):
                    eng = 'act' if c % 2 == 0 else 'dve'
                    if eng == 'act':
                        p.add('act', lambda e, s=s, c=c: e.activation(out=xg[:, s, c, :], in_=xg[:, s, c, :], func=AF.Copy, scale=fg[:, c:c + 1]),
                              reads=[X, 'fg'], writes=[X])
                    else:
                        p.add('dve', lambda e, s=s, c=c: e.tensor_scalar(out=xg[:, s, c, :], in0=xg[:, s, c, :], scalar1=fg[:, c:c + 1], scalar2=None, op0=ALU.mult),
                              reads=[X, 'fg'], writes=[X])
                p.store(cx.outT[:, g * G:(g + 1) * G].rearrange("(c p) t -> p c t", p=128), xg[:, s], reads=[X])
                continue
            o = (which - 1) * 3
            for (c0, ncol, r) in segs_of_group(cx, g):
                for c in range(KC):
                    dst = xg[:, s, c, c0:c0 + ncol] if router else ag[:, s, c, c0:c0 + ncol]
                    wr = [X] if router else [A]
                    if c % 2 == 0:
                        p.add('act', lambda e, dst=dst, s=s, c=c, c0=c0, ncol=ncol, r=r: e.activation(
                            out=dst, in_=xg[:, s, c, c0:c0 + ncol], func=AF.Identity, scale=mv[:, o, c, r:r + 1], bias=mv[:, o + 1, c, r:r + 1]),
                            reads=[X, 'mv'], writes=wr)
                    else:
                        p.add('dve', lambda e, dst=dst, s=s, c=c, c0=c0, ncol=ncol, r=r: e.tensor_scalar(
                            out=dst, in0=xg[:, s, c, c0:c0 + ncol], scalar1=mv[:, o, c, r:r + 1], scalar2=mv[:, o + 1, c, r:r + 1],
                            op0=ALU.mult, op1=ALU.add), reads=[X, 'mv'], writes=wr)
            if router:
                p.add('pool', lambda e, s=s: e.tensor_copy(out=ag[:, s], in_=xg[:, s]), reads=[X], writes=[A])
                GT = ('gt', s)
                for j in range(G // 128):
                    pj = j % 2
                    for c in range(KC):
                        p.mm(pl[:, pj, 0:NE], xg[:, s, c, j * 128:(j + 1) * 128], rw[:, c, :], c == 0, c == KC - 1,
                             reads=[X, 'rw'], writes=[('pl', pj)])
                    sc, bi, t2, msk, w, sel = (tt[:, i, :] for i in range(6))
                    v3 = lambda a: a.rearrange("p (g k) -> p g k", k=8)
                    m1, m2, gs, gm, pen, cnt = (sm[:, i, :] for i in range(6))
                    top8 = sm[:, 6, :]
                    thr = sm[:, 7, 0:1]
                    wsum = sm[:, 7, 1:2]
                    T = 'tt'
                    p.add('act', lambda e, pj=pj, sc=sc: e.activation(out=sc, in_=pl[:, pj, 0:NE], func=AF.Sigmoid), reads=[('pl', pj)], writes=[T])
                    dv = lambda fn: p.add('dve', fn, reads=[T, 'rb'], writes=[T])
                    dv(lambda e, bi=bi, sc=sc: e.tensor_tensor(out=bi, in0=sc, in1=rb[:], op=ALU.add))
                    dv(lambda e, m1=m1, bi=bi: e.tensor_reduce(out=m1, in_=v3(bi), axis=AX.X, op=ALU.max))
                    dv(lambda e, t2=t2, bi=bi, m1=m1: e.tensor_tensor(out=v3(t2), in0=v3(bi), in1=m1.unsqueeze(2).to_broadcast([128, 8, 8]), op=ALU.is_equal))
                    dv(lambda e, t2=t2, bi=bi: e.scalar_tensor_tensor(out=t2, in0=t2, scalar=-1e30, in1=bi, op0=ALU.mult, op1=ALU.add))
                    dv(lambda e, m2=m2, t2=t2: e.tensor_reduce(out=m2, in_=v3(t2), axis=AX.X, op=ALU.max))
                    dv(lambda e, gs=gs, m1=m1, m2=m2: e.tensor_tensor(out=gs, in0=m1, in1=m2, op=ALU.add))
                    dv(lambda e, gs=gs: e.tensor_tensor(out=cmp8[:], in0=gs.unsqueeze(1).to_broadcast([128, 8, 8]),
                                                        in1=gs.unsqueeze(2).to_broadcast([128, 8, 8]), op=ALU.is_gt))
                    dv(lambda e, cnt=cnt: e.tensor_reduce(out=cnt, in_=cmp8[:], axis=AX.X, op=ALU.add))
                    dv(lambda e, pen=pen, cnt=cnt: e.tensor_scalar(out=pen, in0=cnt, scalar1=3.5, scalar2=-1e30, op0=ALU.is_gt, op1=ALU.mult))
                    dv(lambda e, msk=msk, bi=bi, pen=pen: e.tensor_tensor(out=v3(msk), in0=v3(bi), in1=pen.unsqueeze(2).to_broadcast([128, 8, 8]), op=ALU.add))
                    dv(lambda e, top8=top8, msk=msk: e.max(out=top8, in_=msk))
                    dv(lambda e, thr=thr, top8=top8: e.tensor_reduce(out=thr, in_=top8, axis=AX.X, op=ALU.min))
                    dv(lambda e, sel=sel, msk=msk, thr=thr: e.tensor_scalar(out=sel, in0=msk, scalar1=thr, scalar2=None, op0=ALU.is_ge))
                    dv(lambda e, w=w, sc=sc, sel=sel: e.tensor_tensor(out=w, in0=sc, in1=sel, op=ALU.mult))
                    dv(lambda e, wsum=wsum, w=w: e.tensor_reduce(out=wsum, in_=w, axis=AX.X, op=ALU.add))
                    dv(lambda e, wsum=wsum: e.reciprocal(out=wsum, in_=wsum))
                    dv(lambda e, w=w, wsum=wsum: e.tensor_scalar(out=w, in0=w, scalar1=wsum, scalar2=2.5, op0=ALU.mult, op1=ALU.mult))
                    p.add('pe', lambda e, pj=pj, w=w: e.transpose(pt[0:NE, pj, 0:128], w, cx.ident[:]), reads=[T], writes=[('pt', pj)])
                    p.add('act', lambda e, pj=pj, s=s, j=j: e.activation(out=gts[:, s, j * 128:(j + 1) * 128], in_=pt[0:NE, pj, 0:128], func=AF.Copy),
                          reads=[('pt', pj)], writes=[GT])
                p.store(cx.gT[:, g * G:(g + 1) * G], gts[:, s, :], reads=[GT])
            p.store(cx.aT[:, g * G:(g + 1) * G].rearrange("(c p) t -> p c t", p=128), ag[:, s], reads=[A])
        p.emit()


def phase_proj(cx, inT, wimg, nslab, evac_T=None, evac_N=None, modes=None, kc=KC):
    nc = cx.nc
    p = Prog(cx.gl)
    GB = 3
    TBK = GB * G
    nblk = cx.NT // TBK
    assert cx.NT % TBK == 0
    with (nc.sbuf_tensor(U("pj_a"), [128, kc, TBK], BF16) as at, nc.sbuf_tensor(U("pj_w"), [128, 2, kc * 512], BF16) as wt,
          nc.psum_tensor(U("pj_ps"), [128, 4, 512], F32) as ps):
        cx.pj_extra(p)
        it = 0
        pi = 0
        for blk in range(nblk):
            p.load(at[:], inT[:, blk * TBK:(blk + 1) * TBK].rearrange("(c p) t -> p c t", p=128), writes=['at'])
            for s in range(nslab):
                sl = it % 2
                it += 1
                p.load(wt[:, sl, :], wimg[s], writes=[('w', sl)])
                mode = modes[s] if modes else 'T'
                if mode == 'T':
                    for mi in range(4):
                        for gi in range(GB):
                            pb = pi % 4
                            pi += 1
                            for k in range(kc):
                                p.mm(ps[:, pb, 0:G], wt[:, sl, k * 512 + mi * 128:k * 512 + (mi + 1) * 128], at[:, k, gi * G:(gi + 1) * G],
                                     k == 0, k == kc - 1, reads=[('w', sl), 'at'], writes=[('ps', pb)])
                            evac_T(p, ps[:, pb, 0:G], ('ps', pb), s * 4 + mi, blk * GB + gi)
                else:
                    for ti in range(TBK // 128):
                        pb = pi % 4
                        pi += 1
                        for k in range(kc):
                            p.mm(ps[:, pb, :], at[:, k, ti * 128:(ti + 1) * 128], wt[:, sl, k * 512:(k + 1) * 512],
                                 k == 0, k == kc - 1, reads=[('w', sl), 'at'], writes=[('ps', pb)])
                        evac_N(p, ps[:, pb, :], ('ps', pb), s, blk * (TBK // 128) + ti)
        p.emit()


def make_store_T(cx, stage, outT, nslot=4):
    st = {'i': 0}

    def ev(p, ps_ap, pskey, m, g):
        s = st['i'] % nslot
        st['i'] += 1
        key = ('stg', s)
        if st['i'] % 2 == 0:
            p.add('act', lambda e: e.activation(out=stage[:, s, 0:G], in_=ps_ap, func=AF.Copy), reads=[pskey], writes=[key])
        else:
            p.add('dve', lambda e: e.tensor_copy(out=stage[:, s, 0:G], in_=ps_ap), reads=[pskey], writes=[key])
        p.store(outT[m * 128:(m + 1) * 128, g * G:(g + 1) * G], stage[:, s, 0:G], reads=[key])
    return ev


def make_store_N(cx, stage, outN, col_of_slab, nslot=4):
    st = {'i': 0}

    def ev(p, ps_ap, pskey, s_, t):
        s = st['i'] % nslot
        st['i'] += 1
        key = ('stg', s)
        if st['i'] % 2 == 0:
            p.add('act', lambda e: e.activation(out=stage[:, s, :], in_=ps_ap, func=AF.Copy), reads=[pskey], writes=[key])
        else:
            p.add('dve', lambda e: e.tensor_copy(out=stage[:, s, :], in_=ps_ap), reads=[pskey], writes=[key])
        c0 = col_of_slab(s_)
        p.store(outN[t * 128:(t + 1) * 128, c0:c0 + 512], stage[:, s, :], reads=[key])
    return ev


def make_resid_T(cx, xst, gate_idx):
    st = {'i': 0}
    mv = cx.modv

    def ev(p, ps_ap, pskey, m, g):
        s = st['i'] % 3
        st['i'] += 1
        key = ('xs', s)
        xa = cx.xT[m * 128:(m + 1) * 128, g * G:(g + 1) * G]
        p.load(xst[:, s, :], xa, writes=[key])
        for (c0, ncol, r) in segs_of_group(cx, g):
            p.add('dve', lambda e, c0=c0, ncol=ncol, r=r: e.scalar_tensor_tensor(
                out=xst[:, s, c0:c0 + ncol], in0=ps_ap[:, c0:c0 + ncol], scalar=mv[:, gate_idx, m, r:r + 1], in1=xst[:, s, c0:c0 + ncol],
                op0=ALU.mult, op1=ALU.add), reads=[pskey, key, 'mv'], writes=[key])
        p.store(xa, xst[:, s, :], reads=[key])
    return ev


def run_proj_store(cx, inT, wimg, nslab, outT=None, outN=None, modes=None, col_of_slab=None, resid_gate=None):
    nc = cx.nc
    with (nc.sbuf_tensor(U("pj_st"), [128, 4, 512], BF16) as stage, nc.sbuf_tensor(U("pj_xs"), [128, 3, G], F32) as xst):
        cx.pj_extra = lambda p: None
        evT = None
        if resid_gate is not None:
            evT = make_resid_T(cx, xst, resid_gate)
        elif outT is not None:
            evT = make_store_T(cx, stage, outT)
        evN = make_store_N(cx, stage, outN, col_of_slab) if outN is not None else None
        phase_proj(cx, inT, wimg, nslab, evac_T=evT, evac_N=evN, modes=modes)


def rope_load(cx, p, dst, swb, swp, tmp, key, src_rows, t0, n, pos0):
    p.load(dst, src_rows[:, t0:t0 + n], writes=[key])
    p.load(swb[0:64, 0:n], src_rows[64:128, t0:t0 + n], writes=[(key, 'swb')])
    p.load(swb[64:128, 0:n], src_rows[0:64, t0:t0 + n], writes=[(key, 'swb')])
    p.add('pool', lambda e: e.tensor_tensor(out=tmp[:, 0:n], in0=swb[:, 0:n], in1=cx.sin2[:, pos0:pos0 + n], op=ALU.mult),
          reads=[(key, 'swb')], writes=[(key, 'tmp')])
    p.add('dve', lambda e: e.tensor_tensor(out=swp[:, 0:n], in0=dst, in1=cx.cos2[:, pos0:pos0 + n], op=ALU.mult),
          reads=[key], writes=[(key, 'sw')])
    p.add('dve', lambda e: e.tensor_tensor(out=dst, in0=swp[:, 0:n], in1=tmp[:, 0:n], op=ALU.add),
          reads=[(key, 'sw'), (key, 'tmp')], writes=[key])


def phase_attn_a(cx, j):
    nc = cx.nc
    p = Prog(cx.gl)
    S, C, TB = cx.S, cx.C, cx.TB
    NQB = S // 128
    SC = HD ** -0.5
    with (nc.sbuf_tensor(U("aa_k"), [128, 2, TB], BF16) as kt, nc.sbuf_tensor(U("aa_q"), [128, 2, TB], BF16) as qt,
          nc.sbuf_tensor(U("aa_sw"), [128, 2, S], F32) as swp, nc.sbuf_tensor(U("aa_tm"), [128, 2, S], F32) as tmp, nc.sbuf_tensor(U("aa_swb"), [128, 2, S], BF16) as swb,
          nc.sbuf_tensor(U("aa_v"), [128, 2, TB // 128, 128], BF16) as vt,
          nc.sbuf_tensor(U("aa_p"), [128, 2, NQB + 2, 512], BF16) as pt_, nc.sbuf_tensor(U("aa_pc"), [128, 2, 2, S + C], BF16) as pc,
          nc.sbuf_tensor(U("aa_o"), [128, 2, S + C], BF16) as ot, nc.sbuf_tensor(U("aa_r"), [128, 2, 512], F32) as rc,
          nc.sbuf_tensor(U("aa_es"), [128, 32], F32) as es,
          nc.psum_tensor(U("aa_s"), [128, 3, 512], F32) as pss, nc.psum_tensor(U("aa_po"), [128, 2, 512], F32) as pso,
          nc.psum_tensor(U("aa_pz"), [128, 2, 512], F32) as psz):
        p.load(es[:], cx.a_sink[j:j + 1, :].to_broadcast((128, 32)), writes=['es'])
        p.add('act', lambda e: e.activation(out=es[:], in_=es[:], func=AF.Exp), reads=['es'], writes=['es'])
        si = 0
        oi = 0
        hi = 0
        for b in range(cx.B):
            t0 = b * TB
            for kv in range(8):
                ks = (b * 8 + kv) % 2
                K = ('k', ks)
                krows = cx.qkvT[4096 + kv * 128:4096 + (kv + 1) * 128, :]
                rope_load(cx, p, kt[:, ks, 0:S], swb[:, 0, :], swp[:, 0, :], tmp[:, 0, :], K, krows, t0, S, 0)
                p.load(kt[:, ks, S:TB], krows[:, t0 + S:t0 + TB], writes=[K])
                V = ('v', ks)
                p.load(vt[:, ks], cx.vN[t0:t0 + TB, kv * 128:(kv + 1) * 128].rearrange("(n p) d -> p n d", p=128), writes=[V])
                for gq in range(4):
                    h = kv * 4 + gq
                    qs = hi % 2
                    hi += 1
                    Q = ('q', qs)
                    qrows = cx.qkvT[h * 128:(h + 1) * 128, :]
                    rope_load(cx, p, qt[:, qs, 0:S], swb[:, 1, :], swp[:, 1, :], tmp[:, 1, :], Q, qrows, t0, S, 0)
                    p.load(qt[:, qs, S:TB], qrows[:, t0 + S:t0 + TB], writes=[Q])
                    P = ('p', qs)
                    PC = ('pc', qs)
                    for kb in range(NQB):
                        q0 = max(0, kb - 1)
                        q1 = min(NQB, kb + 2)
                        n = (q1 - q0) * 128
                        sb = si % 3
                        si += 1
                        p.mm(pss[:, sb, 0:n], kt[:, ks, kb * 128:(kb + 1) * 128], qt[:, qs, q0 * 128:q1 * 128], True, True,
                             reads=[K, Q], writes=[('pss', sb)])
                        p.add('act', lambda e, sb=sb, n=n, qs=qs, kb=kb: e.activation(out=pt_[:, qs, kb, 0:n], in_=pss[:, sb, 0:n], func=AF.Exp, scale=SC),
                              reads=[('pss', sb)], writes=[P])
                        m0 = (q0 - (kb - 1)) * 128
                        p.add('pool', lambda e, n=n, qs=qs, kb=kb, m0=m0: e.tensor_tensor(out=pt_[:, qs, kb, 0:n], in0=pt_[:, qs, kb, 0:n],
                                                                                         in1=cx.wmask[:, m0:m0 + n], op=ALU.mult),
                              reads=[P], writes=[P])
                    for cb in range(C // 128):
                        for q0 in range(0, S + C, 512):
                            n = min(512, S + C - q0)
                            sb = si % 3
                            si += 1
                            p.mm(pss[:, sb, 0:n], kt[:, ks, S + cb * 128:S + (cb + 1) * 128], qt[:, qs, q0:q0 + n], True, True,
                                 reads=[K, Q], writes=[('pss', sb)])
                            p.add('act', lambda e, sb=sb, n=n, qs=qs, cb=cb, q0=q0: e.activation(out=pc[:, qs, cb, q0:q0 + n], in_=pss[:, sb, 0:n], func=AF.Exp, scale=SC),
                                  reads=[('pss', sb)], writes=[PC])
                    O = ('o', qs)
                    nqb_all = (S + C) // 128
                    for qg in range(0, nqb_all, 4):
                        ob = oi % 2
                        oi += 1
                        nq = min(4, nqb_all - qg)
                        for qi in range(nq):
                            qb = qg + qi
                            terms = []
                            if qb < NQB:
                                for kb in (qb - 1, qb, qb + 1):
                                    if 0 <= kb < NQB:
                                        q0 = max(0, kb - 1)
                                        off = (qb - q0) * 128
                                        terms.append((vt[:, ks, kb, :], pt_[:, qs, kb, off:off + 128]))
                            for cb in range(C // 128):
                                terms.append((vt[:, ks, NQB + cb, :], pc[:, qs, cb, qb * 128:(qb + 1) * 128]))
                            for ti, (vv, pp) in enumerate(terms):
                                p.mm(pso[:, ob, qi * 128:(qi + 1) * 128], vv, pp, ti == 0, ti == len(terms) - 1,
                                     reads=[V, P, PC], writes=[('pso', ob)])
                            for ti, (vv, pp) in enumerate(terms):
                                p.mm(psz[:, ob, qi * 128:(qi + 1) * 128], cx.onesB[:], pp, ti == 0, ti == len(terms) - 1,
                                     reads=[P, PC], writes=[('psz', ob)])
                        n = nq * 128
                        R = ('rc', ob)
                        p.add('dve', lambda e, ob=ob, n=n, h=h: e.tensor_scalar(out=rc[:, ob, 0:n], in0=psz[:, ob, 0:n], scalar1=es[:, h:h + 1], scalar2=None, op0=ALU.add),
                              reads=[('psz', ob), 'es'], writes=[R])
                        p.add('dve', lambda e, ob=ob, n=n: e.reciprocal(out=rc[:, ob, 0:n], in_=rc[:, ob, 0:n]), reads=[R], writes=[R])
                        p.add('dve', lambda e, ob=ob, n=n, qs=qs, qg=qg: e.tensor_tensor(out=ot[:, qs, qg * 128:qg * 128 + n], in0=pso[:, ob, 0:n], in1=rc[:, ob, 0:n], op=ALU.mult),
                              reads=[('pso', ob), R], writes=[O])
                    p.store(cx.oT[h * 128:(h + 1) * 128, t0:t0 + TB], ot[:, qs, :], reads=[O])
        p.emit()


def phase_attn_b(cx, l):
    nc = cx.nc
    p = Prog(cx.gl)
    S, C, TB = cx.S, cx.C, cx.TB
    NKB = TB // 128
    SC = HD ** -0.5
    lam_init = 0.8 - 0.6 * math.exp(-0.3 * l)
    from contextlib import ExitStack
    with ExitStack() as es_:
        kt = es_.enter_context(nc.sbuf_tensor(U("ab_k"), [128, 2, 2, TB], BF16))
        qt = es_.enter_context(nc.sbuf_tensor(U("ab_q"), [128, 2, 2, TB], BF16))
        swp = es_.enter_context(nc.sbuf_tensor(U("ab_sw"), [128, 2, S], F32))
        tmp = es_.enter_context(nc.sbuf_tensor(U("ab_tm"), [128, 2, S], F32))
        swb = es_.enter_context(nc.sbuf_tensor(U("ab_swb"), [128, 2, S], BF16))
        vt = es_.enter_context(nc.sbuf_tensor(U("ab_v"), [128, 2, NKB, 256], BF16))
        pt_ = es_.enter_context(nc.sbuf_tensor(U("ab_p"), [128, 4, 512], BF16))
        o32 = es_.enter_context(nc.sbuf_tensor(U("ab_o"), [128, 2, 2, 512], F32))
        ob16 = es_.enter_context(nc.sbuf_tensor(U("ab_ob"), [128, 2, 2, 512], BF16))
        rc = es_.enter_context(nc.sbuf_tensor(U("ab_r"), [128, 2, 512], F32))
        sq = es_.enter_context(nc.sbuf_tensor(U("ab_sq"), [128, 2, 512], F32))
        lm = es_.enter_context(nc.sbuf_tensor(U("ab_l"), [128, 8], F32))
        sg = es_.enter_context(nc.sbuf_tensor(U("ab_sg"), [128, 2], F32))
        pss = es_.enter_context(nc.psum_tensor(U("ab_s"), [128, 2, 512], F32))
        pso = es_.enter_context(nc.psum_tensor(U("ab_po"), [128, 4, 512], F32))
        psz = es_.enter_context(nc.psum_tensor(U("ab_pz"), [128, 2, 512], F32))
        p.load(lm[:, 0:4], cx.b_lamT, writes=['lm'])
        p.load(sg[:], cx.b_sgT, writes=['sg'])
        p.add('dve', lambda e: e.tensor_tensor(out=lm[:, 4:5], in0=lm[:, 0:1], in1=lm[:, 1:2], op=ALU.mult), reads=['lm'], writes=['lm'])
        p.add('dve', lambda e: e.tensor_tensor(out=lm[:, 5:6], in0=lm[:, 2:3], in1=lm[:, 3:4], op=ALU.mult), reads=['lm'], writes=['lm'])
        p.mm(psz[:, 0, 0:2], cx.onesF[:], lm[:, 4:6], True, True, reads=['lm'], writes=[('psz', 0)])
        p.add('act', lambda e: e.activation(out=lm[:, 6:8], in_=psz[:, 0, 0:2], func=AF.Exp), reads=[('psz', 0)], writes=['lm'])
        p.add('dve', lambda e: e.tensor_tensor(out=lm[:, 6:7], in0=lm[:, 7:8], in1=lm[:, 6:7], op=ALU.subtract), reads=['lm'], writes=['lm'])
        p.add('dve', lambda e: e.tensor_scalar(out=lm[:, 6:7], in0=lm[:, 6:7], scalar1=-lam_init, scalar2=None, op0=ALU.add), reads=['lm'], writes=['lm'])
        p.add('dve', lambda e: e.tensor_scalar(out=sg[:], in0=sg[:], scalar1=1.0 - lam_init, scalar2=None, op0=ALU.mult), reads=['sg'], writes=['sg'])
        si = 0
        pi = 0
        for b in range(cx.B):
            t0 = b * TB
            for h in range(16):
                ks = (b * 16 + h) % 2
                K = ('k', ks)
                Q = ('q', ks)
                V = ('v', ks)
                for c in range(2):
                    krows = cx.qkvT[4096 + (h * 2 + c) * 128:4096 + (h * 2 + c + 1) * 128, :]
                    rope_load(cx, p, kt[:, ks, c, 0:S], swb[:, 0, :], swp[:, 0, :], tmp[:, 0, :], (K, c), krows, t0, S, 0)
                    p.load(kt[:, ks, c, S:TB], krows[:, t0 + S:t0 + TB], writes=[(K, c)])
                    qrows = cx.qkvT[(h * 2 + c) * 128:(h * 2 + c + 1) * 128, :]
                    rope_load(cx, p, qt[:, ks, c, 0:S], swb[:, 1, :], swp[:, 1, :], tmp[:, 1, :], (Q, c), qrows, t0, S, 0)
                    p.load(qt[:, ks, c, S:TB], qrows[:, t0 + S:t0 + TB], writes=[(Q, c)])
                p.load(vt[:, ks], cx.vN[t0:t0 + TB, h * 256:(h + 1) * 256].rearrange("(n p) d -> p n d", p=128), writes=[V])
                chunks = [(q0, 512, 0, NKB) for q0 in range(0, S, 512)] + [(S, C, S // 128, NKB)]
                for (q0, n, kb0, kb1) in chunks:
                    nk = kb1 - kb0
                    for ki, kb in enumerate(range(kb0, kb1)):
                        for c in range(2):
                            sb = si % 2
                            si += 1
                            ps_ = pi % 4
                            pi += 1
                            p.mm(pss[:, sb, 0:n], kt[:, ks, c, kb * 128:(kb + 1) * 128], qt[:, ks, c, q0:q0 + n], True, True,
                                 reads=[(K, c), (Q, c)], writes=[('pss', sb)])
                            p.add('act', lambda e, sb=sb, n=n, ps_=ps_: e.activation(out=pt_[:, ps_, 0:n], in_=pss[:, sb, 0:n], func=AF.Exp, scale=SC),
                                  reads=[('pss', sb)], writes=[('p', ps_)])
                            for et in range(2):
                                p.mm(pso[:, c * 2 + et, 0:n], vt[:, ks, kb, et * 128:(et + 1) * 128], pt_[:, ps_, 0:n], ki == 0, ki == nk - 1,
                                     reads=[V, ('p', ps_)], writes=[('pso', c * 2 + et)])
                            p.mm(psz[:, c, 0:n], cx.onesB[:], pt_[:, ps_, 0:n], ki == 0, ki == nk - 1,
                                 reads=[('p', ps_)], writes=[('psz', c)])
                    for c in range(2):
                        p.add('dve', lambda e, c=c, n=n: e.reciprocal(out=rc[:, c, 0:n], in_=psz[:, c, 0:n]), reads=[('psz', c)], writes=[('rc', c)])
                    p.add('dve', lambda e, n=n: e.tensor_scalar(out=rc[:, 1, 0:n], in0=rc[:, 1, 0:n], scalar1=lm[:, 6:7], scalar2=None, op0=ALU.mult),
                          reads=[('rc', 1), 'lm'], writes=[('rc', 1)])
                    for et in range(2):
                        p.add('dve', lambda e, et=et, n=n: e.tensor_tensor(out=o32[:, 0, et, 0:n], in0=pso[:, et, 0:n], in1=rc[:, 0, 0:n], op=ALU.mult),
                              reads=[('pso', et), ('rc', 0)], writes=[('o32', 0, et)])
                        p.add('dve', lambda e, et=et, n=n: e.tensor_tensor(out=o32[:, 1, et, 0:n], in0=pso[:, 2 + et, 0:n], in1=rc[:, 1, 0:n], op=ALU.mult),
                              reads=[('pso', 2 + et), ('rc', 1)], writes=[('o32', 1, et)])
                        p.add('pool', lambda e, et=et, n=n: e.tensor_tensor(out=o32[:, 0, et, 0:n], in0=o32[:, 0, et, 0:n], in1=o32[:, 1, et, 0:n], op=ALU.add),
                              reads=[('o32', 0, et), ('o32', 1, et)], writes=[('o32', 0, et)])
                        p.add('act', lambda e, et=et, n=n: e.activation(out=sq[:, et, 0:n], in_=o32[:, 0, et, 0:n], func=AF.Square),
                              reads=[('o32', 0, et)], writes=[('sq', et)])
                    sb = si % 2
                    si += 1
                    for et in range(2):
                        p.mm(pss[:, sb, 0:n], cx.onesF[:], sq[:, et, 0:n], et == 0, et == 1, reads=[('sq', et)], writes=[('pss', sb)])
                    p.add('dve', lambda e, sb=sb, n=n: e.tensor_scalar(out=rc[:, 0, 0:n], in0=pss[:, sb, 0:n], scalar1=1.0 / 256, scalar2=EPS, op0=ALU.mult, op1=ALU.add),
                          reads=[('pss', sb)], writes=[('rc', 0)])
                    p.add('act', lambda e, n=n: e.activation(out=rc[:, 0, 0:n], in_=rc[:, 0, 0:n], func=AF.Sqrt), reads=[('rc', 0)], writes=[('rc', 0)])
                    p.add('dve', lambda e, n=n: e.reciprocal(out=rc[:, 0, 0:n], in_=rc[:, 0, 0:n]), reads=[('rc', 0)], writes=[('rc', 0)])
                    osl = (q0 // 512) % 2
                    for et in range(2):
                        p.add('dve', lambda e, et=et, n=n, osl=osl: e.scalar_tensor_tensor(out=ob16[:, osl, et, 0:n], in0=o32[:, 0, et, 0:n], scalar=sg[:, et:et + 1],
                                                                                         in1=rc[:, 0, 0:n], op0=ALU.mult, op1=ALU.mult),
                              reads=[('o32', 0, et), ('rc', 0), 'sg'], writes=[('ob', osl, et)])
                        p.store(cx.oT[h * 256 + et * 128:h * 256 + (et + 1) * 128, t0 + q0:t0 + q0 + n], ob16[:, osl, et, 0:n], reads=[('ob', osl, et)])
        p.emit()


def phase_conv(cx):
    nc = cx.nc
    p = Prog(cx.gl)
    S, C, TB = cx.S, cx.C, cx.TB
    LM = S
    with (nc.sbuf_tensor(U("cv_in"), [128, 2, 3, LM], BF16) as xin, nc.sbuf_tensor(U("cv_v"), [128, 2, LM + 2], F32) as vv,
          nc.sbuf_tensor(U("cv_z"), [128, 2, LM], F32) as zz, nc.sbuf_tensor(U("cv_m"), [128, 2, LM], BF16) as mm_,
          nc.sbuf_tensor(U("cv_w"), [128, KC, 3], F32) as cw):
        p.load(cw[:], cx.c_convT, writes=['cw'])
        it = 0
        for c in range(KC):
            for b in range(cx.B):
                for (o0, L) in ((0, S), (S, C)):
                    s = it % 2
                    it += 1
                    t0 = b * TB + o0
                    I = ('in', s)
                    for w3 in range(3):
                        p.load(xin[:, s, w3, 0:L], cx.qkvT[w3 * 4096 + c * 128:w3 * 4096 + (c + 1) * 128, t0:t0 + L], writes=[I])
                    Vk = ('v', s)
                    p.add('pool', lambda e, s=s, L=L: e.memset(vv[:, s, 0:1], 0.0), writes=[Vk])
                    p.add('pool', lambda e, s=s, L=L: e.memset(vv[:, s, L + 1:L + 2], 0.0), writes=[Vk])
                    p.add('pool', lambda e, s=s, L=L: e.tensor_tensor(out=vv[:, s, 1:L + 1], in0=xin[:, s, 1, 0:L], in1=xin[:, s, 2, 0:L], op=ALU.mult),
                          reads=[I], writes=[Vk])
                    Z = ('z', s)
                    p.add('dve', lambda e, s=s, L=L, c=c: e.tensor_scalar(out=zz[:, s, 0:L], in0=vv[:, s, 0:L], scalar1=cw[:, c, 0:1], scalar2=None, op0=ALU.mult),
                          reads=[Vk, 'cw'], writes=[Z])
                    p.add('dve', lambda e, s=s, L=L, c=c: e.scalar_tensor_tensor(out=zz[:, s, 0:L], in0=vv[:, s, 1:L + 1], scalar=cw[:, c, 1:2], in1=zz[:, s, 0:L],
                                                                                 op0=ALU.mult, op1=ALU.add), reads=[Vk, 'cw', Z], writes=[Z])
                    p.add('dve', lambda e, s=s, L=L, c=c: e.scalar_tensor_tensor(out=zz[:, s, 0:L], in0=vv[:, s, 2:L + 2], scalar=cw[:, c, 2:3], in1=zz[:, s, 0:L],
                                                                                 op0=ALU.mult, op1=ALU.add), reads=[Vk, 'cw', Z], writes=[Z])
                    M = ('m', s)
                    p.add('pool', lambda e, s=s, L=L: e.tensor_tensor(out=mm_[:, s, 0:L], in0=zz[:, s, 0:L], in1=xin[:, s, 0, 0:L], op=ALU.mult),
                          reads=[Z, I], writes=[M])
                    p.store(cx.oT[c * 128:(c + 1) * 128, t0:t0 + L], mm_[:, s, 0:L], reads=[M])
        p.emit()


def phase_convert_pairs(cx, L, exp_gate, exp_up, exp_down):
    nc = cx.nc
    p = Prog(cx.gl)
    NS = 2
    W = 2 * KC * FF
    H = KC * FF
    with nc.sbuf_tensor(U("cp_s"), [128, NS, W], F32) as st, nc.sbuf_tensor(U("cp_b"), [128, NS, W], BF16) as bt:
        i = 0
        for l in range(L):
            for pr in range(NE // 2):
                for (src, dst) in ((exp_gate, cx.img_eg2), (exp_up, cx.img_eu2)):
                    s = i % NS
                    i += 1
                    bv = bt[:, s, :].rearrange("p (c x) -> p c x", x=384)
                    for h in range(2):
                        sv = st[:, s, h * H:(h + 1) * H].rearrange("p (c f) -> p c f", f=FF)
                        p.load(sv, src[l, 2 * pr + h].rearrange("(c p) f -> p c f", p=128), writes=[('cs', s, h)])
                        lo_o, lo_i = bv[:, :, h * 128:(h + 1) * 128], sv[:, :, 0:128]
                        hi_o, hi_i = bv[:, :, 256 + h * 64:256 + (h + 1) * 64], sv[:, :, 128:192]
                        if h == 0:
                            p.add('dve', lambda e, o=lo_o, x=lo_i: e.tensor_copy(out=o, in_=x), reads=[('cs', s, h)], writes=[('cb', s, h, 0)])
                            p.add('dve', lambda e, o=hi_o, x=hi_i: e.tensor_copy(out=o, in_=x), reads=[('cs', s, h)], writes=[('cb', s, h, 1)])
                        else:
                            p.add('act', lambda e, o=lo_o, x=lo_i: e.activation(out=o, in_=x, func=AF.Copy), reads=[('cs', s, h)], writes=[('cb', s, h, 0)])
                            p.add('act', lambda e, o=hi_o, x=hi_i: e.activation(out=o, in_=x, func=AF.Copy), reads=[('cs', s, h)], writes=[('cb', s, h, 1)])
                    p.store(dst[l][pr], bt[:, s, :], reads=[('cb', s, 0, 0), ('cb', s, 0, 1), ('cb', s, 1, 0), ('cb', s, 1, 1)])
                s = i % NS
                i += 1
                p.load(st[:, s, 0:D], exp_down[l, 2 * pr][0:128, :], writes=[('cs', s, 0)])
                p.load(st[:, s, D:2 * D], exp_down[l, 2 * pr + 1][0:128, :], writes=[('cs', s, 1)])
                p.load(st[0:64, s, 2 * D:3 * D], exp_down[l, 2 * pr][128:192, :], writes=[('cs', s, 2)])
                p.load(st[64:128, s, 2 * D:3 * D], exp_down[l, 2 * pr + 1][128:192, :], writes=[('cs', s, 3)])
                p.add('dve', lambda e, o=bt[:, s, 0:H], x=st[:, s, 0:H]: e.tensor_copy(out=o, in_=x),
                      reads=[('cs', s, 0), ('cs', s, 1)], writes=[('cb', s, 0, 0), ('cb', s, 0, 1)])
                p.add('act', lambda e, o=bt[:, s, H:W], x=st[:, s, H:W]: e.activation(out=o, in_=x, func=AF.Copy),
                      reads=[('cs', s, 1), ('cs', s, 2), ('cs', s, 3)], writes=[('cb', s, 1, 0), ('cb', s, 1, 1)])
                p.store(cx.img_ed2[l][pr], bt[:, s, :], reads=[('cb', s, 0, 0), ('cb', s, 0, 1), ('cb', s, 1, 0), ('cb', s, 1, 1)])
        p.emit()


def phase_moe(cx, l):
    nc = cx.nc
    p = Prog(cx.gl)
    mv = cx.modv
    NGR = cx.NT // G
    NP = NE // 2
    W = 2 * KC * FF
    from contextlib import ExitStack
    with ExitStack() as es_:
        E = es_.enter_context
        ft = E(nc.sbuf_tensor(U("mo_f"), [128, KC, G], BF16))
        acc = E(nc.sbuf_tensor(U("mo_acc"), [128, KC, G], F32))
        wg = E(nc.sbuf_tensor(U("mo_wg"), [128, W], BF16))
        wu = E(nc.sbuf_tensor(U("mo_wu"), [128, W], BF16))
        wd = E(nc.sbuf_tensor(U("mo_wd"), [128, W], BF16))
        gt = E(nc.sbuf_tensor(U("mo_gt"), [64, G], BF16))
        sel3 = E(nc.sbuf_tensor(U("mo_sel"), [64, NP, 3, 128], BF16))
        sil = E(nc.sbuf_tensor(U("mo_sil"), [128, 3, G], F32))
        t1 = E(nc.sbuf_tensor(U("mo_t1"), [128, 2, G], F32))
        act = E(nc.sbuf_tensor(U("mo_act"), [128, 2, 3, G], BF16))
        xs = E(nc.sbuf_tensor(U("mo_xs"), [128, 2, G], F32))
        hg = E(nc.psum_tensor(U("mo_hg"), [128, 2, 512], F32))
        hu = E(nc.psum_tensor(U("mo_hu"), [128, 2, 512], F32))
        gb = E(nc.psum_tensor(U("mo_gb"), [128, 512], F32))
        dn = E(nc.psum_tensor(U("mo_dn"), [128, 3, 512], F32))
        identv = cx.identB[0:64, 0:64].rearrange("k (q j) -> k q j", j=2)
        p.add('dve', lambda e: e.tensor_copy(out=sel3[:, :, 0:2, :], in_=identv.unsqueeze(3).to_broadcast([64, NP, 2, 128])), writes=['sel'])
        p.add('dve', lambda e: e.tensor_copy(out=sel3[:, :, 2, :].rearrange("k q (j m) -> k q j m", j=2),
                                             in_=identv.unsqueeze(3).to_broadcast([64, NP, 2, 64])), writes=['sel'])
        it = 0
        di = 0
        xi = 0
        hi_ = 0
        ui_ = 0
        ti_ = 0
        for g in range(NGR):
            p.load(ft[:], cx.aT[:, g * G:(g + 1) * G].rearrange("(c p) t -> p c t", p=128), writes=['ft'])
            p.load(gt[:], cx.gT[:, g * G:(g + 1) * G], writes=['gt'])
            for pr in range(NP + 1):
                shared = (pr == NP)
                s = it % 2
                it += 1
                if not shared:
                    p.load(wg[:], cx.img_eg2[l][pr], writes=['wg'])
                    p.load(wu[:], cx.img_eu2[l][pr], writes=['wu'])
                    p.store(wd[:], cx.img_ed2[l][pr], writes=['wd'])
                    tiles = [(0, 128), (1, 128), (2, 128)]
                else:
                    p.load(wg[:, 0:KC * FF], cx.img_sg[l], writes=['wg'])
                    p.load(wu[:, 0:KC * FF], cx.img_su[l], writes=['wu'])
                    p.store(wd[:, 0:2 * D], cx.img_sd[l], writes=['wd'])
                    tiles = [(0, 128), (1, 64)]
                for (wt_, ps_, nm, wk) in ((wg, hg, 'hg', 'wg'), (wu, hu, 'hu', 'wu')):
                    for (mt, rows) in tiles:
                        if nm == 'hg':
                            pb = hi_ % 2
                            hi_ += 1
                        else:
                            pb = ui_ % 2
                            ui_ += 1
                        for k in range(KC):
                            if not shared:
                                lw = wt_[:, k * 384 + mt * 128:k * 384 + (mt + 1) * 128]
                            else:
                                lw = wt_[:, k * FF + mt * 128:k * FF + mt * 128 + rows]
                            p.mm(ps_[0:rows, pb, 0:G], lw, ft[:, k, :], k == 0, k == KC - 1, reads=[wk, 'ft'], writes=[(nm, pb)])
                        if nm == 'hg':
                            p.add('act', lambda e, mt=mt, rows=rows, pb=pb: e.activation(out=sil[0:rows, mt, :], in_=hg[0:rows, pb, 0:G], func=AF.Silu),
                                  reads=[('hg', pb)], writes=[('sil', mt)])
                        elif not shared:
                            p.mm(gb[:, 0:G], sel3[:, pr, mt, :], gt[:, :], True, True, reads=['sel', 'gt'], writes=['gb'])
                            t = ti_ % 2
                            ti_ += 1
                            p.add('dve', lambda e, mt=mt, pb=pb, t=t: e.tensor_tensor(out=t1[:, t, :], in0=sil[:, mt, :], in1=hu[:, pb, 0:G], op=ALU.mult),
                                  reads=[('sil', mt), ('hu', pb)], writes=[('t1', t)])
                            p.add('dve', lambda e, mt=mt, t=t, s=s: e.tensor_tensor(out=act[:, s, mt, :], in0=t1[:, t, :], in1=gb[:, 0:G], op=ALU.mult),
                                  reads=[('t1', t), 'gb'], writes=[('act', s, mt)])
                        else:
                            p.add('dve', lambda e, mt=mt, rows=rows, pb=pb, s=s: e.tensor_tensor(out=act[0:rows, s, mt, :], in0=sil[0:rows, mt, :], in1=hu[0:rows, pb, 0:G], op=ALU.mult),
                                  reads=[('sil', mt), ('hu', pb)], writes=[('act', s, mt)])
                for c in range(KC):
                    db = di % 3
                    di += 1
                    if not shared:
                        for mt in range(3):
                            p.mm(dn[:, db, 0:G], wd[:, mt * D + c * 128:mt * D + (c + 1) * 128], act[:, s, mt, :], mt == 0, mt == 2,
                                 reads=['wd', ('act', s, mt)], writes=[('dn', db)])
                    else:
                        p.mm(dn[:, db, 0:G], wd[:, c * 128:(c + 1) * 128], act[:, s, 0, :], True, False, reads=['wd', ('act', s, 0)], writes=[('dn', db)])
                        p.mm(dn[:, db, 0:G], wd[0:64, D + c * 128:D + (c + 1) * 128], act[0:64, s, 1, :], False, True, reads=['wd', ('act', s, 1)], writes=[('dn', db)])
                    if pr == 0:
                        p.add('dve', lambda e, c=c, db=db: e.tensor_copy(out=acc[:, c, :], in_=dn[:, db, 0:G]), reads=[('dn', db)], writes=[('acc', c)])
                    else:
                        p.add('dve', lambda e, c=c, db=db: e.tensor_tensor(out=acc[:, c, :], in0=acc[:, c, :], in1=dn[:, db, 0:G], op=ALU.add),
                              reads=[('dn', db), ('acc', c)], writes=[('acc', c)])
            for c in range(KC):
                s = xi % 2
                xi += 1
                key = ('xs', s)
                xa = cx.xT[c * 128:(c + 1) * 128, g * G:(g + 1) * G]
                p.load(xs[:, s, :], xa, writes=[key])
                for (c0, ncol, r) in segs_of_group(cx, g):
                    p.add('dve', lambda e, s=s, c=c, c0=c0, ncol=ncol, r=r: e.scalar_tensor_tensor(
                        out=xs[:, s, c0:c0 + ncol], in0=acc[:, c, c0:c0 + ncol], scalar=mv[:, 5, c, r:r + 1], in1=xs[:, s, c0:c0 + ncol],
                        op0=ALU.mult, op1=ALU.add), reads=[('acc', c), key, 'mv'], writes=[key])
                p.store(xa, xs[:, s, :], reads=[key])
        p.emit()


def build(B, S, layers, n_a, n_b, n_c):
    L = len(layers)
    nc = bass.Bass("TRN2", target_bir_lowering=False)
    cx = Ctx()
    cx.nc = nc
    cx.B, cx.S, cx.C = B, S, CTX
    cx.TB = S + CTX
    cx.NT = B * cx.TB
    NT = cx.NT
    assert cx.TB % G == 0 and NT % (3 * G) == 0

    kinds = [k for (k, j) in layers]
    cx.in_names = []

    def inp(name, shape, need=True):
        if not need:
            return None
        cx.in_names.append(name)
        return nc.dram_tensor(name, list(shape), F32, kind="ExternalInput").ap()

    def scr(name, shape, dt):
        return nc.dram_tensor(name, list(shape), dt).ap()

    xT0 = inp("xT0", [D, NT])
    cx.csT = inp("csT", [D, B + 1])
    ada_down = inp("ada_down", [L, D, 512])
    ada_up = inp("ada_up", [L, 512, 6 * D])
    cx.ada_bT = inp("ada_bT", [L, 128, 192])
    cx.n1gT = inp("n1gT", [L, 128, KC])
    cx.n2gT = inp("n2gT", [L, 128, KC])
    cx.fgT = inp("fgT", [128, KC])
    a_w_qkv = inp("a_w_qkv", [max(n_a, 1), D, 6144], 0 in kinds)
    a_w_o = inp("a_w_o", [max(n_a, 1), D, D], 0 in kinds)
    cx.a_sink = inp("a_sink", [max(n_a, 1), 32], 0 in kinds)
    b_w_qkv = inp("b_w_qkv", [max(n_b, 1), D, 3 * D], 1 in kinds)
    b_w_o = inp("b_w_o", [max(n_b, 1), D, D], 1 in kinds)
    cx.b_lamT = inp("b_lamT", [128, 4], 1 in kinds)
    cx.b_sgT = inp("b_sgT", [128, 2], 1 in kinds)
    c_w_in = inp("c_w_in", [max(n_c, 1), D, 3 * D], 2 in kinds)
    cx.c_convT = inp("c_convT", [128, KC, 3], 2 in kinds)
    c_w_out = inp("c_w_out", [max(n_c, 1), D, D], 2 in kinds)
    cx.router_w = inp("router_w", [L, D, NE])
    cx.router_b = inp("router_b", [L, NE])
    exp_gate = inp("exp_gate", [L, NE, D, FF])
    exp_up = inp("exp_up", [L, NE, D, FF])
    exp_down = inp("exp_down", [L, NE, FF, D])
    sh_gate = inp("sh_gate", [L, D, FF])
    sh_up = inp("sh_up", [L, D, FF])
    sh_down = inp("sh_down", [L, FF, D])
    identD = inp("ident", [128, 128])
    cos2D = inp("cos2", [128, S])
    sin2D = inp("sin2", [128, S])
    wmaskD = inp("wmask", [128, 384])
    cx.outT = nc.dram_tensor("outT", [D, NT], F32, kind="ExternalOutput").ap()

    cx.xT = scr("xT", [D, NT], F32)
    cx.aT = scr("aT", [D, NT], BF16)
    cx.gT = scr("gT", [NE, NT], BF16)
    cx.qkvT = scr("qkvT", [3 * D, NT], BF16)
    cx.vN = scr("vN", [NT, D], BF16)
    cx.oT = scr("oT", [D, NT], BF16)
    cx.img = {}
    jobs = []

    def img_proj(name, src, nl, K, N, slabw):
        kc = K // 128
        nslab = N // slabw
        im = scr("im_" + name, [nl, nslab, 128, kc * slabw], BF16)
        cx.img[name] = im
        for l_ in range(nl):
            v = src[l_].rearrange("(c p) n -> p c n", p=128)
            for s in range(nslab):
                jobs.append((v[:, :, s * slabw:(s + 1) * slabw], im[l_, s], 128, kc, slabw))

    kinds = [k for (k, j) in layers]
    img_proj('ada_down', ada_down, L, D, 512, 512)
    img_proj('ada_up', ada_up, L, 512, 6 * D, 2048)
    if 0 in kinds:
        img_proj('a_w_qkv', a_w_qkv, n_a, D, 6144, 512)
        img_proj('a_w_o', a_w_o, n_a, D, D, 512)
    if 1 in kinds:
        img_proj('b_w_qkv', b_w_qkv, n_b, D, 3 * D, 512)
        img_proj('b_w_o', b_w_o, n_b, D, D, 512)
    if 2 in kinds:
        img_proj('c_w_in', c_w_in, n_c, D, 3 * D, 512)
        img_proj('c_w_out', c_w_out, n_c, D, D, 512)
    cx.img_eg2 = [scr("im_eg%d" % l_, [NE // 2, 128, 2 * KC * FF], BF16) for l_ in range(L)]
    cx.img_eu2 = [scr("im_eu%d" % l_, [NE // 2, 128, 2 * KC * FF], BF16) for l_ in range(L)]
    cx.img_ed2 = [scr("im_ed%d" % l_, [NE // 2, 128, 3 * D], BF16) for l_ in range(L)]
    cx.img_sg = [scr("im_sg%d" % l_, [128, KC * FF], BF16) for l_ in range(L)]
    cx.img_su = [scr("im_su%d" % l_, [128, KC * FF], BF16) for l_ in range(L)]
    cx.img_sd = [scr("im_sd%d" % l_, [128, 2 * D], BF16) for l_ in range(L)]
    for l_ in range(L):
        jobs.append((sh_gate[l_].rearrange("(c p) f -> p c f", p=128), cx.img_sg[l_], 128, KC, FF))
        jobs.append((sh_up[l_].rearrange("(c p) f -> p c f", p=128), cx.img_su[l_], 128, KC, FF))
        jobs.append((sh_down[l_][0:128, :].rearrange("p (a n) -> p a n", a=1), cx.img_sd[l_][:, 0:D], 128, 1, D))
        jobs.append((sh_down[l_][128:192, :].rearrange("p (a n) -> p a n", a=1), cx.img_sd[l_][0:64, D:2 * D], 64, 1, D))

    from contextlib import ExitStack
    with ExitStack() as stack:
        cx.gl = Glob(nc, stack)
        E = stack.enter_context
        cx.modv = E(nc.sbuf_tensor(U("modv"), [128, 6, KC, B + 1], F32))
        cx.ident = E(nc.sbuf_tensor(U("identS"), [128, 128], F32))
        cx.identB = E(nc.sbuf_tensor(U("identB"), [128, 128], BF16))
        cx.onesF = E(nc.sbuf_tensor(U("onesF"), [128, 128], F32))
        cx.onesB = E(nc.sbuf_tensor(U("onesB"), [128, 128], BF16))
        cx.cos2 = E(nc.sbuf_tensor(U("cos2S"), [128, S], F32))
        cx.sin2 = E(nc.sbuf_tensor(U("sin2S"), [128, S], F32))
        cx.wmask = E(nc.sbuf_tensor(U("wmaskS"), [128, 384], BF16))
        wm32 = E(nc.sbuf_tensor(U("wm32"), [128, 384], F32))
        p = Prog(cx.gl)
        p.load(cx.ident[:], identD, writes=['id'])
        p.load(cx.cos2[:], cos2D, writes=['cs'])
        p.load(cx.sin2[:], sin2D, writes=['cs'])
        p.load(wm32[:], wmaskD, writes=['wm'])
        p.add('dve', lambda e: e.tensor_copy(out=cx.identB[:], in_=cx.ident[:]), reads=['id'], writes=['idb'])
        p.add('dve', lambda e: e.tensor_copy(out=cx.wmask[:], in_=wm32[:]), reads=['wm'], writes=['wmb'])
        p.add('dve', lambda e: e.memset(cx.onesF[:], 1.0), writes=['of'])
        p.add('dve', lambda e: e.memset(cx.onesB[:], 1.0), writes=['ob'])
        with nc.sbuf_tensor(U("in_x"), [128, 2, 8192], F32) as xb:
            i = 0
            for r0 in range(0, D, 128):
                for c0 in range(0, NT, 8192):
                    n = min(8192, NT - c0)
                    s = i % 2
                    i += 1
                    p.load(xb[:, s, 0:n], xT0[r0:r0 + 128, c0:c0 + n], writes=[('xb', s)])
                    p.store(cx.xT[r0:r0 + 128, c0:c0 + n], xb[:, s, 0:n], reads=[('xb', s)])
            p.emit()
        phase_convert(cx, jobs)
        phase_convert_pairs(cx, L, exp_gate, exp_up, exp_down)
        for li, (kind, j) in enumerate(layers):
            if DEBUG_MODE == 'convert':
                break
            phase_mod(cx, li)
            phase_norm(cx, li, 1)
            if kind == 0:
                modes = ['T'] * 10 + ['N'] * 2
                run_proj_store(cx, cx.aT, cx.img['a_w_qkv'][j], 12, outT=cx.qkvT, outN=cx.vN, modes=modes,
                               col_of_slab=lambda s: (s - 10) * 512)
                phase_attn_a(cx, j)
                run_proj_store(cx, cx.oT, cx.img['a_w_o'][j], 8, resid_gate=2)
            elif kind == 1:
                modes = ['T'] * 16 + ['N'] * 8
                run_proj_store(cx, cx.aT, cx.img['b_w_qkv'][j], 24, outT=cx.qkvT, outN=cx.vN, modes=modes,
                               col_of_slab=lambda s: (s - 16) * 512)
                phase_attn_b(cx, cx.layer_ids[li])
                run_proj_store(cx, cx.oT, cx.img['b_w_o'][j], 8, resid_gate=2)
            else:
                run_proj_store(cx, cx.aT, cx.img['c_w_in'][j], 24, outT=cx.qkvT)
                phase_conv(cx)
                run_proj_store(cx, cx.oT, cx.img['c_w_out'][j], 8, resid_gate=2)
            if DEBUG_MODE == 'nomoe':
                continue
            phase_norm(cx, li, 2)
            phase_moe(cx, li)
        phase_norm(cx, 0, 1, final=True)
    cx.n_ins = cx.gl.n_ins
    return nc, cx


def rope_tables(S):
    rows = S // 64
    row = np.repeat(np.arange(rows), 64).astype(np.float32)
    col = np.tile(np.arange(64), rows).astype(np.float32)
    n_freq = HD // 4
    inv = (10000.0 ** (-np.arange(n_freq, dtype=np.float32) / n_freq)).astype(np.float32)
    ang = np.concatenate([row[:, None] * inv, col[:, None] * inv], axis=-1)
    cos, sin = np.cos(ang).T.astype(np.float32), np.sin(ang).T.astype(np.float32)
    cos2 = np.concatenate([cos, cos], axis=0)
    sin2 = np.concatenate([-sin, sin], axis=0)
    return np.ascontiguousarray(cos2), np.ascontiguousarray(sin2)


def window_mask():
    i = np.arange(128)[:, None]
    jj = np.arange(128)[None, :]
    m = np.concatenate([(i <= jj), np.ones((128, 128), bool), (jj <= i)], axis=1)
    return m.astype(np.float32)


def prep_shared(inputs, layers, layer_ids, S):
    f = lambda a: np.ascontiguousarray(np.asarray(a, dtype=np.float32))
    li = list(layer_ids)

    def pick(a):
        a = f(a)
        return a if li == list(range(a.shape[0])) else np.ascontiguousarray(a[li])
    pm = lambda a: np.ascontiguousarray(a.reshape(a.shape[0], -1, 128).transpose(0, 2, 1))
    m = {
        'ada_down': pick(inputs['ada_down']), 'ada_up': pick(inputs['ada_up']),
        'ada_bT': pm(pick(inputs['ada_b'])), 'n1gT': pm(pick(inputs['norm1_g'])), 'n2gT': pm(pick(inputs['norm2_g'])),
        'fgT': pm(f(inputs['final_g'])[None])[0],
        'a_w_qkv': f(inputs['a_w_qkv']), 'a_w_o': f(inputs['a_w_o']), 'a_sink': f(inputs['a_sink']),
        'b_w_qkv': f(inputs['b_w_qkv']), 'b_w_o': f(inputs['b_w_o']),
        'b_lamT': np.ascontiguousarray(f(inputs['b_lam'])[0].T), 'b_sgT': np.ascontiguousarray(f(inputs['b_subln_g'])[0].reshape(2, 128).T),
        'c_w_in': f(inputs['c_w_in']), 'c_convT': np.ascontiguousarray(f(inputs['c_conv'])[0].reshape(3, KC, 128).transpose(2, 1, 0)),
        'c_w_out': f(inputs['c_w_out']),
        'router_w': pick(inputs['router_w']), 'router_b': pick(inputs['router_b']),
        'exp_gate': pick(inputs['exp_gate']), 'exp_up': pick(inputs['exp_up']), 'exp_down': pick(inputs['exp_down']),
        'sh_gate': pick(inputs['sh_gate']), 'sh_up': pick(inputs['sh_up']), 'sh_down': pick(inputs['sh_down']),
        'ident': np.eye(128, dtype=np.float32), 'wmask': window_mask(),
    }
    m['cos2'], m['sin2'] = rope_tables(S)
    return m


def run(inputs, layer_ids, layers=None):
    f = lambda a: np.ascontiguousarray(np.asarray(a, dtype=np.float32))
    x, ctx, c, c_ctx = f(inputs['x']), f(inputs['ctx']), f(inputs['c']), f(inputs['c_ctx'])
    B, S, _ = x.shape
    if layers is None:
        layers = [(i % 3, i // 3) for i in layer_ids]
    n_a, n_b, n_c = inputs['a_w_qkv'].shape[0], inputs['b_w_qkv'].shape[0], inputs['c_w_in'].shape[0]
    Ctx.layer_ids = list(layer_ids)
    nc, cx = build(1, S, layers, n_a, n_b, n_c)
    shared = prep_shared(inputs, layers, layer_ids, S)
    maps = []
    for b in range(B):
        m = dict(shared)
        m['xT0'] = np.ascontiguousarray(np.concatenate([x[b], ctx[b]], axis=0).T)
        m['csT'] = np.ascontiguousarray(np.stack([c[b], c_ctx], axis=0).T)
        maps.append({k: v for k, v in m.items() if k in cx.in_names})
    res = run_bass_kernel_spmd(nc, maps, core_ids=list(range(B)))
    out = np.stack([res.results[b]["outT"].T[:S, :] for b in range(B)], axis=0)
    return np.ascontiguousarray(out.astype(np.float32))


def kernel(**inputs):
    return run(inputs, list(range(4)))
```

```python
import math
import numpy as np
import concourse.bass as bass
import concourse.mybir as mybir
from concourse.bass_utils import run_bass_kernel_spmd

F32 = mybir.dt.float32
BF16 = mybir.dt.bfloat16
ALU = mybir.AluOpType
AF = mybir.ActivationFunctionType
AX = mybir.AxisListType

D = 4096
KC = 32
CTX = 256
HD = 128
NE = 64
FF = 192
G = 384
EPS = 1e-6
ENGS = ('pe', 'act', 'dve', 'pool', 'sp')
BNAME = {'pe': 'tensor', 'act': 'scalar', 'dve': 'vector', 'pool': 'gpsimd', 'sp': 'sync'}
DMAK = 6
DEBUG_MODE = ''


class Op:
    __slots__ = ('eng', 'fn', 'deps', 'sig', 'val', 'sem', 'dma', 'prev')


class Glob:
    def __init__(self, nc, stack):
        self.nc = nc
        self.sem = {e: stack.enter_context(nc.semaphore("s_" + e)) for e in ENGS}
        self.cnt = {e: 0 for e in ENGS}
        self.dsem = {e: [stack.enter_context(nc.semaphore("d_%s%d" % (e, i))) for i in range(DMAK)]
                     for e in ('sp', 'pool')}
        self.dcnt = {e: [0] * DMAK for e in ('sp', 'pool')}
        self.di = {e: 0 for e in ('sp', 'pool')}
        self.dlast = {e: [None] * DMAK for e in ('sp', 'pool')}
        self.n_ins = 0


class Prog:
    def __init__(self, gl):
        self.gl = gl
        self.ops = {e: [] for e in ENGS}
        self.lastw = {}
        self.readers = {}
        self.nd = 0

    def add(self, eng, fn, reads=(), writes=(), dma=False):
        op = Op()
        op.eng = eng; op.fn = fn; op.sig = False; op.dma = dma; op.prev = None; op.sem = None; op.val = 0
        deps = {}
        for b in reads:
            w = self.lastw.get(b)
            if w is not None:
                deps[id(w)] = w
        for b in writes:
            w = self.lastw.get(b)
            if w is not None:
                deps[id(w)] = w
            rd = self.readers.get(b)
            if rd:
                for r in rd.values():
                    deps[id(r)] = r
        if dma:
            self.nd += 1
            rk = (eng, self.nd)
        else:
            rk = eng
        for b in reads:
            self.readers.setdefault(b, {})[rk] = op
        for b in writes:
            self.lastw[b] = op
            self.readers[b] = {}
        op.deps = [d for d in deps.values() if d is not op and not (d.eng == 'pe' and eng == 'pe')]
        self.ops[eng].append(op)
        return op

    def mm(self, out, lhsT, rhs, start, stop, reads, writes):
        return self.add('pe', lambda e: e.matmul(out, lhsT=lhsT, rhs=rhs, start=start, stop=stop), reads, writes)

    def load(self, out, in_, reads=(), writes=()):
        return self.add('sp', lambda e: e.dma_start(out=out, in_=in_), reads, writes, dma=True)

    def store(self, out, in_, reads=(), writes=()):
        return self.add('pool', lambda e: e.dma_start(out=out, in_=in_), reads, writes, dma=True)

    def emit(self):
        gl = self.gl
        nc = gl.nc
        bar = [(gl.sem[e], gl.cnt[e]) for e in ENGS if gl.cnt[e] > 0]
        for e in ('sp', 'pool'):
            for i in range(DMAK):
                if gl.dcnt[e][i] > 0:
                    bar.append((gl.dsem[e][i], gl.dcnt[e][i]))
        for e in ENGS:
            for op in self.ops[e]:
                for d in op.deps:
                    d.sig = True
            if self.ops[e]:
                last = [o for o in self.ops[e] if not o.dma]
                if last:
                    last[-1].sig = True
        for e in ENGS:
            for op in self.ops[e]:
                if op.dma:
                    i = gl.di[e] % DMAK
                    gl.di[e] += 1
                    gl.dcnt[e][i] += 16
                    op.sem = gl.dsem[e][i]; op.val = gl.dcnt[e][i]
                    op.prev = gl.dlast[e][i]
                    gl.dlast[e][i] = op
                elif op.sig:
                    gl.cnt[e] += 1
                    op.sem = gl.sem[e]; op.val = gl.cnt[e]
        with nc.Block() as block:
            for e in ENGS:
                ops = self.ops[e]

                def body(eng, e=e, ops=ops):
                    known = {}
                    for (s, v) in bar:
                        eng.wait_ge(s, v)
                        known[id(s)] = v
                    for op in ops:
                        waits = {}
                        for d in op.deps:
                            k = id(d.sem)
                            if waits.get(k, (None, 0))[1] < d.val:
                                waits[k] = (d.sem, d.val)
                        if op.dma and op.prev is not None:
                            k = id(op.prev.sem)
                            if waits.get(k, (None, 0))[1] < op.prev.val:
                                waits[k] = (op.prev.sem, op.prev.val)
                        for k, (s, v) in waits.items():
                            if known.get(k, 0) < v:
                                eng.wait_ge(s, v)
                                known[k] = v
                        ins = op.fn(eng)
                        gl.n_ins += 1
                        if op.dma:
                            ins.then_inc(op.sem, 16)
                        elif op.sig:
                            ins.then_inc(op.sem, 1)
                    if e in ('sp', 'pool'):
                        for i in range(DMAK):
                            if gl.dcnt[e][i] > 0:
                                eng.wait_ge(gl.dsem[e][i], gl.dcnt[e][i])
                getattr(block, BNAME[e])(body)


class Ctx:
    pass


_UC = [0]


def U(name):
    _UC[0] += 1
    return "%s_%d" % (name, _UC[0])


def row_of(cx, t128):
    tb = cx.TB // 128
    b, r = divmod(t128, tb)
    return b if r < cx.S // 128 else cx.B


def segs_of_group(cx, g):
    out = []
    for j in range(G // 128):
        r = row_of(cx, g * (G // 128) + j)
        if out and out[-1][2] == r:
            out[-1] = (out[-1][0], out[-1][1] + 128, r)
        else:
            out.append((j * 128, 128, r))
    return out


def phase_convert(cx, jobs):
    nc = cx.nc
    p = Prog(cx.gl)
    NS = 3
    with nc.sbuf_tensor(U("cv_s"), [128, NS, 8192], F32) as st, nc.sbuf_tensor(U("cv_b"), [128, NS, 8192], BF16) as bt:
        i = 0
        for (src, dst, P, A, Bc) in jobs:
            astep = max(1, 8192 // Bc)
            for a0 in range(0, A, astep):
                a1 = min(A, a0 + astep)
                n = (a1 - a0) * Bc
                s = i % NS
                sv = st[:P, s, 0:n].rearrange("p (a b) -> p a b", b=Bc)
                p.load(sv, src[:, a0:a1, :], writes=[('cs', s)])
                if i % 2 == 0:
                    p.add('dve', lambda e, o=bt[:P, s, 0:n], x=st[:P, s, 0:n]: e.tensor_copy(out=o, in_=x),
                          reads=[('cs', s)], writes=[('cb', s)])
                else:
                    p.add('act', lambda e, o=bt[:P, s, 0:n], x=st[:P, s, 0:n]: e.activation(out=o, in_=x, func=AF.Copy),
                          reads=[('cs', s)], writes=[('cb', s)])
                p.store(dst[:, a0 * Bc:a1 * Bc], bt[:P, s, 0:n], reads=[('cb', s)])
                i += 1
        p.emit()


def phase_mod(cx, l):
    nc = cx.nc
    p = Prog(cx.gl)
    R = cx.B + 1
    with (nc.sbuf_tensor(U("md_src"), [128, KC, R], F32) as src, nc.sbuf_tensor(U("md_srcb"), [128, KC, R], BF16) as srcb,
          nc.sbuf_tensor(U("md_sg"), [128, KC, R], F32) as sg,
          nc.sbuf_tensor(U("md_wd"), [128, KC * 512], BF16) as wd, nc.sbuf_tensor(U("md_h"), [128, 4, R], BF16) as h1,
          nc.sbuf_tensor(U("md_wu"), [128, 2, 4 * 2048], BF16) as wu, nc.sbuf_tensor(U("md_ab"), [128, 192], F32) as ab,
          nc.sbuf_tensor(U("md_raw"), [128, 192, R], F32) as raw, nc.sbuf_tensor(U("md_g"), [128, 2, KC], F32) as ng,
          nc.psum_tensor(U("md_ps"), [128, 4, 512], F32) as ps):
        p.load(src[:], cx.csT.rearrange("(c p) r -> p c r", p=128), writes=['src'])
        p.load(wd[:], cx.img['ada_down'][l, 0], writes=['wd'])
        p.load(ab[:], cx.ada_bT[l], writes=['ab'])
        p.load(ng[:, 0, :], cx.n1gT[l], writes=['ng'])
        p.load(ng[:, 1, :], cx.n2gT[l], writes=['ng'])
        p.add('act', lambda e: e.activation(out=sg[:], in_=src[:], func=AF.Sigmoid), reads=['src'], writes=['sg'])
        p.add('dve', lambda e: e.tensor_tensor(out=srcb[:], in0=src[:], in1=sg[:], op=ALU.mult), reads=['src', 'sg'], writes=['srcb'])
        for m in range(4):
            for k in range(KC):
                p.mm(ps[:, m, 0:R], wd[:, k * 512 + m * 128: k * 512 + (m + 1) * 128], srcb[:, k, :], k == 0, k == KC - 1,
                     reads=['wd', 'srcb'], writes=[('ps', m)])
            p.add('dve', lambda e, m=m: e.tensor_copy(out=h1[:, m, :], in_=ps[:, m, 0:R]), reads=[('ps', m)], writes=['h1'])
        for s in range(12):
            sl = s % 2
            p.load(wu[:, sl, :], cx.img['ada_up'][l, s], writes=[('wu', sl)])
            for mm_ in range(16):
                m = s * 16 + mm_
                pb = mm_ % 4
                for k in range(4):
                    p.mm(ps[:, pb, 0:R], wu[:, sl, k * 2048 + mm_ * 128: k * 2048 + (mm_ + 1) * 128], h1[:, k, :], k == 0, k == 3,
                         reads=[('wu', sl), 'h1'], writes=[('ps', pb)])
                p.add('dve', lambda e, m=m, pb=pb: e.tensor_scalar(out=raw[:, m, :], in0=ps[:, pb, 0:R], scalar1=ab[:, m:m + 1],
                                                                   scalar2=None, op0=ALU.add),
                      reads=[('ps', pb), 'ab'], writes=['raw'])
        mv = cx.modv
        for which, (ish, isc, ig) in enumerate(((0, 1, 2), (3, 4, 5))):
            o = which * 3
            p.add('dve', lambda e, o=o, isc=isc: e.tensor_scalar(out=mv[:, o, :, :], in0=raw[:, isc * 32:(isc + 1) * 32, :], scalar1=1.0,
                                                                   scalar2=None, op0=ALU.add), reads=['raw'], writes=['mv'])
            p.add('dve', lambda e, o=o, which=which: e.tensor_tensor(out=mv[:, o, :, :], in0=mv[:, o, :, :],
                                                                     in1=ng[:, which, :].unsqueeze(2).to_broadcast([128, KC, R]), op=ALU.mult),
                  reads=['mv', 'ng'], writes=['mv'])
            p.add('dve', lambda e, o=o, ish=ish: e.tensor_copy(out=mv[:, o + 1, :, :], in_=raw[:, ish * 32:(ish + 1) * 32, :]), reads=['raw'], writes=['mv'])
            p.add('dve', lambda e, o=o, ig=ig: e.tensor_copy(out=mv[:, o + 2, :, :], in_=raw[:, ig * 32:(ig + 1) * 32, :]), reads=['raw'], writes=['mv'])
        p.emit()


def phase_norm(cx, l, which, final=False):
    nc = cx.nc
    p = Prog(cx.gl)
    mv = cx.modv
    NGR = cx.NT // G
    router = (which == 2 and not final)
    with (nc.sbuf_tensor(U("nm_x"), [128, 2, KC, G], F32) as xg, nc.sbuf_tensor(U("nm_a"), [128, 2, KC, G], BF16) as ag,
          nc.sbuf_tensor(U("nm_sq"), [128, 2, G], F32) as sq, nc.sbuf_tensor(U("nm_r"), [128, G], F32) as rs,
          nc.sbuf_tensor(U("nm_fg"), [128, KC], F32) as fg,
          nc.sbuf_tensor(U("nm_rw"), [128, KC, NE], F32) as rw, nc.sbuf_tensor(U("nm_rb"), [128, NE], F32) as rb,
          nc.sbuf_tensor(U("nm_t"), [128, 12, NE], F32) as tt, nc.sbuf_tensor(U("nm_s"), [128, 8, 8], F32) as sm,
          nc.sbuf_tensor(U("nm_cm"), [128, 8, 8], F32) as cmp8,
          nc.sbuf_tensor(U("nm_gt"), [64, 2, G], BF16) as gts,
          nc.psum_tensor(U("nm_ps"), [128, 512], F32) as ps, nc.psum_tensor(U("nm_pl"), [128, 2, 512], F32) as pl,
          nc.psum_tensor(U("nm_pt"), [128, 2, 512], F32) as pt):
        if final:
            p.load(fg[:], cx.fgT, writes=['fg'])
        if router:
            p.load(rw[:], cx.router_w[l].rearrange("(c p) e -> p c e", p=128), writes=['rw'])
            p.load(rb[:], cx.router_b[l:l + 1, :].to_broadcast((128, NE)), writes=['rb'])
        for g in range(NGR):
            s = g % 2
            X = ('x', s)
            p.load(xg[:, s], cx.xT[:, g * G:(g + 1) * G].rearrange("(c p) t -> p c t", p=128), writes=[X])
            for c in range(KC):
                q = c % 2
                p.add('act', lambda e, s=s, c=c, q=q: e.activation(out=sq[:, q, :], in_=xg[:, s, c, :], func=AF.Square),
                      reads=[X], writes=[('sq', q)])
                p.mm(ps[:, 0:G], cx.onesF[:], sq[:, q, :], c == 0, c == KC - 1, reads=[('sq', q)], writes=['ps'])
            p.add('dve', lambda e: e.tensor_scalar(out=rs[:], in0=ps[:, 0:G], scalar1=1.0 / D, scalar2=EPS, op0=ALU.mult, op1=ALU.add),
                  reads=['ps'], writes=['rs'])
            p.add('act', lambda e: e.activation(out=rs[:], in_=rs[:], func=AF.Sqrt), reads=['rs'], writes=['rs'])
            p.add('dve', lambda e: e.reciprocal(out=rs[:], in_=rs[:]), reads=['rs'], writes=['rs'])
            h = KC // 2
            p.add('dve', lambda e, s=s: e.tensor_tensor(out=xg[:, s, 0:h], in0=xg[:, s, 0:h], in1=rs[:].unsqueeze(1).to_broadcast([128, h, G]), op=ALU.mult),
                  reads=[X, 'rs'], writes=[X])
            p.add('pool', lambda e, s=s: e.tensor_tensor(out=xg[:, s, h:KC], in0=xg[:, s, h:KC], in1=rs[:].unsqueeze(1).to_broadcast([128, h, G]), op=ALU.mult),
                  reads=[X, 'rs'], writes=[X])
            A = ('a', s)
            if final:
                for c in range(KC):
                    eng = 'act' if c % 2 == 0 else 'dve'
                    if eng == 'act':
                        p.add('act', lambda e, s=s, c=c: e.activation(out=xg[:, s, c, :], in_=xg[:, s, c, :], func=AF.Copy, scale=fg[:, c:c + 1]),
                              reads=[X, 'fg'], writes=[X])
                    else:
                        p.add('dve', lambda e, s=s, c=c: e.tensor_scalar(out=xg[:, s, c, :], in0=xg[:, s, c, :], scalar1=fg[:, c:c + 1], scalar2=None, op0=ALU.mult),
                              reads=[X, 'fg'], writes=[X])
                p.store(cx.outT[:, g * G:(g + 1) * G].rearrange("(c p) t -> p c t", p=128), xg[:, s], reads=[X])
                continue
            o = (which - 1) * 3
            for (c0, ncol, r) in segs_of_group(cx, g):
                for c in range(KC):
                    dst = xg[:, s, c, c0:c0 + ncol] if router else ag[:, s, c, c0:c0 + ncol]
                    wr = [X] if router else [A]
                    if c % 2 == 0:
                        p.add('act', lambda e, dst=dst, s=s, c=c, c0=c0, ncol=ncol, r=r: e.activation(
                            out=dst, in_=xg[:, s, c, c0:c0 + ncol], func=AF.Identity, scale=mv[:, o, c, r:r + 1], bias=mv[:, o + 1, c, r:r + 1]),
                            reads=[X, 'mv'], writes=wr)
                    else:
                        p.add('dve', lambda e, dst=dst, s=s, c=c, c0=c0, ncol=ncol, r=r: e.tensor_scalar(
                            out=dst, in0=xg[:, s, c, c0:c0 + ncol], scalar1=mv[:, o, c, r:r + 1], scalar2=mv[:, o + 1, c, r:r + 1],
                            op0=ALU.mult, op1=ALU.add), reads=[X, 'mv'], writes=wr)
            if router:
                p.add('pool', lambda e, s=s: e.tensor_copy(out=ag[:, s], in_=xg[:, s]), reads=[X], writes=[A])
                GT = ('gt', s)
                for j in range(G // 128):
                    pj = j % 2
                    for c in range(KC):
                        p.mm(pl[:, pj, 0:NE], xg[:, s, c, j * 128:(j + 1) * 128], rw[:, c, :], c == 0, c == KC - 1,
                             reads=[X, 'rw'], writes=[('pl', pj)])
                    sc, bi, t2, msk, w, sel = (tt[:, i, :] for i in range(6))
                    v3 = lambda a: a.rearrange("p (g k) -> p g k", k=8)
                    m1, m2, gs, gm, pen, cnt = (sm[:, i, :] for i in range(6))
                    top8 = sm[:, 6, :]
                    thr = sm[:, 7, 0:1]
                    wsum = sm[:, 7, 1:2]
                    T = 'tt'
                    p.add('act', lambda e, pj=pj, sc=sc: e.activation(out=sc, in_=pl[:, pj, 0:NE], func=AF.Sigmoid), reads=[('pl', pj)], writes=[T])
                    dv = lambda fn: p.add('dve', fn, reads=[T, 'rb'], writes=[T])
                    dv(lambda e, bi=bi, sc=sc: e.tensor_tensor(out=bi, in0=sc, in1=rb[:], op=ALU.add))
                    dv(lambda e, m1=m1, bi=bi: e.tensor_reduce(out=m1, in_=v3(bi), axis=AX.X, op=ALU.max))
                    dv(lambda e, t2=t2, bi=bi, m1=m1: e.tensor_tensor(out=v3(t2), in0=v3(bi), in1=m1.unsqueeze(2).to_broadcast([128, 8, 8]), op=ALU.is_equal))
                    dv(lambda e, t2=t2, bi=bi: e.scalar_tensor_tensor(out=t2, in0=t2, scalar=-1e30, in1=bi, op0=ALU.mult, op1=ALU.add))
                    dv(lambda e, m2=m2, t2=t2: e.tensor_reduce(out=m2, in_=v3(t2), axis=AX.X, op=ALU.max))
                    dv(lambda e, gs=gs, m1=m1, m2=m2: e.tensor_tensor(out=gs, in0=m1, in1=m2, op=ALU.add))
                    dv(lambda e, gs=gs: e.tensor_tensor(out=cmp8[:], in0=gs.unsqueeze(1).to_broadcast([128, 8, 8]),
                                                        in1=gs.unsqueeze(2).to_broadcast([128, 8, 8]), op=ALU.is_gt))
                    dv(lambda e, cnt=cnt: e.tensor_reduce(out=cnt, in_=cmp8[:], axis=AX.X, op=ALU.add))
                    dv(lambda e, pen=pen, cnt=cnt: e.tensor_scalar(out=pen, in0=cnt, scalar1=3.5, scalar2=-1e30, op0=ALU.is_gt, op1=ALU.mult))
                    dv(lambda e, msk=msk, bi=bi, pen=pen: e.tensor_tensor(out=v3(msk), in0=v3(bi), in1=pen.unsqueeze(2).to_broadcast([128, 8, 8]), op=ALU.add))
                    dv(lambda e, top8=top8, msk=msk: e.max(out=top8, in_=msk))
                    dv(lambda e, thr=thr, top8=top8: e.tensor_reduce(out=thr, in_=top8, axis=AX.X, op=ALU.min))
                    dv(lambda e, sel=sel, msk=msk, thr=thr: e.tensor_scalar(out=sel, in0=msk, scalar1=thr, scalar2=None, op0=ALU.is_ge))
                    dv(lambda e, w=w, sc=sc, sel=sel: e.tensor_tensor(out=w, in0=sc, in1=sel, op=ALU.mult))
                    dv(lambda e, wsum=wsum, w=w: e.tensor_reduce(out=wsum, in_=w, axis=AX.X, op=ALU.add))
                    dv(lambda e, wsum=wsum: e.reciprocal(out=wsum, in_=wsum))
                    dv(lambda e, w=w, wsum=wsum: e.tensor_scalar(out=w, in0=w, scalar1=wsum, scalar2=2.5, op0=ALU.mult, op1=ALU.mult))
                    p.add('pe', lambda e, pj=pj, w=w: e.transpose(pt[0:NE, pj, 0:128], w, cx.ident[:]), reads=[T], writes=[('pt', pj)])
                    p.add('act', lambda e, pj=pj, s=s, j=j: e.activation(out=gts[:, s, j * 128:(j + 1) * 128], in_=pt[0:NE, pj, 0:128], func=AF.Copy),
                          reads=[('pt', pj)], writes=[GT])
                p.store(cx.gT[:, g * G:(g + 1) * G], gts[:, s, :], reads=[GT])
            p.store(cx.aT[:, g * G:(g + 1) * G].rearrange("(c p) t -> p c t", p=128), ag[:, s], reads=[A])
        p.emit()


def phase_proj(cx, inT, wimg, nslab, evac_T=None, evac_N=None, modes=None, kc=KC):
    nc = cx.nc
    p = Prog(cx.gl)
    GB = 3
    TBK = GB * G
    nblk = cx.NT // TBK
    assert cx.NT % TBK == 0
    with (nc.sbuf_tensor(U("pj_a"), [128, kc, TBK], BF16) as at, nc.sbuf_tensor(U("pj_w"), [128, 2, kc * 512], BF16) as wt,
          nc.psum_tensor(U("pj_ps"), [128, 4, 512], F32) as ps):
        cx.pj_extra(p)
        it = 0
        pi = 0
        for blk in range(nblk):
            AQ = 4
            kq = kc // AQ
            for q in range(AQ):
                p.load(at[:, q * kq:(q + 1) * kq, :],
                       inT[q * kq * 128:(q + 1) * kq * 128, blk * TBK:(blk + 1) * TBK].rearrange("(c p) t -> p c t", p=128), writes=[('at', q)])
            for s in range(nslab):
                sl = it % 2
                it += 1
                p.load(wt[:, sl, :], wimg[s], writes=[('w', sl)])
                mode = modes[s] if modes else 'T'
                if mode == 'T':
                    for mi in range(4):
                        for gi in range(GB):
                            pb = pi % 4
                            pi += 1
                            for k in range(kc):
                                p.mm(ps[:, pb, 0:G], wt[:, sl, k * 512 + mi * 128:k * 512 + (mi + 1) * 128], at[:, k, gi * G:(gi + 1) * G],
                                     k == 0, k == kc - 1, reads=[('w', sl), ('at', k // kq)], writes=[('ps', pb)])
                            evac_T(p, ps[:, pb, 0:G], ('ps', pb), s * 4 + mi, blk * GB + gi)
                else:
                    for ti in range(TBK // 128):
                        pb = pi % 4
                        pi += 1
                        for k in range(kc):
                            p.mm(ps[:, pb, :], at[:, k, ti * 128:(ti + 1) * 128], wt[:, sl, k * 512:(k + 1) * 512],
                                 k == 0, k == kc - 1, reads=[('w', sl), ('at', k // kq)], writes=[('ps', pb)])
                        evac_N(p, ps[:, pb, :], ('ps', pb), s, blk * (TBK // 128) + ti)
        p.emit()


def make_store_T(cx, stage, outT, nslot=4):
    st = {'i': 0}

    def ev(p, ps_ap, pskey, m, g):
        s = st['i'] % nslot
        st['i'] += 1
        key = ('stg', s)
        if st['i'] % 2 == 0:
            p.add('act', lambda e: e.activation(out=stage[:, s, 0:G], in_=ps_ap, func=AF.Copy), reads=[pskey], writes=[key])
        else:
            p.add('dve', lambda e: e.tensor_copy(out=stage[:, s, 0:G], in_=ps_ap), reads=[pskey], writes=[key])
        p.store(outT[m * 128:(m + 1) * 128, g * G:(g + 1) * G], stage[:, s, 0:G], reads=[key])
    return ev


def make_store_N(cx, stage, outN, col_of_slab, nslot=4):
    st = {'i': 0}

    def ev(p, ps_ap, pskey, s_, t):
        s = st['i'] % nslot
        st['i'] += 1
        key = ('stg', s)
        if st['i'] % 2 == 0:
            p.add('act', lambda e: e.activation(out=stage[:, s, :], in_=ps_ap, func=AF.Copy), reads=[pskey], writes=[key])
        else:
            p.add('dve', lambda e: e.tensor_copy(out=stage[:, s, :], in_=ps_ap), reads=[pskey], writes=[key])
        c0 = col_of_slab(s_)
        p.store(outN[t * 128:(t + 1) * 128, c0:c0 + 512], stage[:, s, :], reads=[key])
    return ev


def make_resid_T(cx, xst, gate_idx):
    st = {'i': 0}
    mv = cx.modv

    def ev(p, ps_ap, pskey, m, g):
        s = st['i'] % 3
        st['i'] += 1
        key = ('xs', s)
        xa = cx.xT[m * 128:(m + 1) * 128, g * G:(g + 1) * G]
        p.load(xst[:, s, :], xa, writes=[key])
        for (c0, ncol, r) in segs_of_group(cx, g):
            p.add('dve', lambda e, c0=c0, ncol=ncol, r=r: e.scalar_tensor_tensor(
                out=xst[:, s, c0:c0 + ncol], in0=ps_ap[:, c0:c0 + ncol], scalar=mv[:, gate_idx, m, r:r + 1], in1=xst[:, s, c0:c0 + ncol],
                op0=ALU.mult, op1=ALU.add), reads=[pskey, key, 'mv'], writes=[key])
        p.store(xa, xst[:, s, :], reads=[key])
    return ev


def run_proj_store(cx, inT, wimg, nslab, outT=None, outN=None, modes=None, col_of_slab=None, resid_gate=None):
    nc = cx.nc
    with (nc.sbuf_tensor(U("pj_st"), [128, 4, 512], BF16) as stage, nc.sbuf_tensor(U("pj_xs"), [128, 3, G], F32) as xst):
        cx.pj_extra = lambda p: None
        evT = None
        if resid_gate is not None:
            evT = make_resid_T(cx, xst, resid_gate)
        elif outT is not None:
            evT = make_store_T(cx, stage, outT)
        evN = make_store_N(cx, stage, outN, col_of_slab) if outN is not None else None
        phase_proj(cx, inT, wimg, nslab, evac_T=evT, evac_N=evN, modes=modes)


def rope_load(cx, p, dst, swb, swp, tmp, key, src_rows, t0, n, pos0):
    p.load(dst, src_rows[:, t0:t0 + n], writes=[key])
    p.load(swb[0:64, 0:n], src_rows[64:128, t0:t0 + n], writes=[(key, 'swb')])
    p.load(swb[64:128, 0:n], src_rows[0:64, t0:t0 + n], writes=[(key, 'swb')])
    p.add('pool', lambda e: e.tensor_tensor(out=tmp[:, 0:n], in0=swb[:, 0:n], in1=cx.sin2[:, pos0:pos0 + n], op=ALU.mult),
          reads=[(key, 'swb')], writes=[(key, 'tmp')])
    p.add('dve', lambda e: e.tensor_tensor(out=swp[:, 0:n], in0=dst, in1=cx.cos2[:, pos0:pos0 + n], op=ALU.mult),
          reads=[key], writes=[(key, 'sw')])
    p.add('dve', lambda e: e.tensor_tensor(out=dst, in0=swp[:, 0:n], in1=tmp[:, 0:n], op=ALU.add),
          reads=[(key, 'sw'), (key, 'tmp')], writes=[key])


def phase_attn_a(cx, j):
    nc = cx.nc
    p = Prog(cx.gl)
    S, C, TB = cx.S, cx.C, cx.TB
    NQB = S // 128
    SC = HD ** -0.5
    with (nc.sbuf_tensor(U("aa_k"), [128, 2, TB], BF16) as kt, nc.sbuf_tensor(U("aa_q"), [128, 2, TB], BF16) as qt,
          nc.sbuf_tensor(U("aa_sw"), [128, 2, S], F32) as swp, nc.sbuf_tensor(U("aa_tm"), [128, 2, S], F32) as tmp, nc.sbuf_tensor(U("aa_swb"), [128, 2, S], BF16) as swb,
          nc.sbuf_tensor(U("aa_v"), [128, 2, TB // 128, 128], BF16) as vt,
          nc.sbuf_tensor(U("aa_p"), [128, 2, NQB + 2, 512], BF16) as pt_, nc.sbuf_tensor(U("aa_pc"), [128, 2, 2, S + C], BF16) as pc,
          nc.sbuf_tensor(U("aa_o"), [128, 2, S + C], BF16) as ot, nc.sbuf_tensor(U("aa_r"), [128, 2, 512], F32) as rc,
          nc.sbuf_tensor(U("aa_es"), [128, 32], F32) as es,
          nc.psum_tensor(U("aa_s"), [128, 3, 512], F32) as pss, nc.psum_tensor(U("aa_po"), [128, 2, 512], F32) as pso,
          nc.psum_tensor(U("aa_pz"), [128, 2, 512], F32) as psz):
        p.load(es[:], cx.a_sink[j:j + 1, :].to_broadcast((128, 32)), writes=['es'])
        p.add('act', lambda e: e.activation(out=es[:], in_=es[:], func=AF.Exp), reads=['es'], writes=['es'])
        si = 0
        oi = 0
        hi = 0
        for b in range(cx.B):
            t0 = b * TB
            for kv in range(8):
                ks = (b * 8 + kv) % 2
                K = ('k', ks)
                krows = cx.qkvT[4096 + kv * 128:4096 + (kv + 1) * 128, :]
                rope_load(cx, p, kt[:, ks, 0:S], swb[:, 0, :], swp[:, 0, :], tmp[:, 0, :], K, krows, t0, S, 0)
                p.load(kt[:, ks, S:TB], krows[:, t0 + S:t0 + TB], writes=[K])
                V = ('v', ks)
                p.load(vt[:, ks], cx.vN[t0:t0 + TB, kv * 128:(kv + 1) * 128].rearrange("(n p) d -> p n d", p=128), writes=[V])
                for gq in range(4):
                    h = kv * 4 + gq
                    qs = hi % 2
                    hi += 1
                    Q = ('q', qs)
                    qrows = cx.qkvT[h * 128:(h + 1) * 128, :]
                    rope_load(cx, p, qt[:, qs, 0:S], swb[:, 1, :], swp[:, 1, :], tmp[:, 1, :], Q, qrows, t0, S, 0)
                    p.load(qt[:, qs, S:TB], qrows[:, t0 + S:t0 + TB], writes=[Q])
                    P = ('p', qs)
                    PC = ('pc', qs)
                    for kb in range(NQB):
                        q0 = max(0, kb - 1)
                        q1 = min(NQB, kb + 2)
                        n = (q1 - q0) * 128
                        sb = si % 3
                        si += 1
                        p.mm(pss[:, sb, 0:n], kt[:, ks, kb * 128:(kb + 1) * 128], qt[:, qs, q0 * 128:q1 * 128], True, True,
                             reads=[K, Q], writes=[('pss', sb)])
                        p.add('act', lambda e, sb=sb, n=n, qs=qs, kb=kb: e.activation(out=pt_[:, qs, kb, 0:n], in_=pss[:, sb, 0:n], func=AF.Exp, scale=SC),
                              reads=[('pss', sb)], writes=[P])
                        m0 = (q0 - (kb - 1)) * 128
                        p.add('pool', lambda e, n=n, qs=qs, kb=kb, m0=m0: e.tensor_tensor(out=pt_[:, qs, kb, 0:n], in0=pt_[:, qs, kb, 0:n],
                                                                                         in1=cx.wmask[:, m0:m0 + n], op=ALU.mult),
                              reads=[P], writes=[P])
                    for cb in range(C // 128):
                        for q0 in range(0, S + C, 512):
                            n = min(512, S + C - q0)
                            sb = si % 3
                            si += 1
                            p.mm(pss[:, sb, 0:n], kt[:, ks, S + cb * 128:S + (cb + 1) * 128], qt[:, qs, q0:q0 + n], True, True,
                                 reads=[K, Q], writes=[('pss', sb)])
                            p.add('act', lambda e, sb=sb, n=n, qs=qs, cb=cb, q0=q0: e.activation(out=pc[:, qs, cb, q0:q0 + n], in_=pss[:, sb, 0:n], func=AF.Exp, scale=SC),
                                  reads=[('pss', sb)], writes=[PC])
                    O = ('o', qs)
                    nqb_all = (S + C) // 128
                    for qg in range(0, nqb_all, 4):
                        ob = oi % 2
                        oi += 1
                        nq = min(4, nqb_all - qg)
                        for qi in range(nq):
                            qb = qg + qi
                            terms = []
                            if qb < NQB:
                                for kb in (qb - 1, qb, qb + 1):
                                    if 0 <= kb < NQB:
                                        q0 = max(0, kb - 1)
                                        off = (qb - q0) * 128
                                        terms.append((vt[:, ks, kb, :], pt_[:, qs, kb, off:off + 128]))
                            for cb in range(C // 128):
                                terms.append((vt[:, ks, NQB + cb, :], pc[:, qs, cb, qb * 128:(qb + 1) * 128]))
                            for ti, (vv, pp) in enumerate(terms):
                                p.mm(pso[:, ob, qi * 128:(qi + 1) * 128], vv, pp, ti == 0, ti == len(terms) - 1,
                                     reads=[V, P, PC], writes=[('pso', ob)])
                            for ti, (vv, pp) in enumerate(terms):
                                p.mm(psz[:, ob, qi * 128:(qi + 1) * 128], cx.onesB[:], pp, ti == 0, ti == len(terms) - 1,
                                     reads=[P, PC], writes=[('psz', ob)])
                        n = nq * 128
                        R = ('rc', ob)
                        p.add('dve', lambda e, ob=ob, n=n, h=h: e.tensor_scalar(out=rc[:, ob, 0:n], in0=psz[:, ob, 0:n], scalar1=es[:, h:h + 1], scalar2=None, op0=ALU.add),
                              reads=[('psz', ob), 'es'], writes=[R])
                        p.add('dve', lambda e, ob=ob, n=n: e.reciprocal(out=rc[:, ob, 0:n], in_=rc[:, ob, 0:n]), reads=[R], writes=[R])
                        p.add('dve', lambda e, ob=ob, n=n, qs=qs, qg=qg: e.tensor_tensor(out=ot[:, qs, qg * 128:qg * 128 + n], in0=pso[:, ob, 0:n], in1=rc[:, ob, 0:n], op=ALU.mult),
                              reads=[('pso', ob), R], writes=[O])
                    p.store(cx.oT[h * 128:(h + 1) * 128, t0:t0 + TB], ot[:, qs, :], reads=[O])
        p.emit()


def phase_attn_b(cx, l):
    nc = cx.nc
    p = Prog(cx.gl)
    S, C, TB = cx.S, cx.C, cx.TB
    NKB = TB // 128
    SC = HD ** -0.5
    lam_init = 0.8 - 0.6 * math.exp(-0.3 * l)
    from contextlib import ExitStack
    with ExitStack() as es_:
        kt = es_.enter_context(nc.sbuf_tensor(U("ab_k"), [128, 2, 2, TB], BF16))
        qt = es_.enter_context(nc.sbuf_tensor(U("ab_q"), [128, 2, 2, TB], BF16))
        swp = es_.enter_context(nc.sbuf_tensor(U("ab_sw"), [128, 2, S], F32))
        tmp = es_.enter_context(nc.sbuf_tensor(U("ab_tm"), [128, 2, S], F32))
        swb = es_.enter_context(nc.sbuf_tensor(U("ab_swb"), [128, 2, S], BF16))
        vt = es_.enter_context(nc.sbuf_tensor(U("ab_v"), [128, 2, NKB, 256], BF16))
        pt_ = es_.enter_context(nc.sbuf_tensor(U("ab_p"), [128, 4, 512], BF16))
        o32 = es_.enter_context(nc.sbuf_tensor(U("ab_o"), [128, 2, 2, 512], F32))
        ob16 = es_.enter_context(nc.sbuf_tensor(U("ab_ob"), [128, 2, 2, 512], BF16))
        rc = es_.enter_context(nc.sbuf_tensor(U("ab_r"), [128, 2, 512], F32))
        sq = es_.enter_context(nc.sbuf_tensor(U("ab_sq"), [128, 2, 512], F32))
        lm = es_.enter_context(nc.sbuf_tensor(U("ab_l"), [128, 8], F32))
        sg = es_.enter_context(nc.sbuf_tensor(U("ab_sg"), [128, 2], F32))
        pss = es_.enter_context(nc.psum_tensor(U("ab_s"), [128, 2, 512], F32))
        pso = es_.enter_context(nc.psum_tensor(U("ab_po"), [128, 4, 512], F32))
        psz = es_.enter_context(nc.psum_tensor(U("ab_pz"), [128, 2, 512], F32))
        p.load(lm[:, 0:4], cx.b_lamT, writes=['lm'])
        p.load(sg[:], cx.b_sgT, writes=['sg'])
        p.add('dve', lambda e: e.tensor_tensor(out=lm[:, 4:5], in0=lm[:, 0:1], in1=lm[:, 1:2], op=ALU.mult), reads=['lm'], writes=['lm'])
        p.add('dve', lambda e: e.tensor_tensor(out=lm[:, 5:6], in0=lm[:, 2:3], in1=lm[:, 3:4], op=ALU.mult), reads=['lm'], writes=['lm'])
        p.mm(psz[:, 0, 0:2], cx.onesF[:], lm[:, 4:6], True, True, reads=['lm'], writes=[('psz', 0)])
        p.add('act', lambda e: e.activation(out=lm[:, 6:8], in_=psz[:, 0, 0:2], func=AF.Exp), reads=[('psz', 0)], writes=['lm'])
        p.add('dve', lambda e: e.tensor_tensor(out=lm[:, 6:7], in0=lm[:, 7:8], in1=lm[:, 6:7], op=ALU.subtract), reads=['lm'], writes=['lm'])
        p.add('dve', lambda e: e.tensor_scalar(out=lm[:, 6:7], in0=lm[:, 6:7], scalar1=-lam_init, scalar2=None, op0=ALU.add), reads=['lm'], writes=['lm'])
        p.add('dve', lambda e: e.tensor_scalar(out=sg[:], in0=sg[:], scalar1=1.0 - lam_init, scalar2=None, op0=ALU.mult), reads=['sg'], writes=['sg'])
        si = 0
        pi = 0
        for b in range(cx.B):
            t0 = b * TB
            for h in range(16):
                ks = (b * 16 + h) % 2
                K = ('k', ks)
                Q = ('q', ks)
                V = ('v', ks)
                for c in range(2):
                    krows = cx.qkvT[4096 + (h * 2 + c) * 128:4096 + (h * 2 + c + 1) * 128, :]
                    rope_load(cx, p, kt[:, ks, c, 0:S], swb[:, 0, :], swp[:, 0, :], tmp[:, 0, :], (K, c), krows, t0, S, 0)
                    p.load(kt[:, ks, c, S:TB], krows[:, t0 + S:t0 + TB], writes=[(K, c)])
                    qrows = cx.qkvT[(h * 2 + c) * 128:(h * 2 + c + 1) * 128, :]
                    rope_load(cx, p, qt[:, ks, c, 0:S], swb[:, 1, :], swp[:, 1, :], tmp[:, 1, :], (Q, c), qrows, t0, S, 0)
                    p.load(qt[:, ks, c, S:TB], qrows[:, t0 + S:t0 + TB], writes=[(Q, c)])
                p.load(vt[:, ks], cx.vN[t0:t0 + TB, h * 256:(h + 1) * 256].rearrange("(n p) d -> p n d", p=128), writes=[V])
                chunks = [(q0, 512, 0, NKB) for q0 in range(0, S, 512)] + [(S, C, S // 128, NKB)]
                for (q0, n, kb0, kb1) in chunks:
                    nk = kb1 - kb0
                    for ki, kb in enumerate(range(kb0, kb1)):
                        for c in range(2):
                            sb = si % 2
                            si += 1
                            ps_ = pi % 4
                            pi += 1
                            p.mm(pss[:, sb, 0:n], kt[:, ks, c, kb * 128:(kb + 1) * 128], qt[:, ks, c, q0:q0 + n], True, True,
                                 reads=[(K, c), (Q, c)], writes=[('pss', sb)])
                            p.add('act', lambda e, sb=sb, n=n, ps_=ps_: e.activation(out=pt_[:, ps_, 0:n], in_=pss[:, sb, 0:n], func=AF.Exp, scale=SC),
                                  reads=[('pss', sb)], writes=[('p', ps_)])
                            for et in range(2):
                                p.mm(pso[:, c * 2 + et, 0:n], vt[:, ks, kb, et * 128:(et + 1) * 128], pt_[:, ps_, 0:n], ki == 0, ki == nk - 1,
                                     reads=[V, ('p', ps_)], writes=[('pso', c * 2 + et)])
                            p.mm(psz[:, c, 0:n], cx.onesB[:], pt_[:, ps_, 0:n], ki == 0, ki == nk - 1,
                                 reads=[('p', ps_)], writes=[('psz', c)])
                    for c in range(2):
                        p.add('dve', lambda e, c=c, n=n: e.reciprocal(out=rc[:, c, 0:n], in_=psz[:, c, 0:n]), reads=[('psz', c)], writes=[('rc', c)])
                    p.add('dve', lambda e, n=n: e.tensor_scalar(out=rc[:, 1, 0:n], in0=rc[:, 1, 0:n], scalar1=lm[:, 6:7], scalar2=None, op0=ALU.mult),
                          reads=[('rc', 1), 'lm'], writes=[('rc', 1)])
                    for et in range(2):
                        p.add('dve', lambda e, et=et, n=n: e.tensor_tensor(out=o32[:, 0, et, 0:n], in0=pso[:, et, 0:n], in1=rc[:, 0, 0:n], op=ALU.mult),
                              reads=[('pso', et), ('rc', 0)], writes=[('o32', 0, et)])
                        p.add('dve', lambda e, et=et, n=n: e.tensor_tensor(out=o32[:, 1, et, 0:n], in0=pso[:, 2 + et, 0:n], in1=rc[:, 1, 0:n], op=ALU.mult),
                              reads=[('pso', 2 + et), ('rc', 1)], writes=[('o32', 1, et)])
                        p.add('pool', lambda e, et=et, n=n: e.tensor_tensor(out=o32[:, 0, et, 0:n], in0=o32[:, 0, et, 0:n], in1=o32[:, 1, et, 0:n], op=ALU.add),
                              reads=[('o32', 0, et), ('o32', 1, et)], writes=[('o32', 0, et)])
                        p.add('act', lambda e, et=et, n=n: e.activation(out=sq[:, et, 0:n], in_=o32[:, 0, et, 0:n], func=AF.Square),
                              reads=[('o32', 0, et)], writes=[('sq', et)])
                    sb = si % 2
                    si += 1
                    for et in range(2):
                        p.mm(pss[:, sb, 0:n], cx.onesF[:], sq[:, et, 0:n], et == 0, et == 1, reads=[('sq', et)], writes=[('pss', sb)])
                    p.add('dve', lambda e, sb=sb, n=n: e.tensor_scalar(out=rc[:, 0, 0:n], in0=pss[:, sb, 0:n], scalar1=1.0 / 256, scalar2=EPS, op0=ALU.mult, op1=ALU.add),
                          reads=[('pss', sb)], writes=[('rc', 0)])
                    p.add('act', lambda e, n=n: e.activation(out=rc[:, 0, 0:n], in_=rc[:, 0, 0:n], func=AF.Sqrt), reads=[('rc', 0)], writes=[('rc', 0)])
                    p.add('dve', lambda e, n=n: e.reciprocal(out=rc[:, 0, 0:n], in_=rc[:, 0, 0:n]), reads=[('rc', 0)], writes=[('rc', 0)])
                    osl = (q0 // 512) % 2
                    for et in range(2):
                        p.add('dve', lambda e, et=et, n=n, osl=osl: e.scalar_tensor_tensor(out=ob16[:, osl, et, 0:n], in0=o32[:, 0, et, 0:n], scalar=sg[:, et:et + 1],
                                                                                         in1=rc[:, 0, 0:n], op0=ALU.mult, op1=ALU.mult),
                              reads=[('o32', 0, et), ('rc', 0), 'sg'], writes=[('ob', osl, et)])
                        p.store(cx.oT[h * 256 + et * 128:h * 256 + (et + 1) * 128, t0 + q0:t0 + q0 + n], ob16[:, osl, et, 0:n], reads=[('ob', osl, et)])
        p.emit()


def phase_conv(cx):
    nc = cx.nc
    p = Prog(cx.gl)
    S, C, TB = cx.S, cx.C, cx.TB
    LM = S
    with (nc.sbuf_tensor(U("cv_in"), [128, 2, 3, LM], BF16) as xin, nc.sbuf_tensor(U("cv_v"), [128, 2, LM + 2], F32) as vv,
          nc.sbuf_tensor(U("cv_z"), [128, 2, LM], F32) as zz, nc.sbuf_tensor(U("cv_m"), [128, 2, LM], BF16) as mm_,
          nc.sbuf_tensor(U("cv_w"), [128, KC, 3], F32) as cw):
        p.load(cw[:], cx.c_convT, writes=['cw'])
        it = 0
        for c in range(KC):
            for b in range(cx.B):
                for (o0, L) in ((0, S), (S, C)):
                    s = it % 2
                    it += 1
                    t0 = b * TB + o0
                    I = ('in', s)
                    for w3 in range(3):
                        p.load(xin[:, s, w3, 0:L], cx.qkvT[w3 * 4096 + c * 128:w3 * 4096 + (c + 1) * 128, t0:t0 + L], writes=[I])
                    Vk = ('v', s)
                    p.add('pool', lambda e, s=s, L=L: e.memset(vv[:, s, 0:1], 0.0), writes=[Vk])
                    p.add('pool', lambda e, s=s, L=L: e.memset(vv[:, s, L + 1:L + 2], 0.0), writes=[Vk])
                    p.add('pool', lambda e, s=s, L=L: e.tensor_tensor(out=vv[:, s, 1:L + 1], in0=xin[:, s, 1, 0:L], in1=xin[:, s, 2, 0:L], op=ALU.mult),
                          reads=[I], writes=[Vk])
                    Z = ('z', s)
                    p.add('dve', lambda e, s=s, L=L, c=c: e.tensor_scalar(out=zz[:, s, 0:L], in0=vv[:, s, 0:L], scalar1=cw[:, c, 0:1], scalar2=None, op0=ALU.mult),
                          reads=[Vk, 'cw'], writes=[Z])
                    p.add('dve', lambda e, s=s, L=L, c=c: e.scalar_tensor_tensor(out=zz[:, s, 0:L], in0=vv[:, s, 1:L + 1], scalar=cw[:, c, 1:2], in1=zz[:, s, 0:L],
                                                                                 op0=ALU.mult, op1=ALU.add), reads=[Vk, 'cw', Z], writes=[Z])
                    p.add('dve', lambda e, s=s, L=L, c=c: e.scalar_tensor_tensor(out=zz[:, s, 0:L], in0=vv[:, s, 2:L + 2], scalar=cw[:, c, 2:3], in1=zz[:, s, 0:L],
                                                                                 op0=ALU.mult, op1=ALU.add), reads=[Vk, 'cw', Z], writes=[Z])
                    M = ('m', s)
                    p.add('pool', lambda e, s=s, L=L: e.tensor_tensor(out=mm_[:, s, 0:L], in0=zz[:, s, 0:L], in1=xin[:, s, 0, 0:L], op=ALU.mult),
                          reads=[Z, I], writes=[M])
                    p.store(cx.oT[c * 128:(c + 1) * 128, t0:t0 + L], mm_[:, s, 0:L], reads=[M])
        p.emit()


def phase_convert_pairs(cx, L, exp_gate, exp_up, exp_down):
    nc = cx.nc
    p = Prog(cx.gl)
    NS = 2
    W = 2 * KC * FF
    H = KC * FF
    with nc.sbuf_tensor(U("cp_s"), [128, NS, W], F32) as st, nc.sbuf_tensor(U("cp_b"), [128, NS, W], BF16) as bt:
        i = 0
        for l in range(L):
            for pr in range(NE // 2):
                for (src, dst) in ((exp_gate, cx.img_eg2), (exp_up, cx.img_eu2)):
                    s = i % NS
                    i += 1
                    bv = bt[:, s, :].rearrange("p (c x) -> p c x", x=384)
                    for h in range(2):
                        sv = st[:, s, h * H:(h + 1) * H].rearrange("p (c f) -> p c f", f=FF)
                        p.load(sv, src[l, 2 * pr + h].rearrange("(c p) f -> p c f", p=128), writes=[('cs', s, h)])
                        lo_o, lo_i = bv[:, :, h * 128:(h + 1) * 128], sv[:, :, 0:128]
                        hi_o, hi_i = bv[:, :, 256 + h * 64:256 + (h + 1) * 64], sv[:, :, 128:192]
                        if h == 0:
                            p.add('dve', lambda e, o=lo_o, x=lo_i: e.tensor_copy(out=o, in_=x), reads=[('cs', s, h)], writes=[('cb', s, h, 0)])
                            p.add('dve', lambda e, o=hi_o, x=hi_i: e.tensor_copy(out=o, in_=x), reads=[('cs', s, h)], writes=[('cb', s, h, 1)])
                        else:
                            p.add('act', lambda e, o=lo_o, x=lo_i: e.activation(out=o, in_=x, func=AF.Copy), reads=[('cs', s, h)], writes=[('cb', s, h, 0)])
                            p.add('act', lambda e, o=hi_o, x=hi_i: e.activation(out=o, in_=x, func=AF.Copy), reads=[('cs', s, h)], writes=[('cb', s, h, 1)])
                    p.store(dst[l][pr], bt[:, s, :], reads=[('cb', s, 0, 0), ('cb', s, 0, 1), ('cb', s, 1, 0), ('cb', s, 1, 1)])
                s = i % NS
                i += 1
                p.load(st[:, s, 0:D], exp_down[l, 2 * pr][0:128, :], writes=[('cs', s, 0)])
                p.load(st[:, s, D:2 * D], exp_down[l, 2 * pr + 1][0:128, :], writes=[('cs', s, 1)])
                p.load(st[0:64, s, 2 * D:3 * D], exp_down[l, 2 * pr][128:192, :], writes=[('cs', s, 2)])
                p.load(st[64:128, s, 2 * D:3 * D], exp_down[l, 2 * pr + 1][128:192, :], writes=[('cs', s, 3)])
                p.add('dve', lambda e, o=bt[:, s, 0:H], x=st[:, s, 0:H]: e.tensor_copy(out=o, in_=x),
                      reads=[('cs', s, 0), ('cs', s, 1)], writes=[('cb', s, 0, 0), ('cb', s, 0, 1)])
                p.add('act', lambda e, o=bt[:, s, H:W], x=st[:, s, H:W]: e.activation(out=o, in_=x, func=AF.Copy),
                      reads=[('cs', s, 1), ('cs', s, 2), ('cs', s, 3)], writes=[('cb', s, 1, 0), ('cb', s, 1, 1)])
                p.store(cx.img_ed2[l][pr], bt[:, s, :], reads=[('cb', s, 0, 0), ('cb', s, 0, 1), ('cb', s, 1, 0), ('cb', s, 1, 1)])
        p.emit()


def phase_moe(cx, l):
    nc = cx.nc
    p = Prog(cx.gl)
    mv = cx.modv
    NGR = cx.NT // G
    NP = NE // 2
    W = 2 * KC * FF
    from contextlib import ExitStack
    with ExitStack() as es_:
        E = es_.enter_context
        ft = E(nc.sbuf_tensor(U("mo_f"), [128, KC, G], BF16))
        acc = E(nc.sbuf_tensor(U("mo_acc"), [128, KC, G], F32))
        wg = E(nc.sbuf_tensor(U("mo_wg"), [128, W], BF16))
        wu = E(nc.sbuf_tensor(U("mo_wu"), [128, W], BF16))
        wd = E(nc.sbuf_tensor(U("mo_wd"), [128, W], BF16))
        gt = E(nc.sbuf_tensor(U("mo_gt"), [64, G], BF16))
        sel3 = E(nc.sbuf_tensor(U("mo_sel"), [64, NP, 3, 128], BF16))
        sil = E(nc.sbuf_tensor(U("mo_sil"), [128, 3, G], F32))
        t1 = E(nc.sbuf_tensor(U("mo_t1"), [128, 2, G], F32))
        act = E(nc.sbuf_tensor(U("mo_act"), [128, 2, 3, G], BF16))
        xs = E(nc.sbuf_tensor(U("mo_xs"), [128, 2, G], F32))
        hg = E(nc.psum_tensor(U("mo_hg"), [128, 2, 512], F32))
        hu = E(nc.psum_tensor(U("mo_hu"), [128, 2, 512], F32))
        gb = E(nc.psum_tensor(U("mo_gb"), [128, 512], F32))
        dn = E(nc.psum_tensor(U("mo_dn"), [128, 3, 512], F32))
        identv = cx.identB[0:64, 0:64].rearrange("k (q j) -> k q j", j=2)
        p.add('dve', lambda e: e.tensor_copy(out=sel3[:, :, 0:2, :], in_=identv.unsqueeze(3).to_broadcast([64, NP, 2, 128])), writes=['sel'])
        p.add('dve', lambda e: e.tensor_copy(out=sel3[:, :, 2, :].rearrange("k q (j m) -> k q j m", j=2),
                                             in_=identv.unsqueeze(3).to_broadcast([64, NP, 2, 64])), writes=['sel'])
        it = 0
        di = 0
        xi = 0
        hi_ = 0
        ui_ = 0
        ti_ = 0
        for g in range(NGR):
            p.load(ft[:], cx.aT[:, g * G:(g + 1) * G].rearrange("(c p) t -> p c t", p=128), writes=['ft'])
            p.load(gt[:], cx.gT[:, g * G:(g + 1) * G], writes=['gt'])
            for pr in range(NP + 1):
                shared = (pr == NP)
                s = it % 2
                it += 1
                if not shared:
                    p.load(wg[:], cx.img_eg2[l][pr], writes=['wg'])
                    p.load(wu[:], cx.img_eu2[l][pr], writes=['wu'])
                    p.store(wd[:], cx.img_ed2[l][pr], writes=['wd'])
                    tiles = [(0, 128), (1, 128), (2, 128)]
                else:
                    p.load(wg[:, 0:KC * FF], cx.img_sg[l], writes=['wg'])
                    p.load(wu[:, 0:KC * FF], cx.img_su[l], writes=['wu'])
                    p.store(wd[:, 0:2 * D], cx.img_sd[l], writes=['wd'])
                    tiles = [(0, 128), (1, 64)]
                for (wt_, ps_, nm, wk) in ((wg, hg, 'hg', 'wg'), (wu, hu, 'hu', 'wu')):
                    for (mt, rows) in tiles:
                        if nm == 'hg':
                            pb = hi_ % 2
                            hi_ += 1
                        else:
                            pb = ui_ % 2
                            ui_ += 1
                        for k in range(KC):
                            if not shared:
                                lw = wt_[:, k * 384 + mt * 128:k * 384 + (mt + 1) * 128]
                            else:
                                lw = wt_[:, k * FF + mt * 128:k * FF + mt * 128 + rows]
                            p.mm(ps_[0:rows, pb, 0:G], lw, ft[:, k, :], k == 0, k == KC - 1, reads=[wk, 'ft'], writes=[(nm, pb)])
                        if nm == 'hg':
                            p.add('act', lambda e, mt=mt, rows=rows, pb=pb: e.activation(out=sil[0:rows, mt, :], in_=hg[0:rows, pb, 0:G], func=AF.Silu),
                                  reads=[('hg', pb)], writes=[('sil', mt)])
                        elif not shared:
                            p.mm(gb[:, 0:G], sel3[:, pr, mt, :], gt[:, :], True, True, reads=['sel', 'gt'], writes=['gb'])
                            t = ti_ % 2
                            ti_ += 1
                            p.add('dve', lambda e, mt=mt, pb=pb, t=t: e.tensor_tensor(out=t1[:, t, :], in0=sil[:, mt, :], in1=hu[:, pb, 0:G], op=ALU.mult),
                                  reads=[('sil', mt), ('hu', pb)], writes=[('t1', t)])
                            p.add('dve', lambda e, mt=mt, t=t, s=s: e.tensor_tensor(out=act[:, s, mt, :], in0=t1[:, t, :], in1=gb[:, 0:G], op=ALU.mult),
                                  reads=[('t1', t), 'gb'], writes=[('act', s, mt)])
                        else:
                            p.add('dve', lambda e, mt=mt, rows=rows, pb=pb, s=s: e.tensor_tensor(out=act[0:rows, s, mt, :], in0=sil[0:rows, mt, :], in1=hu[0:rows, pb, 0:G], op=ALU.mult),
                                  reads=[('sil', mt), ('hu', pb)], writes=[('act', s, mt)])
                for c in range(KC):
                    db = di % 3
                    di += 1
                    if not shared:
                        for mt in range(3):
                            p.mm(dn[:, db, 0:G], wd[:, mt * D + c * 128:mt * D + (c + 1) * 128], act[:, s, mt, :], mt == 0, mt == 2,
                                 reads=['wd', ('act', s, mt)], writes=[('dn', db)])
                    else:
                        p.mm(dn[:, db, 0:G], wd[:, c * 128:(c + 1) * 128], act[:, s, 0, :], True, False, reads=['wd', ('act', s, 0)], writes=[('dn', db)])
                        p.mm(dn[:, db, 0:G], wd[0:64, D + c * 128:D + (c + 1) * 128], act[0:64, s, 1, :], False, True, reads=['wd', ('act', s, 1)], writes=[('dn', db)])
                    if pr == 0:
                        p.add('dve', lambda e, c=c, db=db: e.tensor_copy(out=acc[:, c, :], in_=dn[:, db, 0:G]), reads=[('dn', db)], writes=[('acc', c)])
                    else:
                        p.add('dve', lambda e, c=c, db=db: e.tensor_tensor(out=acc[:, c, :], in0=acc[:, c, :], in1=dn[:, db, 0:G], op=ALU.add),
                              reads=[('dn', db), ('acc', c)], writes=[('acc', c)])
            for c in range(KC):
                s = xi % 2
                xi += 1
                key = ('xs', s)
                xa = cx.xT[c * 128:(c + 1) * 128, g * G:(g + 1) * G]
                p.load(xs[:, s, :], xa, writes=[key])
                for (c0, ncol, r) in segs_of_group(cx, g):
                    p.add('dve', lambda e, s=s, c=c, c0=c0, ncol=ncol, r=r: e.scalar_tensor_tensor(
                        out=xs[:, s, c0:c0 + ncol], in0=acc[:, c, c0:c0 + ncol], scalar=mv[:, 5, c, r:r + 1], in1=xs[:, s, c0:c0 + ncol],
                        op0=ALU.mult, op1=ALU.add), reads=[('acc', c), key, 'mv'], writes=[key])
                p.store(xa, xs[:, s, :], reads=[key])
        p.emit()


def build(B, S, layers, n_a, n_b, n_c):
    L = len(layers)
    nc = bass.Bass("TRN2", target_bir_lowering=False)
    cx = Ctx()
    cx.nc = nc
    cx.B, cx.S, cx.C = B, S, CTX
    cx.TB = S + CTX
    cx.NT = B * cx.TB
    NT = cx.NT
    assert cx.TB % G == 0 and NT % (3 * G) == 0

    kinds = [k for (k, j) in layers]
    cx.in_names = []

    def inp(name, shape, need=True):
        if not need:
            return None
        cx.in_names.append(name)
        return nc.dram_tensor(name, list(shape), F32, kind="ExternalInput").ap()

    def scr(name, shape, dt):
        return nc.dram_tensor(name, list(shape), dt).ap()

    xT0 = inp("xT0", [D, NT])
    cx.csT = inp("csT", [D, B + 1])
    ada_down = inp("ada_down", [L, D, 512])
    ada_up = inp("ada_up", [L, 512, 6 * D])
    cx.ada_bT = inp("ada_bT", [L, 128, 192])
    cx.n1gT = inp("n1gT", [L, 128, KC])
    cx.n2gT = inp("n2gT", [L, 128, KC])
    cx.fgT = inp("fgT", [128, KC])
    a_w_qkv = inp("a_w_qkv", [max(n_a, 1), D, 6144], 0 in kinds)
    a_w_o = inp("a_w_o", [max(n_a, 1), D, D], 0 in kinds)
    cx.a_sink = inp("a_sink", [max(n_a, 1), 32], 0 in kinds)
    b_w_qkv = inp("b_w_qkv", [max(n_b, 1), D, 3 * D], 1 in kinds)
    b_w_o = inp("b_w_o", [max(n_b, 1), D, D], 1 in kinds)
    cx.b_lamT = inp("b_lamT", [128, 4], 1 in kinds)
    cx.b_sgT = inp("b_sgT", [128, 2], 1 in kinds)
    c_w_in = inp("c_w_in", [max(n_c, 1), D, 3 * D], 2 in kinds)
    cx.c_convT = inp("c_convT", [128, KC, 3], 2 in kinds)
    c_w_out = inp("c_w_out", [max(n_c, 1), D, D], 2 in kinds)
    cx.router_w = inp("router_w", [L, D, NE])
    cx.router_b = inp("router_b", [L, NE])
    exp_gate = inp("exp_gate", [L, NE, D, FF])
    exp_up = inp("exp_up", [L, NE, D, FF])
    exp_down = inp("exp_down", [L, NE, FF, D])
    sh_gate = inp("sh_gate", [L, D, FF])
    sh_up = inp("sh_up", [L, D, FF])
    sh_down = inp("sh_down", [L, FF, D])
    identD = inp("ident", [128, 128])
    cos2D = inp("cos2", [128, S])
    sin2D = inp("sin2", [128, S])
    wmaskD = inp("wmask", [128, 384])
    cx.outT = nc.dram_tensor("outT", [D, NT], F32, kind="ExternalOutput").ap()

    cx.xT = scr("xT", [D, NT], F32)
    cx.aT = scr("aT", [D, NT], BF16)
    cx.gT = scr("gT", [NE, NT], BF16)
    cx.qkvT = scr("qkvT", [3 * D, NT], BF16)
    cx.vN = scr("vN", [NT, D], BF16)
    cx.oT = scr("oT", [D, NT], BF16)
    cx.img = {}
    jobs = []

    def img_proj(name, src, nl, K, N, slabw):
        kc = K // 128
        nslab = N // slabw
        im = scr("im_" + name, [nl, nslab, 128, kc * slabw], BF16)
        cx.img[name] = im
        for l_ in range(nl):
            v = src[l_].rearrange("(c p) n -> p c n", p=128)
            for s in range(nslab):
                jobs.append((v[:, :, s * slabw:(s + 1) * slabw], im[l_, s], 128, kc, slabw))

    kinds = [k for (k, j) in layers]
    img_proj('ada_down', ada_down, L, D, 512, 512)
    img_proj('ada_up', ada_up, L, 512, 6 * D, 2048)
    if 0 in kinds:
        img_proj('a_w_qkv', a_w_qkv, n_a, D, 6144, 512)
        img_proj('a_w_o', a_w_o, n_a, D, D, 512)
    if 1 in kinds:
        img_proj('b_w_qkv', b_w_qkv, n_b, D, 3 * D, 512)
        img_proj('b_w_o', b_w_o, n_b, D, D, 512)
    if 2 in kinds:
        img_proj('c_w_in', c_w_in, n_c, D, 3 * D, 512)
        img_proj('c_w_out', c_w_out, n_c, D, D, 512)
    cx.img_eg2 = [scr("im_eg%d" % l_, [NE // 2, 128, 2 * KC * FF], BF16) for l_ in range(L)]
    cx.img_eu2 = [scr("im_eu%d" % l_, [NE // 2, 128, 2 * KC * FF], BF16) for l_ in range(L)]
    cx.img_ed2 = [scr("im_ed%d" % l_, [NE // 2, 128, 3 * D], BF16) for l_ in range(L)]
    cx.img_sg = [scr("im_sg%d" % l_, [128, KC * FF], BF16) for l_ in range(L)]
    cx.img_su = [scr("im_su%d" % l_, [128, KC * FF], BF16) for l_ in range(L)]
    cx.img_sd = [scr("im_sd%d" % l_, [128, 2 * D], BF16) for l_ in range(L)]
    for l_ in range(L):
        jobs.append((sh_gate[l_].rearrange("(c p) f -> p c f", p=128), cx.img_sg[l_], 128, KC, FF))
        jobs.append((sh_up[l_].rearrange("(c p) f -> p c f", p=128), cx.img_su[l_], 128, KC, FF))
        jobs.append((sh_down[l_][0:128, :].rearrange("p (a n) -> p a n", a=1), cx.img_sd[l_][:, 0:D], 128, 1, D))
        jobs.append((sh_down[l_][128:192, :].rearrange("p (a n) -> p a n", a=1), cx.img_sd[l_][0:64, D:2 * D], 64, 1, D))

    from contextlib import ExitStack
    with ExitStack() as stack:
        cx.gl = Glob(nc, stack)
        E = stack.enter_context
        cx.modv = E(nc.sbuf_tensor(U("modv"), [128, 6, KC, B + 1], F32))
        cx.ident = E(nc.sbuf_tensor(U("identS"), [128, 128], F32))
        cx.identB = E(nc.sbuf_tensor(U("identB"), [128, 128], BF16))
        cx.onesF = E(nc.sbuf_tensor(U("onesF"), [128, 128], F32))
        cx.onesB = E(nc.sbuf_tensor(U("onesB"), [128, 128], BF16))
        cx.cos2 = E(nc.sbuf_tensor(U("cos2S"), [128, S], F32))
        cx.sin2 = E(nc.sbuf_tensor(U("sin2S"), [128, S], F32))
        cx.wmask = E(nc.sbuf_tensor(U("wmaskS"), [128, 384], BF16))
        wm32 = E(nc.sbuf_tensor(U("wm32"), [128, 384], F32))
        p = Prog(cx.gl)
        p.load(cx.ident[:], identD, writes=['id'])
        p.load(cx.cos2[:], cos2D, writes=['cs'])
        p.load(cx.sin2[:], sin2D, writes=['cs'])
        p.load(wm32[:], wmaskD, writes=['wm'])
        p.add('dve', lambda e: e.tensor_copy(out=cx.identB[:], in_=cx.ident[:]), reads=['id'], writes=['idb'])
        p.add('dve', lambda e: e.tensor_copy(out=cx.wmask[:], in_=wm32[:]), reads=['wm'], writes=['wmb'])
        p.add('dve', lambda e: e.memset(cx.onesF[:], 1.0), writes=['of'])
        p.add('dve', lambda e: e.memset(cx.onesB[:], 1.0), writes=['ob'])
        with nc.sbuf_tensor(U("in_x"), [128, 2, 8192], F32) as xb:
            i = 0
            for r0 in range(0, D, 128):
                for c0 in range(0, NT, 8192):
                    n = min(8192, NT - c0)
                    s = i % 2
                    i += 1
                    p.load(xb[:, s, 0:n], xT0[r0:r0 + 128, c0:c0 + n], writes=[('xb', s)])
                    p.store(cx.xT[r0:r0 + 128, c0:c0 + n], xb[:, s, 0:n], reads=[('xb', s)])
            p.emit()
        phase_convert(cx, jobs)
        phase_convert_pairs(cx, L, exp_gate, exp_up, exp_down)
        for li, (kind, j) in enumerate(layers):
            if DEBUG_MODE == 'convert':
                break
            phase_mod(cx, li)
            phase_norm(cx, li, 1)
            if kind == 0:
                modes = ['T'] * 10 + ['N'] * 2
                run_proj_store(cx, cx.aT, cx.img['a_w_qkv'][j], 12, outT=cx.qkvT, outN=cx.vN, modes=modes,
                               col_of_slab=lambda s: (s - 10) * 512)
                phase_attn_a(cx, j)
                run_proj_store(cx, cx.oT, cx.img['a_w_o'][j], 8, resid_gate=2)
            elif kind == 1:
                modes = ['T'] * 16 + ['N'] * 8
                run_proj_store(cx, cx.aT, cx.img['b_w_qkv'][j], 24, outT=cx.qkvT, outN=cx.vN, modes=modes,
                               col_of_slab=lambda s: (s - 16) * 512)
                phase_attn_b(cx, cx.layer_ids[li])
                run_proj_store(cx, cx.oT, cx.img['b_w_o'][j], 8, resid_gate=2)
            else:
                run_proj_store(cx, cx.aT, cx.img['c_w_in'][j], 24, outT=cx.qkvT)
                phase_conv(cx)
                run_proj_store(cx, cx.oT, cx.img['c_w_out'][j], 8, resid_gate=2)
            if DEBUG_MODE == 'nomoe':
                continue
            phase_norm(cx, li, 2)
            phase_moe(cx, li)
        phase_norm(cx, 0, 1, final=True)
    cx.n_ins = cx.gl.n_ins
    return nc, cx


def rope_tables(S):
    rows = S // 64
    row = np.repeat(np.arange(rows), 64).astype(np.float32)
    col = np.tile(np.arange(64), rows).astype(np.float32)
    n_freq = HD // 4
    inv = (10000.0 ** (-np.arange(n_freq, dtype=np.float32) / n_freq)).astype(np.float32)
    ang = np.concatenate([row[:, None] * inv, col[:, None] * inv], axis=-1)
    cos, sin = np.cos(ang).T.astype(np.float32), np.sin(ang).T.astype(np.float32)
    cos2 = np.concatenate([cos, cos], axis=0)
    sin2 = np.concatenate([-sin, sin], axis=0)
    return np.ascontiguousarray(cos2), np.ascontiguousarray(sin2)


def window_mask():
    i = np.arange(128)[:, None]
    jj = np.arange(128)[None, :]
    m = np.concatenate([(i <= jj), np.ones((128, 128), bool), (jj <= i)], axis=1)
    return m.astype(np.float32)


def prep_shared(inputs, layers, layer_ids, S):
    f = lambda a: np.ascontiguousarray(np.asarray(a, dtype=np.float32))
    li = list(layer_ids)

    def pick(a):
        a = f(a)
        return a if li == list(range(a.shape[0])) else np.ascontiguousarray(a[li])
    pm = lambda a: np.ascontiguousarray(a.reshape(a.shape[0], -1, 128).transpose(0, 2, 1))
    m = {
        'ada_down': pick(inputs['ada_down']), 'ada_up': pick(inputs['ada_up']),
        'ada_bT': pm(pick(inputs['ada_b'])), 'n1gT': pm(pick(inputs['norm1_g'])), 'n2gT': pm(pick(inputs['norm2_g'])),
        'fgT': pm(f(inputs['final_g'])[None])[0],
        'a_w_qkv': f(inputs['a_w_qkv']), 'a_w_o': f(inputs['a_w_o']), 'a_sink': f(inputs['a_sink']),
        'b_w_qkv': f(inputs['b_w_qkv']), 'b_w_o': f(inputs['b_w_o']),
        'b_lamT': np.ascontiguousarray(f(inputs['b_lam'])[0].T), 'b_sgT': np.ascontiguousarray(f(inputs['b_subln_g'])[0].reshape(2, 128).T),
        'c_w_in': f(inputs['c_w_in']), 'c_convT': np.ascontiguousarray(f(inputs['c_conv'])[0].reshape(3, KC, 128).transpose(2, 1, 0)),
        'c_w_out': f(inputs['c_w_out']),
        'router_w': pick(inputs['router_w']), 'router_b': pick(inputs['router_b']),
        'exp_gate': pick(inputs['exp_gate']), 'exp_up': pick(inputs['exp_up']), 'exp_down': pick(inputs['exp_down']),
        'sh_gate': pick(inputs['sh_gate']), 'sh_up': pick(inputs['sh_up']), 'sh_down': pick(inputs['sh_down']),
        'ident': np.eye(128, dtype=np.float32), 'wmask': window_mask(),
    }
    m['cos2'], m['sin2'] = rope_tables(S)
    return m


def run(inputs, layer_ids, layers=None):
    f = lambda a: np.ascontiguousarray(np.asarray(a, dtype=np.float32))
    x, ctx, c, c_ctx = f(inputs['x']), f(inputs['ctx']), f(inputs['c']), f(inputs['c_ctx'])
    B, S, _ = x.shape
    if layers is None:
        layers = [(i % 3, i // 3) for i in layer_ids]
    n_a, n_b, n_c = inputs['a_w_qkv'].shape[0], inputs['b_w_qkv'].shape[0], inputs['c_w_in'].shape[0]
    Ctx.layer_ids = list(layer_ids)
    nc, cx = build(1, S, layers, n_a, n_b, n_c)
    shared = prep_shared(inputs, layers, layer_ids, S)
    maps = []
    for b in range(B):
        m = dict(shared)
        m['xT0'] = np.ascontiguousarray(np.concatenate([x[b], ctx[b]], axis=0).T)
        m['csT'] = np.ascontiguousarray(np.stack([c[b], c_ctx], axis=0).T)
        maps.append({k: v for k, v in m.items() if k in cx.in_names})
    res = run_bass_kernel_spmd(nc, maps, core_ids=list(range(B)))
    out = np.stack([res.results[b]["outT"].T[:S, :] for b in range(B)], axis=0)
    return np.ascontiguousarray(out.astype(np.float32))


def kernel(**inputs):
    return run(inputs, list(range(4)))
```
